# Optimizing a Trainium2 kernel written in Bass

```python
import math
import jax
import jax.numpy as jnp
from jax import lax
import numpy as np

D_MODEL = 1024
BATCH = 16
SEQ = 2048
DEPTH = 2

GRID_W = 64
CTX_LEN = 256
N_MIXERS = 2
NORM_EPS = 1e-6
S5_GROUP = 16
S5_GROUPS = D_MODEL // S5_GROUP
S5_STATE = 64
S5_CHUNK = 128
S5_DT_MIN = 1e-3
S5_DT_MAX = 1e-1
DA_HEAD_DIM = 64
DA_VDIM = 2 * DA_HEAD_DIM
DA_HEADS = D_MODEL // DA_VDIM
Q_BLOCK = 128
ROPE_BASE = 10000.0
N_EXPERTS = 32
TOP_K = 4
D_FF = D_MODEL
SWIGLU_LIMIT = 7.0
SWIGLU_ALPHA = 1.702
MOE_BLOCK = 128

kernel_name = 'hybrid_s5_diffattn_moe_dit'


def rmsnorm(x, g):
    xf = x.astype(jnp.float32)
    y = xf * lax.rsqrt(jnp.mean(xf * xf, axis=-1, keepdims=True) + NORM_EPS)
    return (y * g.astype(jnp.float32)).astype(x.dtype)


def axial_rope(n_tokens, dim):
    rows = n_tokens // GRID_W
    row = jnp.repeat(jnp.arange(rows, dtype=jnp.float32), GRID_W)
    col = jnp.tile(jnp.arange(GRID_W, dtype=jnp.float32), rows)
    n_freq = dim // 4
    inv_freq = jnp.exp(-math.log(ROPE_BASE) * jnp.arange(n_freq, dtype=jnp.float32) / n_freq)
    ang = jnp.concatenate([row[:, None] * inv_freq, col[:, None] * inv_freq], axis=-1)
    return jnp.cos(ang), jnp.sin(ang)


def apply_rope(x, cos, sin):
    half = x.shape[-1] // 2
    x1, x2 = x[..., :half], x[..., half:]
    out = jnp.concatenate([x1 * cos - x2 * sin, x2 * cos + x1 * sin], axis=-1)
    return out.astype(x.dtype)


def s5_discretise(a_re, a_im, log_dt, b_re, b_im):
    a = lax.complex(a_re.astype(jnp.float32), a_im.astype(jnp.float32))
    dt = jnp.exp(log_dt.astype(jnp.float32))[:, None]
    log_lam = a * dt
    b = lax.complex(b_re.astype(jnp.float32), b_im.astype(jnp.float32))
    b_bar = ((jnp.exp(log_lam) - 1.0) / a)[..., None] * b
    return log_lam, b_bar


def _linear_combine(left, right):
    a1, b1 = left
    a2, b2 = right
    return a1 * a2, a2 * b1 + b2


def s5_scan(u, log_lam, b_bar, c_mat, h0):
    bsz, n = u.shape[0], u.shape[1]
    n_chunks = n // S5_CHUNK
    u_chunks = jnp.moveaxis(u.reshape((bsz, n_chunks, S5_CHUNK) + u.shape[2:]), 1, 0)
    lam = jnp.exp(log_lam)
    steps = jnp.arange(1, S5_CHUNK + 1, dtype=jnp.float32)[:, None, None]
    lam_pow = jnp.exp(log_lam[None] * steps)

    def step(h, u_c):
        bu = jnp.einsum('btgc,gpc->btgp', u_c.astype(jnp.complex64), b_bar)
        lam_b = jnp.broadcast_to(lam, bu.shape)
        _, hs = lax.associative_scan(_linear_combine, (lam_b, bu), axis=1)
        hs = hs + lam_pow[None] * h[:, None]
        y = jnp.einsum('btgp,gcp->btgc', hs, c_mat).real
        return hs[:, -1], y

    h_fin, ys = lax.scan(step, h0, u_chunks)
    return jnp.moveaxis(ys, 0, 1).reshape(u.shape), h_fin


def s5_mixer(h_lat, h_ctx, a_re, a_im, log_dt, b_re, b_im, c_re, c_im, d_skip, w_glu, b_glu, need_ctx):
    def grouped(h):
        return h.astype(jnp.float32).reshape(h.shape[:2] + (S5_GROUPS, S5_GROUP))
    u_l, u_c = grouped(h_lat), grouped(h_ctx)
    y_l = jnp.zeros_like(u_l)
    y_c = jnp.zeros_like(u_c)
    for d in range(2):
        log_lam, b_bar = s5_discretise(a_re[d], a_im[d], log_dt[d], b_re[d], b_im[d])
        c_mat = lax.complex(c_re[d].astype(jnp.float32), c_im[d].astype(jnp.float32))
        orient = (lambda t: jnp.flip(t, axis=1)) if d == 1 else (lambda t: t)
        h0 = jnp.zeros((u_c.shape[0], S5_GROUPS, S5_STATE), jnp.complex64)
        yc, h_ctx_fin = s5_scan(orient(u_c), log_lam, b_bar, c_mat, h0)
        yl, _ = s5_scan(orient(u_l), log_lam, b_bar, c_mat, h_ctx_fin)
        y_l = y_l + orient(yl)
        y_c = y_c + orient(yc)
    d_g = d_skip.astype(jnp.float32).reshape(S5_GROUPS, S5_GROUP)

    def finish(y, u, dtype):
        y = (y + d_g * u).reshape(y.shape[:2] + (D_MODEL,))
        y = jax.nn.gelu(y).astype(dtype)
        a, g = jnp.split(y @ w_glu + b_glu, 2, axis=-1)
        return a * jax.nn.sigmoid(g)

    out_l = finish(y_l, u_l, h_lat.dtype)
    out_c = finish(y_c, u_c, h_ctx.dtype) if need_ctx else None
    return out_l, out_c


def diff_core(q, k, v, lam):
    s = jnp.einsum('bqhcd,bkhcd->bhcqk', q, k).astype(jnp.float32) * (DA_HEAD_DIM ** -0.5)
    p = jax.nn.softmax(s, axis=-1)
    a = p[:, :, 0] - lam * p[:, :, 1]
    return jnp.einsum('bhqk,bkhe->bqhe', a.astype(v.dtype), v)


def diff_attention(h_lat, h_ctx, w_qkv, w_o, q_gain, k_gain, lam_q1, lam_k1, lam_q2, lam_k2,
                   sub_gain, lambda_init, need_ctx):
    bsz, n_lat = h_lat.shape[0], h_lat.shape[1]

    def project(h):
        q, k, v = jnp.split(h @ w_qkv, 3, axis=-1)
        shp = h.shape[:2] + (DA_HEADS, 2, DA_HEAD_DIM)
        q = rmsnorm(q.reshape(shp), q_gain)
        k = rmsnorm(k.reshape(shp), k_gain)
        v = v.reshape(h.shape[:2] + (DA_HEADS, DA_VDIM))
        return q, k, v

    q_l, k_l, v_l = project(h_lat)
    cos, sin = axial_rope(n_lat, DA_HEAD_DIM)
    cos = cos[:, None, None, :]
    sin = sin[:, None, None, :]
    q_l = apply_rope(q_l, cos, sin)
    k_l = apply_rope(k_l, cos, sin)
    q_c, k_c, v_c = project(h_ctx)
    k_all = jnp.concatenate([k_c, k_l], axis=1)
    v_all = jnp.concatenate([v_c, v_l], axis=1)
    lam = (jnp.exp(jnp.sum(lam_q1.astype(jnp.float32) * lam_k1.astype(jnp.float32)))
           - jnp.exp(jnp.sum(lam_q2.astype(jnp.float32) * lam_k2.astype(jnp.float32)))
           + lambda_init)

    def finish(o):
        o = rmsnorm(o, sub_gain) * (1.0 - lambda_init)
        return o.reshape(o.shape[:2] + (D_MODEL,)) @ w_o

    n_blk = n_lat // Q_BLOCK
    q_blocks = jnp.moveaxis(q_l.reshape((bsz, n_blk, Q_BLOCK) + q_l.shape[2:]), 1, 0)
    o_blocks = lax.map(lambda qb: diff_core(qb, k_all, v_all, lam), q_blocks)
    o_l = jnp.moveaxis(o_blocks, 0, 1).reshape((bsz, n_lat, DA_HEADS, DA_VDIM))
    y_l = finish(o_l)
    y_c = finish(diff_core(q_c, k_c, v_c, lam)) if need_ctx else None
    return y_l, y_c


def moe_ffn(h, w_router, b_router, w_gu, b_gu, w_down, b_down):
    n_tok, d = h.shape
    logits = (h @ w_router).astype(jnp.float32) + b_router.astype(jnp.float32)
    top_logit, top_e = lax.top_k(logits, TOP_K)
    gates = jax.nn.softmax(top_logit, axis=-1)
    nk = n_tok * TOP_K
    flat_e = top_e.reshape(nk)
    flat_tok = jnp.arange(nk, dtype=jnp.int32) // TOP_K
    flat_gate = gates.reshape(nk)
    order = jnp.argsort(flat_e, stable=True)
    sorted_e = flat_e[order]
    counts = jnp.bincount(flat_e, length=N_EXPERTS)
    padded = (counts + MOE_BLOCK - 1) // MOE_BLOCK * MOE_BLOCK
    pad_end = jnp.cumsum(padded)
    pad_start = pad_end - padded
    start = jnp.cumsum(counts) - counts
    dest = pad_start[sorted_e] + jnp.arange(nk, dtype=jnp.int32) - start[sorted_e]
    n_blocks = -(-nk // MOE_BLOCK) + N_EXPERTS
    buf = n_blocks * MOE_BLOCK
    tok_buf = jnp.zeros((buf,), jnp.int32).at[dest].set(flat_tok[order])
    gate_buf = jnp.zeros((buf,), jnp.float32).at[dest].set(flat_gate[order])
    block_start = jnp.arange(n_blocks, dtype=jnp.int32) * MOE_BLOCK
    block_e = jnp.minimum(jnp.searchsorted(pad_end, block_start, side='right'), N_EXPERTS - 1)

    def expert_block(args):
        toks, e = args
        xb = h[toks]
        gu = xb @ w_gu[e] + b_gu[e]
        gate, up = jnp.split(gu, 2, axis=-1)
        gate = jnp.minimum(gate, SWIGLU_LIMIT)
        up = jnp.clip(up, -SWIGLU_LIMIT, SWIGLU_LIMIT)
        act = (up + 1.0) * gate * jax.nn.sigmoid(SWIGLU_ALPHA * gate)
        return act @ w_down[e] + b_down[e]

    y = lax.map(expert_block, (tok_buf.reshape(n_blocks, MOE_BLOCK), block_e))
    y = y.reshape(buf, d) * gate_buf[:, None].astype(y.dtype)
    return jnp.zeros_like(h).at[tok_buf].add(y)


def setup_inputs(seed: int = 0) -> dict:
    key = jax.random.key(seed)
    ks = iter(jax.random.split(key, 48))
    f32 = jnp.float32
    n_a = (DEPTH + N_MIXERS - 1) // N_MIXERS
    n_b = DEPTH // N_MIXERS
    G, P, CG = S5_GROUPS, S5_STATE, S5_GROUP

    def nrm(shape, std):
        return jax.random.normal(next(ks), shape, f32) * std

    x = nrm((BATCH, SEQ, D_MODEL), 1.0)
    c = nrm((BATCH, D_MODEL), 1.0)
    ctx = nrm((BATCH, CTX_LEN, D_MODEL), 1.0)
    c_ctx = nrm((D_MODEL,), 1.0)
    w_ada = nrm((DEPTH, D_MODEL, 6 * D_MODEL), 0.3 * D_MODEL ** -0.5)
    b_ada = nrm((DEPTH, 6 * D_MODEL), 0.02)
    g_mix = 1.0 + nrm((DEPTH, D_MODEL), 0.02)
    g_ffn = 1.0 + nrm((DEPTH, D_MODEL), 0.02)
    s5_a_re = -0.5 + nrm((n_a, 2, G, P), 0.01)
    s5_a_im = math.pi * jnp.arange(P, dtype=f32) + nrm((n_a, 2, G, P), 0.01)
    s5_log_dt = jax.random.uniform(next(ks), (n_a, 2, G), f32, math.log(S5_DT_MIN), math.log(S5_DT_MAX))
    s5_b_re = nrm((n_a, 2, G, P, CG), (2 * CG) ** -0.5)
    s5_b_im = nrm((n_a, 2, G, P, CG), (2 * CG) ** -0.5)
    s5_c_re = nrm((n_a, 2, G, CG, P), P ** -0.5)
    s5_c_im = nrm((n_a, 2, G, CG, P), P ** -0.5)
    s5_d = nrm((n_a, D_MODEL), 0.5)
    s5_w_glu = nrm((n_a, D_MODEL, 2 * D_MODEL), D_MODEL ** -0.5)
    s5_b_glu = nrm((n_a, 2 * D_MODEL), 0.02)
    da_w_qkv = nrm((n_b, D_MODEL, 3 * D_MODEL), D_MODEL ** -0.5)
    da_w_o = nrm((n_b, D_MODEL, D_MODEL), D_MODEL ** -0.5)
    da_q_gain = 1.0 + nrm((n_b, DA_HEAD_DIM), 0.02)
    da_k_gain = 1.0 + nrm((n_b, DA_HEAD_DIM), 0.02)
    da_lam_q1 = nrm((n_b, DA_HEAD_DIM), 0.1)
    da_lam_k1 = nrm((n_b, DA_HEAD_DIM), 0.1)
    da_lam_q2 = nrm((n_b, DA_HEAD_DIM), 0.1)
    da_lam_k2 = nrm((n_b, DA_HEAD_DIM), 0.1)
    da_sub_gain = 1.0 + nrm((n_b, DA_VDIM), 0.02)
    moe_w_router = nrm((DEPTH, D_MODEL, N_EXPERTS), D_MODEL ** -0.5)
    moe_b_router = nrm((DEPTH, N_EXPERTS), 0.01)
    moe_w_gu = nrm((DEPTH, N_EXPERTS, D_MODEL, 2 * D_FF), D_MODEL ** -0.5)
    moe_b_gu = nrm((DEPTH, N_EXPERTS, 2 * D_FF), 0.02)
    moe_w_down = nrm((DEPTH, N_EXPERTS, D_FF, D_MODEL), D_FF ** -0.5)
    moe_b_down = nrm((DEPTH, N_EXPERTS, D_MODEL), 0.02)
    return {'x': x, 'c': c, 'ctx': ctx, 'c_ctx': c_ctx,
            'w_ada': w_ada, 'b_ada': b_ada, 'g_mix': g_mix, 'g_ffn': g_ffn,
            's5_a_re': s5_a_re, 's5_a_im': s5_a_im, 's5_log_dt': s5_log_dt,
            's5_b_re': s5_b_re, 's5_b_im': s5_b_im, 's5_c_re': s5_c_re, 's5_c_im': s5_c_im,
            's5_d': s5_d, 's5_w_glu': s5_w_glu, 's5_b_glu': s5_b_glu,
            'da_w_qkv': da_w_qkv, 'da_w_o': da_w_o, 'da_q_gain': da_q_gain, 'da_k_gain': da_k_gain,
            'da_lam_q1': da_lam_q1, 'da_lam_k1': da_lam_k1, 'da_lam_q2': da_lam_q2, 'da_lam_k2': da_lam_k2,
            'da_sub_gain': da_sub_gain,
            'moe_w_router': moe_w_router, 'moe_b_router': moe_b_router, 'moe_w_gu': moe_w_gu,
            'moe_b_gu': moe_b_gu, 'moe_w_down': moe_w_down, 'moe_b_down': moe_b_down}


def reference(x, c, ctx, c_ctx, w_ada, b_ada, g_mix, g_ffn,
              s5_a_re, s5_a_im, s5_log_dt, s5_b_re, s5_b_im, s5_c_re, s5_c_im,
              s5_d, s5_w_glu, s5_b_glu,
              da_w_qkv, da_w_o, da_q_gain, da_k_gain, da_lam_q1, da_lam_k1, da_lam_q2, da_lam_k2,
              da_sub_gain,
              moe_w_router, moe_b_router, moe_w_gu, moe_b_gu, moe_w_down, moe_b_down):
    silu_c = jax.nn.silu(c)
    silu_cc = jax.nn.silu(c_ctx)
    for i in range(DEPTH):
        last = i == DEPTH - 1
        j = i // N_MIXERS
        mod_l = (silu_c @ w_ada[i] + b_ada[i])[:, None, :]
        mod_c = silu_cc @ w_ada[i] + b_ada[i]
        sh1, sc1, gt1, sh2, sc2, gt2 = jnp.split(mod_l, 6, axis=-1)
        csh1, csc1, cgt1, csh2, csc2, cgt2 = jnp.split(mod_c, 6, axis=-1)
        h_l = rmsnorm(x, g_mix[i]) * (1.0 + sc1) + sh1
        h_c = rmsnorm(ctx, g_mix[i]) * (1.0 + csc1) + csh1
        if i % N_MIXERS == 0:
            y_l, y_c = s5_mixer(h_l, h_c, s5_a_re[j], s5_a_im[j], s5_log_dt[j], s5_b_re[j], s5_b_im[j],
                                s5_c_re[j], s5_c_im[j], s5_d[j], s5_w_glu[j], s5_b_glu[j], not last)
        else:
            lambda_init = 0.8 - 0.6 * math.exp(-0.3 * i)
            y_l, y_c = diff_attention(h_l, h_c, da_w_qkv[j], da_w_o[j], da_q_gain[j], da_k_gain[j],
                                      da_lam_q1[j], da_lam_k1[j], da_lam_q2[j], da_lam_k2[j],
                                      da_sub_gain[j], lambda_init, not last)
        x = x + gt1 * y_l
        if not last:
            ctx = ctx + cgt1 * y_c
        h_l = rmsnorm(x, g_ffn[i]) * (1.0 + sc2) + sh2
        if last:
            out = moe_ffn(h_l.reshape(-1, D_MODEL), moe_w_router[i], moe_b_router[i], moe_w_gu[i],
                          moe_b_gu[i], moe_w_down[i], moe_b_down[i])
            x = x + gt2 * out.reshape(x.shape)
        else:
            h_c = rmsnorm(ctx, g_ffn[i]) * (1.0 + csc2) + csh2
            n_c = h_c.shape[0] * h_c.shape[1]
            tokens = jnp.concatenate([h_c.reshape(-1, D_MODEL), h_l.reshape(-1, D_MODEL)], axis=0)
            out = moe_ffn(tokens, moe_w_router[i], moe_b_router[i], moe_w_gu[i],
                          moe_b_gu[i], moe_w_down[i], moe_b_down[i])
            ctx = ctx + cgt2 * out[:n_c].reshape(ctx.shape)
            x = x + gt2 * out[n_c:].reshape(x.shape)
    return x
```

```python
import numpy as np
from contextlib import ExitStack
import concourse.bass as bass
import concourse.mybir as mybir

F32 = mybir.dt.float32
F32R = mybir.dt.float32r
I32 = mybir.dt.int32
U32 = mybir.dt.uint32
ALU = mybir.AluOpType
AF = mybir.ActivationFunctionType
AX = mybir.AxisListType

ENGS = ['tensor', 'vector', 'scalar', 'gpsimd', 'sync']
DMA_SLOTS = {'sync': 12, 'gpsimd': 8, 'scalar': 6}


class Prog:
    def __init__(self, nc):
        self.nc = nc
        self.ops = {e: [] for e in ENGS}
        self.cnt = {e: 0 for e in ENGS}
        self.seen = {e: {} for e in ENGS}
        self.lastw = {}
        self.readers = {}
        self.dma_next = {q: 0 for q in DMA_SLOTS}
        self.dma_uses = {}
        self.dma_ep = {}
        self.epoch = {e: 0 for e in ENGS}
        self.used = set()
        self.nops = 0

    def _deps(self, eng, reads, writes):
        deps = {}

        def add(t):
            if t is None:
                return
            s, v = t
            if deps.get(s, 0) < v:
                deps[s] = v
        for r in reads:
            add(self.lastw.get(r))
        for w in writes:
            add(self.lastw.get(w))
            for s, v in self.readers.get(w, {}).items():
                add((s, v))
        waits = []
        for s, v in deps.items():
            if eng == 'tensor' and s[0] == 'tensor':
                continue
            if self.seen[eng].get(s, 0) >= v:
                continue
            self.seen[eng][s] = v
            waits.append((s, v))
        return waits

    def _update(self, tk, reads, writes):
        s, v = tk
        for w in writes:
            self.lastw[w] = tk
            self.readers[w] = {}
        for r in reads:
            d = self.readers.setdefault(r, {})
            if d.get(s, 0) < v:
                d[s] = v

    def op(self, eng, fn, reads=(), writes=()):
        waits = self._deps(eng, reads, writes)
        self.cnt[eng] += 1
        if self.cnt[eng] > 12000:
            self.epoch[eng] += 1
            self.cnt[eng] = 1
        ek = (eng, self.epoch[eng])
        self.used.add(ek)
        tk = (ek, self.cnt[eng])
        self.ops[eng].append((waits, fn, (ek, 1)))
        self._update(tk, reads, writes)
        self.nops += 1

    def dma(self, q, fn, reads=(), writes=()):
        waits = self._deps(q, reads, writes)
        n = DMA_SLOTS[q]
        slot = self.dma_next[q]
        self.dma_next[q] = (slot + 1) % n
        ep = self.dma_ep.get((q, slot), 0)
        if self.dma_uses.get(('d', q, slot, ep), 0) >= 700:
            ep += 1
            self.dma_ep[(q, slot)] = ep
            old = ('d', q, slot, ep - 1)
            if self.seen[q].get(old, 0) < 16 * 700:
                self.seen[q][old] = 16 * 700
                waits.append((old, 16 * 700))
        key = ('d', q, slot, ep)
        uses = self.dma_uses.get(key, 0)
        if uses > 0 and self.seen[q].get(key, 0) < 16 * uses:
            self.seen[q][key] = 16 * uses
            waits.append((key, 16 * uses))
        self.dma_uses[key] = uses + 1
        tk = (key, 16 * (uses + 1))
        self.ops[q].append((waits, fn, (key, 16)))
        self._update(tk, reads, writes)
        self.nops += 1

    def barrier(self):
        latest = {}
        for e in ENGS:
            if self.cnt[e] > 0:
                latest[(e, self.epoch[e])] = self.cnt[e]
        for key, uses in self.dma_uses.items():
            latest[key] = 16 * uses
        for e in ENGS:
            waits = []
            for s, v in latest.items():
                if self.seen[e].get(s, 0) < v:
                    self.seen[e][s] = v
                    waits.append((s, v))
            if waits:
                self.ops[e].append((waits, None, None))
        self.lastw.clear()
        self.readers.clear()

    def emit(self):
        nc = self.nc
        self.barrier()
        keys = sorted(self.used) + list(self.dma_uses.keys())
        with ExitStack() as st:
            semh = {}
            for i, k in enumerate(keys):
                semh[k] = st.enter_context(nc.semaphore("s%d" % i))
            block = st.enter_context(nc.Block())
            for e in ENGS:
                def body(engobj, e=e):
                    for waits, fn, inc in self.ops[e]:
                        for s, v in waits:
                            engobj.wait_ge(semh[s], v)
                        if fn is not None:
                            ins = fn(engobj)
                            ins.then_inc(semh[inc[0]], inc[1])
                getattr(block, e)(body)


class Arena:
    def __init__(self, big, ncols):
        self.big = big
        self.ncols = ncols
        self.off = 0
        self.gen = 0

    def reset(self):
        self.off = 0
        self.gen += 1

    def alloc(self, name, cols):
        cols = (cols + 1) // 2 * 2
        assert self.off + cols <= self.ncols, (name, self.off, cols, self.ncols)
        ap = self.big[:, self.off:self.off + cols]
        self.off += cols
        return ap, (name, self.gen)

BF16 = mybir.dt.bfloat16
from concourse.bass_utils import run_bass_kernel_spmd
import math

NB = 2; NL = 2048; NCX = 256; D = 1024
TL = NB * NL; TT = TL + NB * NCX
NT = TT // 128
CAPT = 8; CAP = CAPT * 128; NSLOT = 32 * CAP
HALF = CAP // 2
LAMBDA_INIT = 0.8 - 0.6 * math.exp(-0.3 * 1)
TWO_PI = 2.0 * math.pi


class Bld:
    def __init__(self, nc, P, big, pst, ncols):
        self.nc = nc; self.P = P; self.ar = Arena(big, ncols); self.pst = pst; self.pb = 0; self.brange = (0, 8); self.pools = {}

    def T(self, name, cols, dt=F32):
        if dt == BF16:
            ap, k = self.ar.alloc(name, (cols + 1) // 2)
            return ap.bitcast(BF16)[:, 0:cols], k
        ap, k = self.ar.alloc(name, cols)
        if dt != F32:
            ap = ap.bitcast(dt)
        return ap, k

    def bank(self):
        lo, hi = self.brange
        i = lo + self.pb % (hi - lo); self.pb += 1
        return self.pst[i][:], 'ps%d' % i

    def bankp(self, name, banks):
        idx = self.pools.get(name, 0); self.pools[name] = idx + 1
        i = banks[idx % len(banks)]
        return self.pst[i][:], 'ps%d' % i

    def mm(self, out, lhsT, rhs, start, stop, r, w):
        self.P.op('tensor', lambda e: e.matmul(out, lhsT=lhsT, rhs=rhs, start=start, stop=stop), r, w)

    def tr(self, out, in_, r, w):
        ident = self.ident
        self.P.op('tensor', lambda e: e.transpose(out, in_, ident), list(r) + [self.identk], w)

    def act(self, out, in_, func, r, w, **kw):
        self.P.op('scalar', lambda e: e.activation(out=out, in_=in_, func=func, **kw), r, w)

    def ts(self, eng, out, in0, s1, s2, op0, op1, r, w):
        if op1 is None:
            self.P.op(eng, lambda e: e.tensor_scalar(out=out, in0=in0, scalar1=s1, scalar2=None, op0=op0), r, w)
        else:
            self.P.op(eng, lambda e: e.tensor_scalar(out=out, in0=in0, scalar1=s1, scalar2=s2, op0=op0, op1=op1), r, w)

    def tt(self, eng, out, in0, in1, op, r, w):
        self.P.op(eng, lambda e: e.tensor_tensor(out=out, in0=in0, in1=in1, op=op), r, w)

    def stt(self, eng, out, in0, scalar, in1, op0, op1, r, w):
        self.P.op(eng, lambda e: e.scalar_tensor_tensor(out=out, in0=in0, scalar=scalar, in1=in1, op0=op0, op1=op1), r, w)

    def cp(self, eng, out, in_, r, w):
        if eng == 'scalar':
            self.P.op(eng, lambda e: e.copy(out=out, in_=in_), r, w)
        else:
            self.P.op(eng, lambda e: e.tensor_copy(out=out, in_=in_), r, w)

    def ms(self, eng, out, val, w):
        self.P.op(eng, lambda e: e.memset(out, val), (), w)

    def dma(self, q, out, in_, r, w, nc_ok=False):
        if nc_ok:
            self.P.dma(q, lambda e: e.dma_start(out=out, in_=in_, allow_slow_non_contiguous=True), r, w)
        else:
            self.P.dma(q, lambda e: e.dma_start(out=out, in_=in_), r, w)

    def phase(self):
        self.P.barrier()
        self.ar.reset()
        self.ar.off = self.persist


def tile_row(i):
    return i // 16 if i < 32 else 2


def run_pipeline(gens):
    active = []
    for g in gens:
        for a in list(active):
            if next(a, 'done') == 'done':
                active.remove(a)
        active.append(g)
        if next(g, 'done') == 'done':
            active.remove(g)
    while active:
        for a in list(active):
            if next(a, 'done') == 'done':
                active.remove(a)


def build_nc(stop_after=None, debug=False):
    nc = bass.Bass("TRN2", target_bir_lowering=False)

    def din(name, shape, dt=F32):
        return nc.dram_tensor(name, shape, dt, kind="ExternalInput").ap()

    def dscr(name, shape, dt=F32):
        return nc.dram_tensor(name, shape, dt, kind=("ExternalOutput" if debug else "Internal")).ap()

    x_in = din("x", [NB, NL, D]); ctx_in = din("ctx", [NB, NCX, D]); c_in = din("c", [NB, D]); cc_in = din("c_ctx", [1, D])
    w_ada = din("w_ada", [2, D, 6 * D]); b_ada = din("b_ada", [2, 6 * D]); g_mix = din("g_mix", [2, D]); g_ffn = din("g_ffn", [2, D])
    s5_a_re = din("s5_a_re", [1, 2, 64, 64]); s5_a_im = din("s5_a_im", [1, 2, 64, 64]); s5_log_dt = din("s5_log_dt", [1, 2, 64])
    s5_b_re = din("s5_b_re", [1, 2, 64, 64, 16]); s5_b_im = din("s5_b_im", [1, 2, 64, 64, 16])
    s5_c_re = din("s5_c_re", [1, 2, 64, 16, 64]); s5_c_im = din("s5_c_im", [1, 2, 64, 16, 64])
    s5_d = din("s5_d", [1, D]); s5_w_glu = din("s5_w_glu", [1, D, 2 * D]); s5_b_glu = din("s5_b_glu", [1, 2 * D])
    da_w_qkv = din("da_w_qkv", [1, D, 3 * D]); da_w_o = din("da_w_o", [1, D, D])
    da_q_gain = din("da_q_gain", [1, 64]); da_k_gain = din("da_k_gain", [1, 64])
    da_lam = [din("da_lam_" + n, [1, 64]) for n in ("q1", "k1", "q2", "k2")]
    da_sub_gain = din("da_sub_gain", [1, 128])
    moe_w_router = din("moe_w_router", [2, D, 32]); moe_b_router = din("moe_b_router", [2, 32])
    moe_w_gu = din("moe_w_gu", [2, 32, D, 2 * D]); moe_b_gu = din("moe_b_gu", [2, 32, 2 * D])
    moe_w_down = din("moe_w_down", [2, 32, D, D]); moe_b_down = din("moe_b_down", [2, 32, D])
    k_ident = din("k_ident", [128, 128]); k_ltri = din("k_ltri", [128, 128]); k_ones = din("k_ones", [128, 128])
    k_blk16 = din("k_blk16", [128, 128]); k_g2m = din("k_g2m", [128, 2]); k_blk64 = din("k_blk64", [128, 128])
    k_rot = din("k_rot", [128, 128]); k_cos = din("k_cos", [128, NL]); k_sin = din("k_sin", [128, NL])
    k_slot = din("k_slot", [128, 32]); k_pm = din("k_pm", [128, 2])
    out = nc.dram_tensor("out", [NB, NL, D], F32, kind="ExternalOutput").ap()
    cnt_out = nc.dram_tensor("cnt_out", [128, 64], F32, kind="ExternalOutput").ap()
    xa = dscr("xa", [TT, D]); mod = dscr("mod", [2, 3, 6 * D]); hT = dscr("hT", [D, TT]); sT = dscr("sT", [D, TT], BF16)
    Xs = dscr("Xs", [NSLOT, D], BF16); Ys = dscr("Ys", [NSLOT, D])
    Wblk_d = dscr("Wblk_d", [2, 128, 8, 8, 128], BF16); Bst_d = dscr("Bst_d", [2, 128, 8, 8, 2, 128], BF16)
    Cst_d = dscr("Cst_d", [2, 128, 8, 2, 32, 32], BF16)
    outf = out.rearrange("b n d -> (b n) d")

    with ExitStack() as st:
        NCOLS = 53000
        big_t = st.enter_context(nc.sbuf_tensor("big", [128, NCOLS], F32))
        pst = [st.enter_context(nc.psum_tensor("ps%d" % i, [128, 512], F32)) for i in range(8)]
        P = Prog(nc)
        B = Bld(nc, P, big_t[:], pst, NCOLS)
        T = B.T
        B.ident, B.identk = T("ident", 128)
        B.dma('sync', B.ident, k_ident, [], [B.identk])
        ltri, ltrik = T("ltri", 128); B.dma('sync', ltri, k_ltri, [], [ltrik])
        ones, onesk = T("ones", 128); B.dma('sync', ones, k_ones, [], [onesk])
        onesb, onesbk = T("onesb", 128, BF16); B.cp('vector', onesb, ones, [onesk], [onesbk])
        identb, identbk = T("identb", 128, BF16); B.cp('vector', identb, B.ident, [B.identk], [identbk])
        blk16, blk16k = T("blk16", 128); B.dma('sync', blk16, k_blk16, [], [blk16k])
        g2m, g2mk = T("g2m", 2); B.dma('sync', g2m, k_g2m, [], [g2mk])
        DEST, DESTk = T("DEST", NT * 4, I32); GATE, GATEk = T("GATE", NT * 4)
        DEST3 = DEST.rearrange("p (i k) -> p i k", k=4); GATE3 = GATE.rearrange("p (i k) -> p i k", k=4)
        persist_small = B.ar.off
        CA, CAk = T("CA", 2 * 8 * 2 * 64)
        CA5 = CA.rearrange("p (d i r g) -> p d i r g", d=2, i=8, r=2)
        CBn, CBnk = T("CBn", 2 * 8 * 64); CBn4 = CBn.rearrange("p (d i g) -> p d i g", d=2, i=8)
        CBp, CBpk = T("CBp", 2 * 8 * 64); CBp4 = CBp.rearrange("p (d i g) -> p d i g", d=2, i=8)
        B.persist = B.ar.off
        xkeys = [('xa', i) for i in range(NT)]

        def done(name):
            return stop_after == name

        for b in range(NB):
            B.dma('sync', xa[b * NL:(b + 1) * NL, :], x_in[b], [], xkeys[b * 16:(b + 1) * 16])
            B.dma('sync', xa[TL + b * NCX:TL + (b + 1) * NCX, :], ctx_in[b], [], xkeys[32 + 2 * b:34 + 2 * b])

        cT, cTk = T("cT", 24)
        cT3 = cT.rearrange("p (k r) -> p k r", r=3)
        for r in range(3):
            src = (c_in[r] if r < 2 else cc_in[0]).rearrange("(k p) -> p k", p=128)
            B.dma('sync', cT3[:, :, r], src, [], [cTk], nc_ok=True)
        B.act(cT, cT, AF.Silu, [cTk], [cTk])
        bt, btk = T("bt", 6 * D); modt, modtk = T("modt", 6 * D)
        wts = [T("wada%d" % i, 8 * 512) for i in range(2)]
        for l in range(2):
            B.dma('sync', bt[0:3, :], b_ada[l:l + 1, :].partition_broadcast(3), [modtk], [btk])
            for n in range(12):
                wt, wtk = wts[n % 2]
                wt3 = wt.rearrange("p (k n) -> p k n", n=512)
                B.dma('sync' if n % 2 == 0 else 'scalar', wt3, w_ada[l][:, n * 512:(n + 1) * 512].rearrange("(k p) n -> p k n", p=128), [], [wtk])
                ps, psk = B.bank()
                for k in range(8):
                    B.mm(ps[0:3, :], cT3[:, k, :], wt3[:, k, :], k == 0, k == 7, [cTk, wtk], [psk])
                B.tt('vector', modt[0:3, n * 512:(n + 1) * 512], ps[0:3, :], bt[0:3, n * 512:(n + 1) * 512], ALU.add, [psk, btk], [modtk])
            B.dma('sync', mod[l], modt[0:3, :], [modtk], [('mod', l)])
        if done('ada'):
            P.emit(); return nc

        def load_mod_tiles(l, gvec, off_sh, off_sc):
            gb, gbk = T("gvecb", D)
            B.dma('sync', gb, gvec[l:l + 1, :].partition_broadcast(128), [], [gbk])
            res = []
            for r in range(3):
                G, Gk = T("G%d" % r, D); S, Sk = T("S%d" % r, D)
                B.dma('sync', G, mod[l, r:r + 1, off_sc:off_sc + D].partition_broadcast(128), [('mod', l)], [Gk])
                B.dma('scalar', S, mod[l, r:r + 1, off_sh:off_sh + D].partition_broadcast(128), [('mod', l)], [Sk])
                B.stt('vector', G, G, 1.0, gb, ALU.add, ALU.mult, [Gk, gbk], [Gk])
                res.append((G, Gk, S, Sk))
            return res

        def load_gate_tiles(l, off):
            res = []
            for r in range(3):
                G, Gk = T("GT%d" % r, D)
                B.dma('sync', G, mod[l, r:r + 1, off:off + D].partition_broadcast(128), [('mod', l)], [Gk])
                res.append((G, Gk))
            return res

        def norm_tile(xt, xk, h, hk, mt, ss, ssk):
            G, Gk, S, Sk = mt
            B.ms('gpsimd', ss, 0.0, [ssk])
            B.act(h, xt, AF.Square, [xk, ssk], [hk, ssk], accum_out=ss[:, 0:1])
            B.ts('vector', ss[:, 1:2], ss[:, 0:1], 1.0 / D, 1e-6, ALU.mult, ALU.add, [ssk], [ssk])
            B.act(ss[:, 1:2], ss[:, 1:2], AF.Sqrt, [ssk], [ssk])
            B.P.op('vector', lambda e: e.reciprocal(out=ss[:, 1:2], in_=ss[:, 1:2]), [ssk], [ssk])
            B.stt('vector', h, xt, ss[:, 1:2], G, ALU.mult, ALU.mult, [xk, ssk, Gk, hk], [hk])
            B.tt('gpsimd', h, h, S, ALU.add, [hk, Sk], [hk])

        def norm_tile_g(xt, xk, h, hk, mt, ss, ssk):
            G, Gk, S, Sk = mt
            B.ms('gpsimd', ss, 0.0, [ssk])
            B.act(h, xt, AF.Square, [xk, ssk], [hk, ssk], accum_out=ss[:, 0:1])
            yield
            B.ts('vector', ss[:, 1:2], ss[:, 0:1], 1.0 / D, 1e-6, ALU.mult, ALU.add, [ssk], [ssk])
            B.act(ss[:, 1:2], ss[:, 1:2], AF.Sqrt, [ssk], [ssk])
            yield
            B.P.op('vector', lambda e: e.reciprocal(out=ss[:, 1:2], in_=ss[:, 1:2]), [ssk], [ssk])
            B.stt('vector', h, xt, ss[:, 1:2], G, ALU.mult, ALU.mult, [xk, ssk, Gk, hk], [hk])
            B.tt('gpsimd', h, h, S, ALU.add, [hk, Sk], [hk])
            yield

        def transpose_tile(h, hk, dst3, dstk):
            for half in range(2):
                ps, psk = B.bank()
                for kk in range(4):
                    k = half * 4 + kk
                    B.tr(ps[:, kk * 128:(kk + 1) * 128], h[:, k * 128:(k + 1) * 128], [hk], [psk])
                B.cp('scalar' if half == 0 else 'vector', dst3[:, half * 4:(half + 1) * 4, :], ps.rearrange("p (k t) -> p k t", t=128), [psk], [dstk])

        hT3 = hT.rearrange("(k p) t -> p k t", p=128)
        sT3 = sT.rearrange("(k p) t -> p k t", p=128)

        def phase_norm_T(l):
            B.phase()
            mts = load_mod_tiles(l, g_mix, 0, D)
            bufs = [(T("xt%d" % i, D), T("h%d" % i, D), T("hTt%d" % i, D), T("ss%d" % i, 2)) for i in range(5)]

            def tile_gen(i):
                (xt, xk), (h, hk), (ht, htk), (ss, ssk) = bufs[i % 5]
                B.dma('sync', xt, xa[i * 128:(i + 1) * 128, :], [xkeys[i]], [xk])
                yield
                for _ in norm_tile_g(xt, xk, h, hk, mts[tile_row(i)], ss, ssk):
                    yield
                ht3 = ht.rearrange("p (k t) -> p k t", t=128)
                transpose_tile(h, hk, ht3, htk)
                B.dma('scalar', hT3[:, :, i * 128:(i + 1) * 128], ht3, [htk], [('hT', i)])
            run_pipeline([tile_gen(i) for i in range(NT)])

        def bc3(ap2):
            return ap2.unsqueeze(2).to_broadcast([128, 32, 16])

        def phase_s5_setup():
            B.phase()
            dcol, dcolk = T("dcol", 8)
            B.dma('sync', dcol, s5_d[0].rearrange("(j p) -> p j", p=128), [], [dcolk], nc_ok=True)
            Wsb, Wsbk = T("Wsb", 8 * 8 * 128, BF16); Wsb4 = Wsb.rearrange("p (j e m) -> p j e m", j=8, e=8)
            Bsb, Bsbk = T("Bsb", 8 * 8 * 2 * 128, BF16); Bsb5 = Bsb.rearrange("p (j s r m) -> p j s r m", j=8, s=8, r=2)
            Csb, Csbk = T("Csb", 8 * 2 * 32 * 32, BF16); Csb5 = Csb.rearrange("p (t r g m) -> p t r g m", t=8, r=2, g=32)
            Are, Arek = T("Are", 32); Aim, Aimk = T("Aim", 32); DT, DTk = T("DT", 32)
            lr, lrk = T("lr", 32); li, lik = T("li", 32)
            LP, LPk = T("LP", 9 * 2 * 32); LP4 = LP.rearrange("p (e r g) -> p e r g", e=9, r=2)
            sm = [T("sm%d" % i, 32) for i in range(6)]
            ki, kik = T("ki", 32, I32)
            Bre, Brek = T("Bre", 512); Bim, Bimk = T("Bim", 512); Cre, Crek = T("Cre", 512); Cim, Cimk = T("Cim", 512)
            bbr, bbrk = T("bbr", 512); bbi, bbik = T("bbi", 512)
            t1, t1k = T("t1", 512); t2, t2k = T("t2", 512); Ere, Erek = T("Ere", 512); Eim, Eimk = T("Eim", 512)
            Cmr, Cmrk = T("Cmr", 1024); Cmi, Cmik = T("Cmi", 1024); Emr, Emrk = T("Emr", 1024); Emi, Emik = T("Emi", 1024)
            v3 = lambda a: a.rearrange("p (g c) -> p g c", c=16)
            v4 = lambda a: a.rearrange("p (g h c) -> p g h c", h=2, c=16)
            Cns = [T("Cn%d" % z, 8 * 64) for z in range(2)]
            Cxs = [T("Cx%d" % z, 128) for z in range(2)]; Cts = [T("Ct%d" % z, 128) for z in range(2)]
            pm, pmk = T("pm", 2); B.dma('sync', pm, k_pm, [], [pmk])
            for d in range(2):
                B.dma('sync', Are, s5_a_re[0, d].rearrange("(gp g2) p -> (g2 p) gp", g2=2), [], [Arek], nc_ok=True)
                B.dma('sync', Aim, s5_a_im[0, d].rearrange("(gp g2) p -> (g2 p) gp", g2=2), [], [Aimk], nc_ok=True)
                for g2 in range(2):
                    B.dma('sync', DT[g2 * 64:(g2 + 1) * 64, :], s5_log_dt[0, d:d + 1, :].rearrange("o (gp g2) -> o gp g2", g2=2)[:, :, g2].partition_broadcast(64), [], [DTk], nc_ok=True)
                B.dma('sync', v3(Bre), s5_b_re[0, d].rearrange("(gp g2) p c -> (g2 p) gp c", g2=2), [], [Brek], nc_ok=True)
                B.dma('scalar', v3(Bim), s5_b_im[0, d].rearrange("(gp g2) p c -> (g2 p) gp c", g2=2), [], [Bimk], nc_ok=True)
                for ci, (src, dst, dk) in enumerate(((s5_c_re, Cre, Crek), (s5_c_im, Cim, Cimk))):
                    Cn, Cnk = Cns[ci]
                    Cn3 = Cn.rearrange("p (j q) -> p j q", q=64)
                    B.dma('sync' if ci == 0 else 'scalar', Cn3, src[0, d].rearrange("(j g) c p -> (g c) j p", g=8), [], [Cnk])
                    for j in range(8):
                        Cx, Cxk = Cxs[j % 2]; Ct, Ctk = Cts[j % 2]
                        Cx3 = Cx.rearrange("p (h q) -> p h q", q=64)
                        for h in range(2):
                            B.ts('vector' if h == 0 else 'gpsimd', Cx3[:, h, :], Cn3[:, j, :], pm[:, h:h + 1], None, ALU.mult, None, [Cnk, pmk], [Cxk])
                        ps, psk = B.bank()
                        B.tr(ps[:, 0:128], Cx, [Cxk], [psk])
                        B.cp('scalar', Ct, ps[:, 0:128], [psk], [Ctk])
                        Ct4 = Ct.rearrange("p (q h c) -> p q h c", q=4, h=2)
                        B.tt('vector', v3(dst)[:, 4 * j:4 * j + 4, :], Ct4[:, :, 0, :], Ct4[:, :, 1, :], ALU.add, [Ctk], [dk])
                B.act(DT, DT, AF.Exp, [DTk], [DTk])
                B.tt('vector', lr, Are, DT, ALU.mult, [Arek, DTk], [lrk])
                B.tt('vector', li, Aim, DT, ALU.mult, [Aimk, DTk], [lik])
                B.ms('vector', LP4[:, 0, 0, :], 1.0, [LPk]); B.ms('vector', LP4[:, 0, 1, :], 0.0, [LPk])
                (mag, magk), (tq, tqk), (kf, kfk), (yy, yyk), (sn, snk), (den, denk) = sm
                for e in range(1, 9):
                    B.act(mag, lr, AF.Exp, [lrk], [magk], scale=float(e))
                    for which, off in ((1, 0.0), (0, 0.25)):
                        B.ts('vector', tq, li, e / TWO_PI, off, ALU.mult, ALU.add, [lik], [tqk])
                        B.cp('vector', ki, tq, [tqk], [kik])
                        B.cp('vector', kf, ki, [kik], [kfk])
                        B.ts('vector', kf, kf, -TWO_PI, off * TWO_PI, ALU.mult, ALU.add, [kfk], [kfk])
                        B.stt('vector', yy, li, float(e), kf, ALU.mult, ALU.add, [lik, kfk], [yyk])
                        B.act(sn, yy, AF.Sin, [yyk], [snk])
                        B.tt('vector', LP4[:, e, which, :], mag, sn, ALU.mult, [magk, snk], [LPk])
                for i8 in range(8):
                    e8 = 8 * (i8 + 1)
                    pr = []
                    B.act(mag, lr, AF.Exp, [lrk], [magk], scale=float(e8))
                    for which, off in ((1, 0.0), (0, 0.25)):
                        B.ts('vector', tq, li, e8 / TWO_PI, off, ALU.mult, ALU.add, [lik], [tqk])
                        B.cp('vector', ki, tq, [tqk], [kik])
                        B.cp('vector', kf, ki, [kik], [kfk])
                        B.ts('vector', kf, kf, -TWO_PI, off * TWO_PI, ALU.mult, ALU.add, [kfk], [kfk])
                        B.stt('vector', yy, li, float(e8), kf, ALU.mult, ALU.add, [lik, kfk], [yyk])
                        B.act(sn, yy, AF.Sin, [yyk], [snk])
                        if which == 1:
                            B.tt('vector', den, mag, sn, ALU.mult, [magk, snk], [denk])
                        else:
                            B.tt('vector', tq, mag, sn, ALU.mult, [magk, snk], [tqk])
                    g2v = lambda a: a.rearrange("p (g b) -> p g b", b=2)
                    reb = tq.unsqueeze(2).to_broadcast([128, 32, 2]); imb = den.unsqueeze(2).to_broadcast([128, 32, 2])
                    for r_ in range(2):
                        B.cp('vector', g2v(CA5[:, d, i8, r_, :]), reb, [tqk], [CAk])
                    B.cp('vector', g2v(CBp4[:, d, i8, :]), imb, [denk], [CBpk])
                    B.ts('vector', g2v(CBn4[:, d, i8, :]), imb, -1.0, None, ALU.mult, None, [denk], [CBnk])
                nr, nrk = sm[0]; qr, qrk = sm[1]; qi, qik = sm[2]; ta, tak = sm[3]; tb, tbk = sm[4]
                B.ts('vector', nr, LP4[:, 1, 0, :], -1.0, None, ALU.add, None, [LPk], [nrk])
                ni = LP4[:, 1, 1, :]
                B.tt('vector', den, Are, Are, ALU.mult, [Arek], [denk])
                B.tt('vector', ta, Aim, Aim, ALU.mult, [Aimk], [tak])
                B.tt('vector', den, den, ta, ALU.add, [denk, tak], [denk])
                B.P.op('vector', lambda e: e.reciprocal(out=den, in_=den), [denk], [denk])
                B.tt('vector', ta, nr, Are, ALU.mult, [nrk, Arek], [tak])
                B.tt('vector', tb, ni, Aim, ALU.mult, [LPk, Aimk], [tbk])
                B.tt('vector', ta, ta, tb, ALU.add, [tak, tbk], [tak])
                B.tt('vector', qr, ta, den, ALU.mult, [tak, denk], [qrk])
                B.tt('vector', ta, ni, Are, ALU.mult, [LPk, Arek], [tak])
                B.tt('vector', tb, nr, Aim, ALU.mult, [nrk, Aimk], [tbk])
                B.tt('vector', ta, ta, tb, ALU.subtract, [tak, tbk], [tak])
                B.tt('vector', qi, ta, den, ALU.mult, [tak, denk], [qik])

                def cmul(outr, outrk, outi, outik, ar_, ark, ai_, aik, br_, brk, bi_, bik):
                    B.tt('vector', v3(t1), v3(ar_), br_, ALU.mult, [ark, brk], [t1k])
                    B.tt('gpsimd', v3(t2), v3(ai_), bi_, ALU.mult, [aik, bik], [t2k])
                    B.tt('vector', outr, t1, t2, ALU.subtract, [t1k, t2k], [outrk])
                    B.tt('vector', v3(t1), v3(ar_), bi_, ALU.mult, [ark, bik, outrk], [t1k])
                    B.tt('gpsimd', v3(t2), v3(ai_), br_, ALU.mult, [aik, brk, outrk], [t2k])
                    B.tt('vector', outi, t1, t2, ALU.add, [t1k, t2k], [outik])
                cmul(bbr, bbrk, bbi, bbik, Bre, Brek, Bim, Bimk, bc3(qr), qrk, bc3(qi), qik)
                for h in range(2):
                    B.ts('vector', v4(Cmr)[:, :, h, :], v3(Cre), g2m[:, h:h + 1], None, ALU.mult, None, [Crek, g2mk], [Cmrk])
                    B.ts('vector', v4(Cmi)[:, :, h, :], v3(Cim), g2m[:, h:h + 1], -1.0, ALU.mult, ALU.mult, [Cimk, g2mk], [Cmik])
                for e in range(9):
                    Lr_b = bc3(LP4[:, e, 0, :]); Li_b = bc3(LP4[:, e, 1, :])
                    if e <= 7:
                        cmul(Ere, Erek, Eim, Eimk, bbr, bbrk, bbi, bbik, Lr_b, LPk, Li_b, LPk)
                        for h in range(2):
                            B.ts('vector', v4(Emr)[:, :, h, :], v3(Ere), g2m[:, h:h + 1], None, ALU.mult, None, [Erek, g2mk], [Emrk])
                            B.ts('gpsimd', v4(Emi)[:, :, h, :], v3(Eim), g2m[:, h:h + 1], None, ALU.mult, None, [Eimk, g2mk], [Emik])
                        s_idx = (7 - e) if d == 0 else e
                        for j in range(8):
                            sl = slice(j * 128, (j + 1) * 128)
                            for ri, (Em, Emk) in enumerate(((Emr, Emrk), (Emi, Emik))):
                                ps, psk = B.bank()
                                B.tr(ps[:, 0:128], Em[:, sl], [Emk], [psk])
                                B.cp('scalar', Bsb5[:, j, s_idx, ri, :], ps[:, 0:128], [psk], [Bsbk])
                            ps, psk = B.bank()
                            B.mm(ps[:, 0:128], Emr[:, sl], Cmr[:, sl], True, False, [Emrk, Cmrk], [psk])
                            B.mm(ps[:, 0:128], Emi[:, sl], Cmi[:, sl], False, True, [Emik, Cmik], [psk])
                            B.tt('vector', Wsb4[:, j, e, :], ps[:, 0:128], blk16, ALU.mult, [psk, blk16k], [Wsbk])
                            if d == 0 and e == 0:
                                B.stt('vector', Wsb4[:, j, 0, :], B.ident, dcol[:, j:j + 1], Wsb4[:, j, 0, :], ALU.mult, ALU.add, [B.identk, dcolk, Wsbk], [Wsbk])
                    if e >= 1:
                        cmul(Ere, Erek, Eim, Eimk, Cre, Crek, Cim, Cimk, Lr_b, LPk, Li_b, LPk)
                        t_idx = (e - 1) if d == 0 else (8 - e)
                        for h in range(2):
                            B.ts('vector', Csb5[:, t_idx, 0, :, h * 16:(h + 1) * 16], v3(Ere), g2m[:, h:h + 1], None, ALU.mult, None, [Erek, g2mk], [Csbk])
                            B.ts('vector', Csb5[:, t_idx, 1, :, h * 16:(h + 1) * 16], v3(Eim), g2m[:, h:h + 1], -1.0, ALU.mult, ALU.mult, [Eimk, g2mk], [Csbk])
                B.dma('sync', Wblk_d[d], Wsb4, [Wsbk], [('Wblk', d)])
                B.dma('sync', Bst_d[d], Bsb5, [Bsbk], [('Bst', d)])
                B.dma('sync', Cst_d[d], Csb5, [Csbk], [('Cst', d)])

        def phase_s5_main():
            B.phase()
            NBUF = 2
            hjs = [T("hj%d" % z, NB * 2304, BF16) for z in range(NBUF)]
            Wjs = [T("Wj%d" % z, 2 * 8 * 128, BF16) for z in range(NBUF)]
            Bjs = [T("Bj%d" % z, 2 * 8 * 2 * 128, BF16) for z in range(NBUF)]
            Cjs = [T("Cj%d" % z, 2 * 8 * 2 * 4 * 32, BF16) for z in range(NBUF)]
            Bjzs = [T("Bjz%d" % z, 2 * 8 * 2 * 128, BF16) for z in range(NBUF)]
            Cjzs = [T("Cjz%d" % z, 2 * 8 * 2 * 64, BF16) for z in range(NBUF)]
            Hbs = [[T("Hb%d_%d" % (z, d), 16 * 288, BF16) for d in range(2)] for z in range(NBUF)]
            Hl = [T("Hl%d" % d, 16 * 288) for d in range(2)]
            v5 = lambda a_, k: a_.rearrange("p (r q b k) -> p r q b k", r=2, q=4, b=2, k=k)
            Sa = [T("Sa%d" % d, 576) for d in range(2)]; Si = [T("Si%d" % d, 576) for d in range(2)]
            tA = [T("tA%d" % d, 576) for d in range(2)]; tB = [T("tB%d" % d, 576) for d in range(2)]
            vA = lambda a_: a_.rearrange("p (r g k i) -> p r g k i", r=2, g=8, i=8)
            vS = lambda a_: a_.rearrange("p (r g k) -> p r g k", r=2, g=8)
            ysb, ysbk = T("ysb", NB * 2304, BF16)
            ysb3 = ysb.rearrange("p (b t) -> p b t", b=NB); ysb4 = ysb.rearrange("p (b k s) -> p b k s", b=NB, s=8)
            yas = [(T("ya%d" % z, 288), T("yb%d" % z, 288)) for z in range(2)]
            engs = ['vector', 'gpsimd']
            hkeys = [('hT', i) for i in range(NT)]

            def views(j):
                z = j % NBUF
                hj, hjk = hjs[z]; Wj, Wjk = Wjs[z]; Bj, Bjk = Bjs[z]; Cj, Cjk = Cjs[z]; Bjz, Bjzk = Bjzs[z]; Cjz, Cjzk = Cjzs[z]
                return dict(hj=hj, hjk=hjk, hj3=hj.rearrange("p (b t) -> p b t", b=NB), hj4=hj.rearrange("p (b k s) -> p b k s", b=NB, s=8),
                            Wj4=Wj.rearrange("p (d e m) -> p d e m", d=2, e=8), Wjk=Wjk,
                            Bj=Bj, Bj5=Bj.rearrange("p (d s r m) -> p d s r m", d=2, s=8, r=2), Bjk=Bjk,
                            Cj6=Cj.rearrange("p (d t r q m) -> p d t r q m", d=2, t=8, r=2, q=4), Cjk=Cjk,
                            Bjz=Bjz, Bjz5=Bjz.rearrange("p (d s r m) -> p d s r m", d=2, s=8, r=2), Bjzk=Bjzk,
                            Cjz=Cjz, Cjz5=Cjz.rearrange("p (d t r m) -> p d t r m", d=2, t=8, r=2), Cjzk=Cjzk, Hb=Hbs[z])

            def load(j):
                v = views(j); rows = slice(j * 128, (j + 1) * 128)
                for b in range(NB):
                    B.dma('gpsimd', v['hj3'][:, b, 0:NCX], hT[rows, TL + b * NCX:TL + (b + 1) * NCX], hkeys, [v['hjk']])
                    B.dma('gpsimd', v['hj3'][:, b, NCX:2304], hT[rows, b * NL:(b + 1) * NL], hkeys, [v['hjk']])
                for d in range(2):
                    B.dma('sync', v['Wj4'][:, d], Wblk_d[d, :, j], [('Wblk', d)], [v['Wjk']])
                    B.dma('sync', v['Bj5'][:, d], Bst_d[d, :, j], [('Bst', d)], [v['Bjk']])
                    B.dma('sync', v['Cj6'][:, d], Cst_d[d, :, :, :, 4 * j:4 * j + 4, :], [('Cst', d)], [v['Cjk']])
                B.cp('vector', v['Bjz'][64:128, :], v['Bj'][64:128, :], [v['Bjk']], [v['Bjzk']])
                B.ms('vector', v['Bjz'][64:96, :], 0.0, [v['Bjzk']])
                B.ms('gpsimd', v['Cjz'], 0.0, [v['Cjzk']])
                for d in range(2):
                    B.cp('gpsimd', v['Cjz5'][:, d, :, :, 32:64], v['Cj6'][:, d, :, :, 3, :], [v['Cjk'], v['Cjzk']], [v['Cjzk']])

            def state(j):
                v = views(j)
                for d in range(2):
                    Hl5 = v5(Hl[d][0], 288)
                    for ri in range(2):
                        for q in range(4):
                            for b in range(NB):
                                ps, psk = B.bank()
                                for s_ in range(8):
                                    if q < 3:
                                        B.mm(ps[:, 0:288], v['Bj5'][32 * q:32 * q + 32, d, s_, ri, :], v['hj4'][32 * q:32 * q + 32, b, :, s_], s_ == 0, s_ == 7, [v['Bjk'], v['hjk']], [psk])
                                    else:
                                        B.mm(ps[:, 0:288], v['Bjz5'][64:128, d, s_, ri, :], v['hj4'][64:128, b, :, s_], s_ == 0, s_ == 7, [v['Bjzk'], v['hjk']], [psk])
                                B.cp('scalar', Hl5[:, ri, q, b, :], ps[:, 0:288], [psk], [Hl[d][1]])

            def chain(j):
                v = views(j)

                def cm(d, src, i8, n):
                    eng = engs[d]
                    gsl = slice(8 * j, 8 * j + 8)
                    LAb = CA5[:, d, i8, :, gsl].unsqueeze(3).to_broadcast([128, 2, 8, n])
                    LNb = CBn4[:, d, i8, gsl].unsqueeze(2).to_broadcast([128, 8, n])
                    LPb = CBp4[:, d, i8, gsl].unsqueeze(2).to_broadcast([128, 8, n])
                    t1 = tA[d][0][:, 0:16 * n].rearrange("p (r g k) -> p r g k", r=2, g=8); t1k = tA[d][1]
                    t2 = tB[d][0][:, 0:16 * n].rearrange("p (r g k) -> p r g k", r=2, g=8); t2k = tB[d][1]
                    rk = [Hl[d][1], Sa[d][1], Si[d][1], CAk, CBnk, CBpk]
                    B.tt(eng, t1, src, LAb, ALU.mult, rk, [t1k])
                    B.tt(eng, t2[:, 0], src[:, 1], LNb, ALU.mult, rk, [t2k])
                    B.tt(eng, t2[:, 1], src[:, 0], LPb, ALU.mult, rk, [t2k])
                    B.tt(eng, t1, t1, t2, ALU.add, [t1k, t2k], [t1k])
                    return t1, t1k
                A5 = [vA(Hl[d][0]) for d in range(2)]; Ak = [Hl[d][1] for d in range(2)]
                S4 = [vS(Sa[d][0]) for d in range(2)]; Sk = [Sa[d][1] for d in range(2)]
                I4 = [vS(Si[d][0]) for d in range(2)]; Ik = [Si[d][1] for d in range(2)]
                for step in range(7):
                    for d in range(2):
                        i = step + 1 if d == 0 else 6 - step
                        prev = i - 1 if d == 0 else i + 1
                        t1, t1k = cm(d, A5[d][:, :, :, :, prev], 0, 36)
                        B.tt(engs[d], A5[d][:, :, :, :, i], A5[d][:, :, :, :, i], t1, ALU.add, [Ak[d], t1k], [Ak[d]])
                    yield
                seqs = [[(0, None)] + [(b_, b_ - 1) for b_ in range(1, 36)],
                        [(3, None), (2, 3), (1, 2), (0, 1), (35, 0)] + [(b_, b_ + 1) for b_ in range(34, 3, -1)]]
                endi = [7, 0]
                for step in range(36):
                    for d in range(2):
                        bd_, bs_ = seqs[d][step]
                        if bs_ is None:
                            B.cp(engs[d], S4[d][:, :, :, bd_:bd_ + 1], A5[d][:, :, :, bd_:bd_ + 1, endi[d]], [Ak[d]], [Sk[d]])
                        else:
                            t1, t1k = cm(d, S4[d][:, :, :, bs_:bs_ + 1], 7, 1)
                            B.tt(engs[d], S4[d][:, :, :, bd_:bd_ + 1], A5[d][:, :, :, bd_:bd_ + 1, endi[d]], t1, ALU.add, [Ak[d], t1k], [Sk[d]])
                    if step % 2 == 1:
                        yield
                B.ms(engs[0], I4[0][:, :, :, 0:1], 0.0, [Ik[0]])
                B.cp(engs[0], I4[0][:, :, :, 1:36], S4[0][:, :, :, 0:35], [Sk[0]], [Ik[0]])
                B.ms(engs[1], I4[1][:, :, :, 3:4], 0.0, [Ik[1]])
                B.cp(engs[1], I4[1][:, :, :, 0:3], S4[1][:, :, :, 1:4], [Sk[1]], [Ik[1]])
                B.cp(engs[1], I4[1][:, :, :, 4:35], S4[1][:, :, :, 5:36], [Sk[1]], [Ik[1]])
                B.cp(engs[1], I4[1][:, :, :, 35:36], S4[1][:, :, :, 0:1], [Sk[1]], [Ik[1]])
                yield
                for i in range(8):
                    for d in range(2):
                        pw = i if d == 0 else 7 - i
                        t1, t1k = cm(d, I4[d], pw, 36)
                        B.tt(engs[d], A5[d][:, :, :, :, i], A5[d][:, :, :, :, i], t1, ALU.add, [Ak[d], t1k], [Ak[d]])
                    yield
                vK = lambda a_: a_.rearrange("p (r g k) -> p r g k", r=2, g=8)
                Af = vK(Hl[0][0]); Ab = vK(Hl[1][0]); Hbf = vK(v['Hb'][0][0]); Hbb = vK(v['Hb'][1][0])
                hk0 = v['Hb'][0][1]; hk1 = v['Hb'][1][1]
                B.ms('vector', Hbf[:, :, :, 0:1], 0.0, [hk0])
                B.cp('scalar', Hbf[:, :, :, 1:288], Af[:, :, :, 0:287], [Hl[0][1]], [hk0])
                B.cp('scalar', Hbb[:, :, :, 0:287], Ab[:, :, :, 1:288], [Hl[1][1]], [hk1])
                B.ms('vector', Hbb[:, :, :, 31:32], 0.0, [hk1])
                B.cp('vector', Hbb[:, :, :, 287:288], Ab[:, :, :, 0:1], [Hl[1][1]], [hk1])
                yield

            def outp(j, gen):
                v = views(j); rows = slice(j * 128, (j + 1) * 128)
                gi = 0
                for t in range(8):
                    for b in range(NB):
                        ps, psk = B.bank()
                        first = True
                        for s_ in range(0, t + 1):
                            B.mm(ps[:, 0:288], v['Wj4'][:, 0, t - s_, :], v['hj4'][:, b, :, s_], first, False, [v['Wjk'], v['hjk']], [psk]); first = False
                        for s_ in range(t, 8):
                            B.mm(ps[:, 0:288], v['Wj4'][:, 1, s_ - t, :], v['hj4'][:, b, :, s_], False, False, [v['Wjk'], v['hjk']], [psk])
                        cnt_ = 0
                        for d in range(2):
                            Hb5 = v5(v['Hb'][d][0], 288)
                            for ri in range(2):
                                for q in range(4):
                                    cnt_ += 1
                                    if q < 3:
                                        B.mm(ps[32 * q:32 * q + 32, 0:288], v['Cj6'][:, d, t, ri, q, :], Hb5[:, ri, q, b, 0:288], False, cnt_ == 16, [v['Cjk'], v['Hb'][d][1]], [psk])
                                    else:
                                        B.mm(ps[64:128, 0:288], v['Cjz5'][:, d, t, ri, :], Hb5[:, ri, q, b, 0:288], False, cnt_ == 16, [v['Cjzk'], v['Hb'][d][1]], [psk])
                        (ya, yak), (yb, ybk) = yas[gi % 2]; gi += 1
                        B.cp('scalar', ya, ps[:, 0:288], [psk], [yak])
                        B.act(yb, ya, AF.Square, [yak], [ybk])
                        B.ts('vector', yb, yb, 0.044715, 1.0, ALU.mult, ALU.add, [ybk], [ybk])
                        B.tt('vector', yb, yb, ya, ALU.mult, [ybk, yak], [ybk])
                        B.act(yb, yb, AF.Sigmoid, [ybk], [ybk], scale=1.5957691216057308)
                        B.tt('vector', ysb4[:, b, :, t], ya, yb, ALU.mult, [yak, ybk], [ysbk])
                        if gen is not None:
                            for _ in range(5):
                                next(gen, None)
                if gen is not None:
                    for _ in gen:
                        pass
                for b in range(NB):
                    B.dma('sync', sT[rows, TL + b * NCX:TL + (b + 1) * NCX], ysb3[:, b, 0:NCX], [ysbk], [('sT', j)])
                    B.dma('sync', sT[rows, b * NL:(b + 1) * NL], ysb3[:, b, NCX:2304], [ysbk], [('sT', j)])

            load(0); state(0)
            for _ in chain(0):
                pass
            load(1)
            for j in range(8):
                gen = None
                if j + 1 < 8:
                    state(j + 1)
                    gen = chain(j + 1)
                outp(j, gen)
                if j + 2 < 8:
                    load(j + 2)

        def phase_glu():
            B.phase()
            wg, wgk = T("wglu", 8 * 2048, BF16); wg3 = wg.rearrange("p (k n) -> p k n", n=2048)
            for k in range(8):
                B.dma('gpsimd', wg3[:, k, :], s5_w_glu[0, k * 128:(k + 1) * 128, :], [], [wgk])
            bg, bgk = T("bglu", 2048)
            B.dma('sync', bg, s5_b_glu[0:1, :].partition_broadcast(128), [], [bgk])
            gts = load_gate_tiles(0, 2 * D)
            bufs = [(T("yt%d" % i, 1024, BF16), T("xt%d" % i, D), T("xn%d" % i, D), T("at%d" % i, 512), T("gt%d" % i, 512)) for i in range(3)]
            skeys = [('sT', j) for j in range(8)]
            def glu_gen(i):
                (yt, ytk), (xt, xk), (xn, xnk), (a_t, atk), (g_t, gtk) = bufs[i % 3]
                yt3 = yt.rearrange("p (k t) -> p k t", t=128)
                B.dma('scalar', yt3, sT3[:, :, i * 128:(i + 1) * 128], skeys, [ytk])
                B.dma('sync', xt, xa[i * 128:(i + 1) * 128, :], [xkeys[i]], [xk])
                yield
                pss = [B.bank() for n in range(4)]
                for n in range(4):
                    for k in range(8):
                        B.mm(pss[n][0], yt3[:, k, :], wg3[:, k, n * 512:(n + 1) * 512], k == 0, k == 7, [ytk, wgk], [pss[n][1]])
                yield
                GT, GTk = gts[tile_row(i)]
                for hh in range(2):
                    cs = slice(hh * 512, (hh + 1) * 512)
                    B.tt('vector', a_t, pss[hh][0], bg[:, cs], ALU.add, [pss[hh][1], bgk], [atk])
                    B.tt('vector', g_t, pss[2 + hh][0], bg[:, 1024 + hh * 512:1024 + (hh + 1) * 512], ALU.add, [pss[2 + hh][1], bgk], [gtk])
                    B.act(g_t, g_t, AF.Sigmoid, [gtk], [gtk])
                    B.tt('gpsimd', a_t, a_t, g_t, ALU.mult, [atk, gtk], [atk])
                    B.tt('gpsimd', a_t, a_t, GT[:, cs], ALU.mult, [atk, GTk], [atk])
                    B.tt('gpsimd', xn[:, cs], a_t, xt[:, cs], ALU.add, [atk, xk], [xnk])
                B.dma('sync', xa[i * 128:(i + 1) * 128, :], xn, [xnk], [xkeys[i]])
            run_pipeline([glu_gen(i) for i in range(NT)])


        def phase_moe(l, ntiles, final):
            B.phase()
            mts = load_mod_tiles(l, g_ffn, 3 * D, 4 * D)
            wr, wrk = T("wr", 8 * 32); wr3 = wr.rearrange("p (k e) -> p k e", e=32)
            B.dma('sync', wr3, moe_w_router[l].rearrange("(k p) e -> p k e", p=128), [], [wrk])
            brb, brbk = T("brb", 32); B.dma('sync', brb, moe_b_router[l:l + 1, :].partition_broadcast(128), [], [brbk])
            slot0, slot0k = T("slot0", 32); B.dma('sync', slot0, k_slot, [], [slot0k])
            base, basek = T("base", 32); B.ms('vector', base, 0.0, [basek])
            NRB = 4
            bufs = [(T("xt%d" % i, D), T("h%d" % i, D), T("hTt%d" % i, D), T("ss%d" % i, 2)) for i in range(NRB)]
            smalls = [(T("lg%d" % z, 32), T("top8%d" % z, 8), T("mask%d" % z, 32), T("sl%d" % z, 32),
                       T("oh%d" % z, 32 * 4), T("nb%d" % z, 2), T("ex%d" % z, 4), T("destf%d" % z, 4)) for z in range(NRB)]

            def router_gen(i):
                (lg, lgk), (top8, top8k), (mask, maskk), (sl, slk), (oh4, ohk), (nb, nbk), (ex, exk), (destf, destfk) = smalls[i % NRB]
                (xt, xk), (h, hk), (ht, htk), (ss, ssk) = bufs[i % NRB]
                B.dma('sync', xt, xa[i * 128:(i + 1) * 128, :], [xkeys[i]], [xk])
                g_ = norm_tile_g(xt, xk, h, hk, mts[tile_row(i)], ss, ssk)
                next(g_)
                yield
                next(g_); next(g_)
                ht3 = ht.rearrange("p (k t) -> p k t", t=128)
                transpose_tile(h, hk, ht3, htk)
                yield
                ps, psk = B.bank()
                for k in range(8):
                    B.mm(ps[:, 0:32], ht3[:, k, :], wr3[:, k, :], k == 0, k == 7, [htk, wrk], [psk])
                B.tt('vector', lg, ps[:, 0:32], brb, ALU.add, [psk, brbk], [lgk])
                B.P.op('vector', lambda e: e.max(out=top8, in_=lg), [lgk], [top8k])
                B.ts('vector', mask, lg, top8[:, 3:4], None, ALU.is_ge, None, [lgk, top8k], [maskk])
                B.ts('vector', nb[:, 0:1], top8[:, 0:1], -1.0, None, ALU.mult, None, [top8k], [nbk])
                B.ms('vector', nb[:, 1:2], 0.0, [nbk])
                B.act(ex, top8[:, 0:4], AF.Exp, [top8k, nbk], [exk, nbk], bias=nb[:, 0:1], scale=1.0, accum_out=nb[:, 1:2])
                ps2, ps2k = B.bank()
                B.mm(ps2[:, 0:32], ltri, mask, True, True, [ltrik, maskk], [ps2k])
                ps3, ps3k = B.bank()
                B.mm(ps3[:, 0:32], ones, mask, True, True, [onesk, maskk], [ps3k])
                yield
                B.P.op('vector', lambda e: e.reciprocal(out=nb[:, 1:2], in_=nb[:, 1:2]), [nbk], [nbk])
                B.ts('vector', GATE3[:, i, :], ex, nb[:, 1:2], None, ALU.mult, None, [exk, nbk], [GATEk])
                B.tt('vector', sl, ps2[:, 0:32], base, ALU.add, [ps2k, basek], [slk])
                B.ts('vector', sl, sl, float(CAP - 1), None, ALU.min, None, [slk], [slk])
                B.tt('vector', sl, sl, slot0, ALU.add, [slk, slot0k], [slk])
                B.tt('vector', base, base, ps3[:, 0:32], ALU.add, [basek, ps3k, slk], [basek])
                for k in range(4):
                    oh = oh4[:, 32 * k:32 * (k + 1)]
                    B.ts('vector', oh, lg, top8[:, k:k + 1], None, ALU.is_equal, None, [lgk, top8k], [ohk])
                    B.tt('vector', oh, oh, sl, ALU.mult, [ohk, slk], [ohk])
                for k in range(4):
                    B.P.op('vector', lambda e, k=k: e.reduce_sum(out=destf[:, k:k + 1], in_=oh4[:, 32 * k:32 * (k + 1)], axis=AX.X), [ohk], [destfk])
                B.cp('vector', DEST3[:, i, :], destf, [destfk], [DESTk])
                for k in range(4):
                    B.P.dma('gpsimd', lambda e, k=k: e.indirect_dma_start(out=Xs, out_offset=bass.IndirectOffsetOnAxis(ap=DEST3[:, i, k:k + 1], axis=0), in_=h, in_offset=None), [hk, DESTk], ['Xs'])
            run_pipeline([router_gen(i) for i in range(ntiles)])
            B.dma('sync', cnt_out[:, 32 * l:32 * (l + 1)], base, [basek], [('cnt', l)])
            B.phase()
            wgu = [T("wgu%d" % i, 8 * 2048, BF16) for i in range(2)]
            wd = [T("wd%d" % i, 8 * 1024, BF16) for i in range(2)]
            bgu = [T("bgu%d" % i, 16) for i in range(2)]; bdb = [T("bdb%d" % i, D) for i in range(2)]
            xTs = [T("xT%d" % i, 8 * CAP, BF16) for i in range(2)]
            aT, aTk = T("aT", 8 * CAP, BF16); aT3 = aT.rearrange("p (k t) -> p k t", t=CAP)
            xs = [T("xs%d" % i, D, BF16) for i in range(CAPT)]; yo = [T("yo%d" % i, D) for i in range(2)]
            ep = [(T("g_t%d" % i, HALF), T("u_t%d" % i, HALF), T("s_t%d" % i, HALF)) for i in range(2)]
            cnt = 0

            def load_w(e):
                (wg, wgk) = wgu[e % 2]; (wdd, wdk) = wd[e % 2]; (bg, bgk) = bgu[e % 2]; (bd, bdk) = bdb[e % 2]
                wg3 = wg.rearrange("p (k n) -> p k n", n=2048); wd3 = wdd.rearrange("p (k n) -> p k n", n=1024)
                for k4 in range(2):
                    B.dma('gpsimd', wg3[:, 4 * k4:4 * k4 + 4, :], moe_w_gu[l, e, 512 * k4:512 * (k4 + 1), :].rearrange("(k p) n -> p k n", p=128), [], [wgk])
                B.dma('gpsimd', wd3, moe_w_down[l, e].rearrange("(k p) n -> p k n", p=128), [], [wdk])
                B.dma('scalar', bg, moe_b_gu[l, e].rearrange("(c p) -> p c", p=128), [], [bgk], nc_ok=True)
                B.dma('scalar', bd, moe_b_down[l, e:e + 1, :].partition_broadcast(128), [], [bdk])

            def load_x(e):
                for stl in range(CAPT):
                    r0 = e * CAP + stl * 128
                    B.dma('sync', xs[stl][0], Xs[r0:r0 + 128, :], ['Xs'], [xs[stl][1]])

            def transposes(e):
                xT, xTk = xTs[e % 2]; xT3 = xT.rearrange("p (k t) -> p k t", t=CAP)
                for stl in range(CAPT):
                    (x_, x_k) = xs[stl]
                    ps, psk = B.bank()
                    psb = ps.bitcast(BF16)
                    for k in range(8):
                        B.P.op('tensor', lambda e, o=psb[:, k * 128:(k + 1) * 128], i_=x_[:, k * 128:(k + 1) * 128]: e.transpose(o, i_, identb), [x_k, identbk], [psk])
                    B.cp('scalar' if stl % 2 == 0 else 'vector', xT3[:, :, stl * 128:(stl + 1) * 128], psb.rearrange("p (k t) -> p k t", t=128), [psk], [xTk])
            load_w(0); load_x(0); transposes(0); load_x(1)
            for e in range(32):
                (wg, wgk) = wgu[e % 2]; (wdd, wdk) = wd[e % 2]; (bg, bgk) = bgu[e % 2]; (bd, bdk) = bdb[e % 2]
                wg3 = wg.rearrange("p (k n) -> p k n", n=2048); wd3 = wdd.rearrange("p (k n) -> p k n", n=1024)
                xT, xTk = xTs[e % 2]; xT3 = xT.rearrange("p (k t) -> p k t", t=CAP)
                if e + 1 < 32:
                    load_w(e + 1)
                for fc in range(8):
                    for hf in range(2):
                        cols = slice(hf * HALF, (hf + 1) * HALF)
                        (g_t, gk_), (u_t, uk_), (s_t, sk_) = ep[cnt % 2]; cnt += 1
                        psg, psgk = B.bank(); psu, psuk = B.bank()
                        for k in range(8):
                            B.mm(psg[:, 0:HALF], wg3[:, k, fc * 128:(fc + 1) * 128], xT3[:, k, cols], k == 0, k == 7, [wgk, xTk], [psgk])
                        for k in range(8):
                            B.mm(psu[:, 0:HALF], wg3[:, k, 1024 + fc * 128:1024 + (fc + 1) * 128], xT3[:, k, cols], k == 0, k == 7, [wgk, xTk], [psuk])
                        B.ts('vector', g_t, psg[:, 0:HALF], bg[:, fc:fc + 1], 7.0, ALU.add, ALU.min, [psgk, bgk], [gk_])
                        B.ts('vector', u_t, psu[:, 0:HALF], bg[:, 8 + fc:9 + fc], 7.0, ALU.add, ALU.min, [psuk, bgk], [uk_])
                        B.ts('vector', u_t, u_t, -7.0, 1.0, ALU.max, ALU.add, [uk_], [uk_])
                        B.act(s_t, g_t, AF.Sigmoid, [gk_], [sk_], scale=1.702)
                        B.tt('gpsimd', g_t, g_t, s_t, ALU.mult, [gk_, sk_], [gk_])
                        B.tt('vector', aT3[:, fc, cols], g_t, u_t, ALU.mult, [gk_, uk_], [aTk])
                if e + 1 < 32:
                    transposes(e + 1)
                    if e + 2 < 32:
                        load_x(e + 2)
                for stl in range(CAPT):
                    (y_, y_k) = yo[stl % 2]
                    for nh in range(2):
                        ps, psk = B.bank()
                        for k in range(8):
                            B.mm(ps, aT3[:, k, stl * 128:(stl + 1) * 128], wd3[:, k, nh * 512:(nh + 1) * 512], k == 0, k == 7, [aTk, wdk], [psk])
                        B.tt('vector', y_[:, nh * 512:(nh + 1) * 512], ps, bd[:, nh * 512:(nh + 1) * 512], ALU.add, [psk, bdk], [y_k])
                    r0 = e * CAP + stl * 128
                    B.dma('sync', Ys[r0:r0 + 128, :], y_, [y_k], ['Ys'])
            B.phase()
            gts = load_gate_tiles(l, 5 * D)
            bufs = [([T("cg%d_%d" % (i, k), D) for k in range(4)], T("acc%d" % i, D), T("cx%d" % i, D)) for i in range(3)]
            def comb_gen(i):
                gl, (acc, acck), (xt, xk) = bufs[i % 3]
                B.dma('sync', xt, xa[i * 128:(i + 1) * 128, :], [xkeys[i]], [xk])
                for k in range(4):
                    B.P.dma('gpsimd', lambda e, i=i, k=k, g=gl[k][0]: e.indirect_dma_start(out=g, out_offset=None, in_=Ys, in_offset=bass.IndirectOffsetOnAxis(ap=DEST3[:, i, k:k + 1], axis=0)), ['Ys', DESTk], [gl[k][1]])
                yield
                yield
                B.ts('vector', acc, gl[0][0], GATE3[:, i, 0:1], None, ALU.mult, None, [gl[0][1], GATEk], [acck])
                for k in range(1, 4):
                    B.stt('vector', acc, gl[k][0], GATE3[:, i, k:k + 1], acc, ALU.mult, ALU.add, [gl[k][1], GATEk, acck], [acck])
                GT, GTk = gts[tile_row(i)]
                B.tt('gpsimd', acc, acc, GT, ALU.mult, [acck, GTk], [acck])
                B.tt('gpsimd', acc, acc, xt, ALU.add, [acck, xk], [acck])
                if final:
                    B.dma('sync', outf[i * 128:(i + 1) * 128, :], acc, [acck], [('out', i)])
                else:
                    B.dma('sync', xa[i * 128:(i + 1) * 128, :], acc, [acck], [xkeys[i]])
            run_pipeline([comb_gen(i) for i in range(ntiles)])

        def phase_attn():
            B.phase()
            B.brange = (0, 3)
            blk64, blk64k = T("blk64", 128); B.dma('sync', blk64, k_blk64, [], [blk64k])
            rot, rotk = T("rot", 128); B.dma('sync', rot, k_rot, [], [rotk])
            cs, csk = T("cos", NL); B.dma('sync', cs, k_cos, [], [csk])
            sn, snk = T("sin", NL); B.dma('scalar', sn, k_sin, [], [snk])
            gc, gck = T("gcols", 8)
            for c in range(2):
                B.dma('sync', gc[c * 64:(c + 1) * 64, 0:1], da_q_gain[0:1, :].rearrange("o d -> d o"), [], [gck], nc_ok=True)
                B.dma('sync', gc[c * 64:(c + 1) * 64, 1:2], da_k_gain[0:1, :].rearrange("o d -> d o"), [], [gck], nc_ok=True)
            B.dma('sync', gc[:, 2:3], da_sub_gain[0:1, :].rearrange("o d -> d o"), [], [gck], nc_ok=True)
            B.ts('vector', gc[:, 2:3], gc[:, 2:3], 1.0 - LAMBDA_INIT, None, ALU.mult, None, [gck], [gck])
            lt = [T("lamt%d" % i, 64) for i in range(4)]
            for i in range(4):
                B.dma('sync', lt[i][0], da_lam[i][0:1, :].partition_broadcast(128), [], [lt[i][1]])
            for pr in range(2):
                a_, ak_ = lt[2 * pr]; b_, bk_ = lt[2 * pr + 1]
                B.tt('vector', a_, a_, b_, ALU.mult, [ak_, bk_], [ak_])
                B.P.op('vector', lambda e, a_=a_, pr=pr: e.reduce_sum(out=gc[:, 5 + pr:6 + pr], in_=a_, axis=AX.X), [ak_], [gck])
            B.act(gc[:, 5:7], gc[:, 5:7], AF.Exp, [gck], [gck])
            B.tt('vector', gc[:, 3:4], gc[:, 5:6], gc[:, 6:7], ALU.subtract, [gck], [gck])
            B.ts('vector', gc[:, 4:5], gc[:, 3:4], LAMBDA_INIT, -1.0, ALU.add, ALU.mult, [gck], [gck])
            wo, wok = T("wo", 8 * 1024, BF16); wo3 = wo.rearrange("p (k n) -> p k n", n=1024)
            B.dma('gpsimd', wo3, da_w_o[0].rearrange("(k p) n -> p k n", p=128), [], [wok])
            gts = load_gate_tiles(1, 2 * D)
            hb, hbk = T("hb", 8 * 2304, BF16); hb3 = hb.rearrange("p (k t) -> p k t", t=2304)
            onT, onTk = T("onT", 8 * NL, BF16); onT3 = onT.rearrange("p (h t) -> p h t", t=NL)
            HW = []
            for hp in range(2):
                wq, wqk = T("wq%d" % hp, 1024, BF16); wk_, wkk = T("wk%d" % hp, 1024, BF16); wv, wvk = T("wv%d" % hp, 1024, BF16)
                qn, qnk = T("qn%d" % hp, NL, BF16)
                kz = [T("kz%d_%d" % (hp, c), 2304, BF16) for c in range(2)]
                for c in range(2):
                    B.ms('vector' if c == 0 else 'gpsimd', kz[c][0], 0.0, [kz[c][1]])
                vv, vvk = T("vv%d" % hp, 18 * 128, BF16)
                HW.append(dict(wq3=wq.rearrange("p (k n) -> p k n", n=128), wqk=wqk, wk3=wk_.rearrange("p (k n) -> p k n", n=128), wkk=wkk,
                               wv3=wv.rearrange("p (k n) -> p k n", n=128), wvk=wvk, qn=qn, qnk=qnk, kz=kz,
                               vv3=vv.rearrange("p (t e) -> p t e", e=128), vvk=vvk))
            Es2 = [[T("Es%d_%d" % (p_, c), 512) for c in range(2)] for p_ in range(2)]
            Eb = [T("E%d" % i, 512, BF16) for i in range(6)]
            pn_bufs = [(T("qf%d" % z, 512), T("sq%d" % z, 512), T("rs%d" % z, 512), T("trp%d" % z, 512)) for z in range(2)]
            pn_cnt = [0]
            (sq, sqk), (rs, rsk) = pn_bufs[0][1], pn_bufs[0][2]
            c0, c0k = T("c0", 512); c1, c1k = T("c1", 512)
            xb = [(T("axt%d" % i, D), T("axn%d" % i, D)) for i in range(1)]
            hkeys = [('hT', i) for i in range(NT)]
            wqkv3 = da_w_qkv[0].rearrange("(k p) n -> p k n", p=128)
            print("attn arena cols used", B.ar.off)

            def proj_norm(w3, wkey, col0, ncols, gcol, rope0, dst, dstk):
                (qf, qfk), (sq, sqk), (rs, rsk), (tr_, trk) = pn_bufs[pn_cnt[0] % 2]; pn_cnt[0] += 1
                ps, psk = B.bankp('pj', [6, 7])
                for k in range(8):
                    B.mm(ps[:, 0:ncols], w3[:, k, :], hb3[:, k, col0:col0 + ncols], k == 0, k == 7, [wkey, hbk], [psk])
                B.cp('scalar', qf[:, 0:ncols], ps[:, 0:ncols], [psk], [qfk])
                B.act(sq[:, 0:ncols], qf[:, 0:ncols], AF.Square, [qfk], [sqk])
                ps2, ps2k = B.bankp('pj', [6, 7])
                B.mm(ps2[:, 0:ncols], blk64, sq[:, 0:ncols], True, True, [blk64k, sqk], [ps2k])
                B.ts('vector', rs[:, 0:ncols], ps2[:, 0:ncols], 1e-6, None, ALU.add, None, [ps2k], [rsk])
                B.act(rs[:, 0:ncols], rs[:, 0:ncols], AF.Ln, [rsk], [rsk])
                B.act(rs[:, 0:ncols], rs[:, 0:ncols], AF.Exp, [rsk], [rsk], scale=-0.5)
                B.stt('vector', qf[:, 0:ncols], qf[:, 0:ncols], gcol, rs[:, 0:ncols], ALU.mult, ALU.mult, [qfk, gck, rsk], [qfk])
                if rope0 is None:
                    if isinstance(dst, list):
                        for (d_ap, d_k, rs_) in dst:
                            B.cp('vector', d_ap[rs_], qf[rs_, 0:ncols], [qfk], [d_k])
                    else:
                        B.cp('vector', dst, qf[:, 0:ncols], [qfk], [dstk])
                else:
                    ps3, ps3k = B.bankp('pj', [6, 7])
                    B.mm(ps3[:, 0:ncols], rot, qf[:, 0:ncols], True, True, [rotk, qfk], [ps3k])
                    B.tt('vector', tr_[:, 0:ncols], ps3[:, 0:ncols], sn[:, rope0:rope0 + ncols], ALU.mult, [ps3k, snk], [trk])
                    B.tt('gpsimd', sq[:, 0:ncols], qf[:, 0:ncols], cs[:, rope0:rope0 + ncols], ALU.mult, [qfk, csk, sqk], [sqk])
                    if isinstance(dst, list):
                        for (d_ap, d_k, rs_) in dst:
                            B.tt('vector', d_ap[rs_], sq[rs_, 0:ncols], tr_[rs_, 0:ncols], ALU.add, [sqk, trk], [d_k])
                    else:
                        B.tt('vector', dst, sq[:, 0:ncols], tr_[:, 0:ncols], ALU.add, [sqk, trk], [dstk])

            ecnt = 0
            heads = [(b_, h_) for b_ in range(NB) for h_ in range(8)]

            def proj_gen(idx):
                b, h = heads[idx]; W = HW[idx % 2]
                if h == 0:
                    B.dma('gpsimd', hb3[:, :, 0:NCX], hT3[:, :, TL + b * NCX:TL + (b + 1) * NCX], hkeys, [hbk])
                    B.dma('gpsimd', hb3[:, :, NCX:2304], hT3[:, :, b * NL:(b + 1) * NL], hkeys, [hbk])
                B.dma('gpsimd', W['wq3'], wqkv3[:, :, h * 128:(h + 1) * 128], [], [W['wqk']])
                B.dma('gpsimd', W['wk3'], wqkv3[:, :, D + h * 128:D + (h + 1) * 128], [], [W['wkk']])
                B.dma('gpsimd', W['wv3'], wqkv3[:, :, 2 * D + h * 128:2 * D + (h + 1) * 128], [], [W['wvk']])
                yield
                for qc in range(4):
                    proj_norm(W['wq3'], W['wqk'], NCX + qc * 512, 512, gc[:, 0:1], qc * 512, W['qn'][:, qc * 512:(qc + 1) * 512], W['qnk'])
                    yield

                def kdst(c0_, c1_):
                    return [(W['kz'][c][0][:, c0_:c1_], W['kz'][c][1], slice(c * 64, (c + 1) * 64)) for c in range(2)]
                proj_norm(W['wk3'], W['wkk'], 0, NCX, gc[:, 1:2], None, kdst(0, NCX), None)
                yield
                for qc in range(4):
                    proj_norm(W['wk3'], W['wkk'], NCX + qc * 512, 512, gc[:, 1:2], qc * 512, kdst(NCX + qc * 512, NCX + (qc + 1) * 512), None)
                    yield
                for kt in range(18):
                    ps, psk = B.bankp('pj', [6, 7])
                    for k in range(8):
                        B.mm(ps[:, 0:128], hb3[:, k, kt * 128:(kt + 1) * 128], W['wv3'][:, k, :], k == 0, k == 7, [hbk, W['wvk']], [psk])
                    B.cp('scalar' if kt % 2 == 0 else 'vector', W['vv3'][:, kt, :], ps[:, 0:128], [psk], [W['vvk']])
                    if kt % 2 == 1:
                        yield

            g0 = proj_gen(0)
            for _ in g0:
                pass
            for idx, (b, h) in enumerate(heads):
                W = HW[idx % 2]
                qn, qnk, kz, vv3, vvk = W['qn'], W['qnk'], W['kz'], W['vv3'], W['vvk']
                nxt = proj_gen(idx + 1) if idx + 1 < len(heads) else None
                if True:
                    for qc in range(4):
                        qs = slice(qc * 512, (qc + 1) * 512)
                        items = [(c, kt) for kt in range(18) for c in range(2)]
                        sc = {}

                        def score(ii, qs=qs):
                            c, kt = items[ii]
                            ps, psk = B.bankp('sc', [0, 1, 2])
                            B.mm(ps, kz[c][0][:, kt * 128:(kt + 1) * 128], qn[:, qs], True, True, [kz[c][1], qnk], [psk])
                            sc[ii] = (ps, psk)
                        score(0); score(1)
                        for ii, (c, kt) in enumerate(items):
                            if ii + 2 < len(items):
                                score(ii + 2)
                            po, pok = pst[3 + c][:], 'ps%d' % (3 + c)
                            Es_, Esk_ = Es2[0][c]
                            ps, psk = sc.pop(ii)
                            E_, Ek_ = Eb[ecnt % 6]; ecnt += 1
                            B.act(E_, ps, AF.Exp, [psk], [Ek_], scale=0.125)
                            B.mm(po, vv3[:, kt, :], E_, kt == 0, kt == 17, [vvk, Ek_], [pok])
                            eng_ = 'vector' if c == 0 else 'gpsimd'
                            if kt == 0:
                                B.cp(eng_, Es_, E_, [Ek_], [Esk_])
                            else:
                                B.tt(eng_, Es_, Es_, E_, ALU.add, [Ek_, Esk_], [Esk_])
                            if nxt is not None and ii % 4 == 3:
                                next(nxt, None)
                        pd, pdk = pst[5][:], 'ps5'
                        for c, (cc, cck) in enumerate(((c0, c0k), (c1, c1k))):
                            po, pok = pst[3 + c][:], 'ps%d' % (3 + c)
                            Es_, Esk_ = Es2[0][c]
                            B.mm(pd, ones, Es_, True, True, [onesk, Esk_], [pdk])
                            B.act(cc, pd, AF.Ln, [pdk], [cck])
                            B.act(cc, cc, AF.Exp, [cck], [cck], scale=-1.0)
                            B.tt('vector', cc, cc, po, ALU.mult, [cck, pok], [cck])
                        B.stt('vector', c0, c1, gc[:, 4:5], c0, ALU.mult, ALU.add, [c1k, gck, c0k], [c0k])
                        B.act(sq, c0, AF.Square, [c0k], [sqk])
                        B.mm(pd, ones, sq, True, True, [onesk, sqk], [pdk])
                        B.ts('vector', rs, pd, 1.0 / 128, 1e-6, ALU.mult, ALU.add, [pdk], [rsk])
                        B.act(rs, rs, AF.Ln, [rsk], [rsk])
                        B.act(rs, rs, AF.Exp, [rsk], [rsk], scale=-0.5)
                        B.stt('vector', onT3[:, h, qs], c0, gc[:, 2:3], rs, ALU.mult, ALU.mult, [c0k, gck, rsk], [onTk])
                if nxt is not None:
                    for _ in nxt:
                        pass
                if h != 7:
                    continue
                GT, GTk = gts[b]
                for tt_ in range(16):
                    i = b * 16 + tt_
                    (xt, xk), (xn, xnk) = xb[0]
                    B.dma('sync', xt, xa[i * 128:(i + 1) * 128, :], [xkeys[i]], [xk])
                    for nh in range(2):
                        cs_ = slice(nh * 512, (nh + 1) * 512)
                        ps, psk = B.bank()
                        for h in range(8):
                            B.mm(ps, onT3[:, h, tt_ * 128:(tt_ + 1) * 128], wo3[:, h, cs_], h == 0, h == 7, [onTk, wok], [psk])
                        B.tt('vector', xn[:, cs_], ps, GT[:, cs_], ALU.mult, [psk, GTk], [xnk])
                        B.tt('gpsimd', xn[:, cs_], xn[:, cs_], xt[:, cs_], ALU.add, [xnk, xk], [xnk])
                    B.dma('sync', xa[i * 128:(i + 1) * 128, :], xn, [xnk], [xkeys[i]])
            B.brange = (0, 8)

        phase_norm_T(0)
        if done('norm0'):
            P.emit(); return nc
        phase_s5_setup()
        if done('s5setup'):
            P.emit(); return nc
        phase_s5_main()
        if done('s5main'):
            P.emit(); return nc
        phase_glu()
        if done('glu'):
            P.emit(); return nc
        B.persist = persist_small
        phase_moe(0, NT, False)
        if done('moe0'):
            P.emit(); return nc
        phase_norm_T(1)
        phase_attn()
        if done('attn'):
            P.emit(); return nc
        phase_moe(1, 32, True)
        P.emit()
        print("nops", P.nops, {e: len(P.ops[e]) for e in ENGS})
    return nc


def make_consts():
    i = np.arange(128)
    k = {}
    k["k_ident"] = np.eye(128, dtype=np.float32)
    k["k_ltri"] = (i[:, None] < i[None, :]).astype(np.float32)
    k["k_ones"] = np.ones((128, 128), np.float32)
    k["k_blk16"] = (i[:, None] // 16 == i[None, :] // 16).astype(np.float32)
    k["k_g2m"] = (i[:, None] // 64 == np.arange(2)[None, :]).astype(np.float32)
    k["k_blk64"] = (i[:, None] // 64 == i[None, :] // 64).astype(np.float32) / 64.0
    rot = np.zeros((128, 128), np.float32)
    for c in range(2):
        for d in range(64):
            if d < 32:
                rot[c * 64 + d + 32, c * 64 + d] = -1.0
            else:
                rot[c * 64 + d - 32, c * 64 + d] = 1.0
    k["k_rot"] = rot
    tok = np.arange(NL)
    row = (tok // 64).astype(np.float32); col = (tok % 64).astype(np.float32)
    inv = np.exp(-math.log(10000.0) * np.arange(16, dtype=np.float32) / 16).astype(np.float32)
    ang = np.concatenate([row[:, None] * inv, col[:, None] * inv], axis=-1).astype(np.float32)
    f = (i % 64) % 32
    k["k_cos"] = np.ascontiguousarray(np.cos(ang).astype(np.float32)[:, f].T)
    k["k_sin"] = np.ascontiguousarray(np.sin(ang).astype(np.float32)[:, f].T)
    k["k_pm"] = (((i[:, None] // 16) % 2) == np.arange(2)[None, :]).astype(np.float32)
    k["k_slot"] = np.tile((np.arange(32, dtype=np.float32) * CAP)[None, :], (128, 1))
    return k


_NC_CACHE = {}


def kernel(**inputs):
    inp = {k: np.ascontiguousarray(np.asarray(v, dtype=np.float32)) for k, v in inputs.items()}
    if "nc" not in _NC_CACHE:
        _NC_CACHE["nc"] = build_nc()
    nc = _NC_CACHE["nc"]
    consts = make_consts()
    shared = {k: v for k, v in inp.items() if k not in ("x", "c", "ctx", "c_ctx")}
    for n in ("q1", "k1", "q2", "k2"):
        pass
    in_maps = []
    for core in range(8):
        m = dict(shared)
        m.update(consts)
        m["x"] = inp["x"][2 * core:2 * core + 2]
        m["ctx"] = inp["ctx"][2 * core:2 * core + 2]
        m["c"] = inp["c"][2 * core:2 * core + 2]
        m["c_ctx"] = inp["c_ctx"].reshape(1, D)
        in_maps.append(m)
    res = run_bass_kernel_spmd(nc, in_maps, core_ids=list(range(8)))
    _NC_CACHE["cnt"] = [r["cnt_out"][0] for r in res.results]
    return np.concatenate([r["out"] for r in res.results], axis=0).astype(np.float32)
```

```python
import numpy as np
from contextlib import ExitStack
import concourse.bass as bass
import concourse.mybir as mybir

F32 = mybir.dt.float32
F32R = mybir.dt.float32r
I32 = mybir.dt.int32
U32 = mybir.dt.uint32
ALU = mybir.AluOpType
AF = mybir.ActivationFunctionType
AX = mybir.AxisListType

ENGS = ['tensor', 'vector', 'scalar', 'gpsimd', 'sync']
DMA_SLOTS = {'sync': 12, 'gpsimd': 8, 'scalar': 6}


class Prog:
    def __init__(self, nc):
        self.nc = nc
        self.ops = {e: [] for e in ENGS}
        self.cnt = {e: 0 for e in ENGS}
        self.seen = {e: {} for e in ENGS}
        self.lastw = {}
        self.readers = {}
        self.dma_next = {q: 0 for q in DMA_SLOTS}
        self.dma_uses = {}
        self.dma_ep = {}
        self.epoch = {e: 0 for e in ENGS}
        self.used = set()
        self.nops = 0

    def _deps(self, eng, reads, writes):
        deps = {}

        def add(t):
            if t is None:
                return
            s, v = t
            if deps.get(s, 0) < v:
                deps[s] = v
        for r in reads:
            add(self.lastw.get(r))
        for w in writes:
            add(self.lastw.get(w))
            for s, v in self.readers.get(w, {}).items():
                add((s, v))
        waits = []
        for s, v in deps.items():
            if eng == 'tensor' and s[0] == 'tensor':
                continue
            if self.seen[eng].get(s, 0) >= v:
                continue
            self.seen[eng][s] = v
            waits.append((s, v))
        return waits

    def _update(self, tk, reads, writes):
        s, v = tk
        for w in writes:
            self.lastw[w] = tk
            self.readers[w] = {}
        for r in reads:
            d = self.readers.setdefault(r, {})
            if d.get(s, 0) < v:
                d[s] = v

    def op(self, eng, fn, reads=(), writes=()):
        waits = self._deps(eng, reads, writes)
        self.cnt[eng] += 1
        if self.cnt[eng] > 12000:
            self.epoch[eng] += 1
            self.cnt[eng] = 1
        ek = (eng, self.epoch[eng])
        self.used.add(ek)
        tk = (ek, self.cnt[eng])
        self.ops[eng].append((waits, fn, (ek, 1)))
        self._update(tk, reads, writes)
        self.nops += 1

    def dma(self, q, fn, reads=(), writes=()):
        waits = self._deps(q, reads, writes)
        n = DMA_SLOTS[q]
        slot = self.dma_next[q]
        self.dma_next[q] = (slot + 1) % n
        ep = self.dma_ep.get((q, slot), 0)
        if self.dma_uses.get(('d', q, slot, ep), 0) >= 700:
            ep += 1
            self.dma_ep[(q, slot)] = ep
            old = ('d', q, slot, ep - 1)
            if self.seen[q].get(old, 0) < 16 * 700:
                self.seen[q][old] = 16 * 700
                waits.append((old, 16 * 700))
        key = ('d', q, slot, ep)
        uses = self.dma_uses.get(key, 0)
        if uses > 0 and self.seen[q].get(key, 0) < 16 * uses:
            self.seen[q][key] = 16 * uses
            waits.append((key, 16 * uses))
        self.dma_uses[key] = uses + 1
        tk = (key, 16 * (uses + 1))
        self.ops[q].append((waits, fn, (key, 16)))
        self._update(tk, reads, writes)
        self.nops += 1

    def barrier(self):
        latest = {}
        for e in ENGS:
            if self.cnt[e] > 0:
                latest[(e, self.epoch[e])] = self.cnt[e]
        for key, uses in self.dma_uses.items():
            latest[key] = 16 * uses
        for e in ENGS:
            waits = []
            for s, v in latest.items():
                if self.seen[e].get(s, 0) < v:
                    self.seen[e][s] = v
                    waits.append((s, v))
            if waits:
                self.ops[e].append((waits, None, None))
        self.lastw.clear()
        self.readers.clear()

    def emit(self):
        nc = self.nc
        self.barrier()
        keys = sorted(self.used) + list(self.dma_uses.keys())
        with ExitStack() as st:
            semh = {}
            for i, k in enumerate(keys):
                semh[k] = st.enter_context(nc.semaphore("s%d" % i))
            block = st.enter_context(nc.Block())
            for e in ENGS:
                def body(engobj, e=e):
                    for waits, fn, inc in self.ops[e]:
                        for s, v in waits:
                            engobj.wait_ge(semh[s], v)
                        if fn is not None:
                            ins = fn(engobj)
                            ins.then_inc(semh[inc[0]], inc[1])
                getattr(block, e)(body)


class Arena:
    def __init__(self, big, ncols):
        self.big = big
        self.ncols = ncols
        self.off = 0
        self.gen = 0

    def reset(self):
        self.off = 0
        self.gen += 1

    def alloc(self, name, cols):
        cols = (cols + 1) // 2 * 2
        assert self.off + cols <= self.ncols, (name, self.off, cols, self.ncols)
        ap = self.big[:, self.off:self.off + cols]
        self.off += cols
        return ap, (name, self.gen)

BF16 = mybir.dt.bfloat16
from concourse.bass_utils import run_bass_kernel_spmd
import math

NB = 2; NL = 2048; NCX = 256; D = 1024
TL = NB * NL; TT = TL + NB * NCX
NT = TT // 128
CAPT = 8; CAP = CAPT * 128; NSLOT = 32 * CAP
HALF = CAP // 2
LAMBDA_INIT = 0.8 - 0.6 * math.exp(-0.3 * 1)
TWO_PI = 2.0 * math.pi


class Bld:
    def __init__(self, nc, P, big, pst, ncols):
        self.nc = nc; self.P = P; self.ar = Arena(big, ncols); self.pst = pst; self.pb = 0; self.brange = (0, 8)

    def T(self, name, cols, dt=F32):
        if dt == BF16:
            ap, k = self.ar.alloc(name, (cols + 1) // 2)
            return ap.bitcast(BF16)[:, 0:cols], k
        ap, k = self.ar.alloc(name, cols)
        if dt != F32:
            ap = ap.bitcast(dt)
        return ap, k

    def bank(self):
        lo, hi = self.brange
        i = lo + self.pb % (hi - lo); self.pb += 1
        return self.pst[i][:], 'ps%d' % i

    def mm(self, out, lhsT, rhs, start, stop, r, w):
        self.P.op('tensor', lambda e: e.matmul(out, lhsT=lhsT, rhs=rhs, start=start, stop=stop), r, w)

    def tr(self, out, in_, r, w):
        ident = self.ident
        self.P.op('tensor', lambda e: e.transpose(out, in_, ident), list(r) + [self.identk], w)

    def act(self, out, in_, func, r, w, **kw):
        self.P.op('scalar', lambda e: e.activation(out=out, in_=in_, func=func, **kw), r, w)

    def ts(self, eng, out, in0, s1, s2, op0, op1, r, w):
        if op1 is None:
            self.P.op(eng, lambda e: e.tensor_scalar(out=out, in0=in0, scalar1=s1, scalar2=None, op0=op0), r, w)
        else:
            self.P.op(eng, lambda e: e.tensor_scalar(out=out, in0=in0, scalar1=s1, scalar2=s2, op0=op0, op1=op1), r, w)

    def tt(self, eng, out, in0, in1, op, r, w):
        self.P.op(eng, lambda e: e.tensor_tensor(out=out, in0=in0, in1=in1, op=op), r, w)

    def stt(self, eng, out, in0, scalar, in1, op0, op1, r, w):
        self.P.op(eng, lambda e: e.scalar_tensor_tensor(out=out, in0=in0, scalar=scalar, in1=in1, op0=op0, op1=op1), r, w)

    def cp(self, eng, out, in_, r, w):
        if eng == 'scalar':
            self.P.op(eng, lambda e: e.copy(out=out, in_=in_), r, w)
        else:
            self.P.op(eng, lambda e: e.tensor_copy(out=out, in_=in_), r, w)

    def ms(self, eng, out, val, w):
        self.P.op(eng, lambda e: e.memset(out, val), (), w)

    def dma(self, q, out, in_, r, w, nc_ok=False):
        if nc_ok:
            self.P.dma(q, lambda e: e.dma_start(out=out, in_=in_, allow_slow_non_contiguous=True), r, w)
        else:
            self.P.dma(q, lambda e: e.dma_start(out=out, in_=in_), r, w)

    def phase(self):
        self.P.barrier()
        self.ar.reset()
        self.ar.off = self.persist


def tile_row(i):
    return i // 16 if i < 32 else 2


def run_pipeline(gens):
    active = []
    for g in gens:
        for a in list(active):
            if next(a, 'done') == 'done':
                active.remove(a)
        active.append(g)
        if next(g, 'done') == 'done':
            active.remove(g)
    while active:
        for a in list(active):
            if next(a, 'done') == 'done':
                active.remove(a)


def build_nc(stop_after=None, debug=False):
    nc = bass.Bass("TRN2", target_bir_lowering=False)

    def din(name, shape, dt=F32):
        return nc.dram_tensor(name, shape, dt, kind="ExternalInput").ap()

    def dscr(name, shape, dt=F32):
        return nc.dram_tensor(name, shape, dt, kind=("ExternalOutput" if debug else "Internal")).ap()

    x_in = din("x", [NB, NL, D]); ctx_in = din("ctx", [NB, NCX, D]); c_in = din("c", [NB, D]); cc_in = din("c_ctx", [1, D])
    w_ada = din("w_ada", [2, D, 6 * D]); b_ada = din("b_ada", [2, 6 * D]); g_mix = din("g_mix", [2, D]); g_ffn = din("g_ffn", [2, D])
    s5_a_re = din("s5_a_re", [1, 2, 64, 64]); s5_a_im = din("s5_a_im", [1, 2, 64, 64]); s5_log_dt = din("s5_log_dt", [1, 2, 64])
    s5_b_re = din("s5_b_re", [1, 2, 64, 64, 16]); s5_b_im = din("s5_b_im", [1, 2, 64, 64, 16])
    s5_c_re = din("s5_c_re", [1, 2, 64, 16, 64]); s5_c_im = din("s5_c_im", [1, 2, 64, 16, 64])
    s5_d = din("s5_d", [1, D]); s5_w_glu = din("s5_w_glu", [1, D, 2 * D]); s5_b_glu = din("s5_b_glu", [1, 2 * D])
    da_w_qkv = din("da_w_qkv", [1, D, 3 * D]); da_w_o = din("da_w_o", [1, D, D])
    da_q_gain = din("da_q_gain", [1, 64]); da_k_gain = din("da_k_gain", [1, 64])
    da_lam = [din("da_lam_" + n, [1, 64]) for n in ("q1", "k1", "q2", "k2")]
    da_sub_gain = din("da_sub_gain", [1, 128])
    moe_w_router = din("moe_w_router", [2, D, 32]); moe_b_router = din("moe_b_router", [2, 32])
    moe_w_gu = din("moe_w_gu", [2, 32, D, 2 * D]); moe_b_gu = din("moe_b_gu", [2, 32, 2 * D])
    moe_w_down = din("moe_w_down", [2, 32, D, D]); moe_b_down = din("moe_b_down", [2, 32, D])
    k_ident = din("k_ident", [128, 128]); k_ltri = din("k_ltri", [128, 128]); k_ones = din("k_ones", [128, 128])
    k_blk16 = din("k_blk16", [128, 128]); k_g2m = din("k_g2m", [128, 2]); k_blk64 = din("k_blk64", [128, 128])
    k_rot = din("k_rot", [128, 128]); k_cos = din("k_cos", [128, NL]); k_sin = din("k_sin", [128, NL])
    k_slot = din("k_slot", [128, 32]); k_pm = din("k_pm", [128, 2])
    out = nc.dram_tensor("out", [NB, NL, D], F32, kind="ExternalOutput").ap()
    cnt_out = nc.dram_tensor("cnt_out", [128, 64], F32, kind="ExternalOutput").ap()
    xa = dscr("xa", [TT, D]); mod = dscr("mod", [2, 3, 6 * D]); hT = dscr("hT", [D, TT]); sT = dscr("sT", [D, TT], BF16)
    Xs = dscr("Xs", [NSLOT, D], BF16); Ys = dscr("Ys", [NSLOT, D], BF16)
    Wblk_d = dscr("Wblk_d", [2, 128, 8, 8, 128], BF16); Bst_d = dscr("Bst_d", [2, 128, 8, 8, 2, 128], BF16)
    Cst_d = dscr("Cst_d", [2, 128, 8, 2, 32, 32], BF16)
    outf = out.rearrange("b n d -> (b n) d")

    with ExitStack() as st:
        NCOLS = 53000
        big_t = st.enter_context(nc.sbuf_tensor("big", [128, NCOLS], F32))
        pst = [st.enter_context(nc.psum_tensor("ps%d" % i, [128, 512], F32)) for i in range(8)]
        P = Prog(nc)
        B = Bld(nc, P, big_t[:], pst, NCOLS)
        T = B.T
        B.ident, B.identk = T("ident", 128)
        B.dma('sync', B.ident, k_ident, [], [B.identk])
        ltri, ltrik = T("ltri", 128); B.dma('sync', ltri, k_ltri, [], [ltrik])
        ones, onesk = T("ones", 128); B.dma('sync', ones, k_ones, [], [onesk])
        onesb, onesbk = T("onesb", 128, BF16); B.cp('vector', onesb, ones, [onesk], [onesbk])
        identb, identbk = T("identb", 128, BF16); B.cp('vector', identb, B.ident, [B.identk], [identbk])
        blk16, blk16k = T("blk16", 128); B.dma('sync', blk16, k_blk16, [], [blk16k])
        g2m, g2mk = T("g2m", 2); B.dma('sync', g2m, k_g2m, [], [g2mk])
        DEST, DESTk = T("DEST", NT * 4, I32); GATE, GATEk = T("GATE", NT * 4)
        DEST3 = DEST.rearrange("p (i k) -> p i k", k=4); GATE3 = GATE.rearrange("p (i k) -> p i k", k=4)
        persist_small = B.ar.off
        CA, CAk = T("CA", 2 * 8 * 2 * 64)
        CA5 = CA.rearrange("p (d i r g) -> p d i r g", d=2, i=8, r=2)
        CBn, CBnk = T("CBn", 2 * 8 * 64); CBn4 = CBn.rearrange("p (d i g) -> p d i g", d=2, i=8)
        CBp, CBpk = T("CBp", 2 * 8 * 64); CBp4 = CBp.rearrange("p (d i g) -> p d i g", d=2, i=8)
        B.persist = B.ar.off
        xkeys = [('xa', i) for i in range(NT)]

        def done(name):
            return stop_after == name

        for b in range(NB):
            B.dma('sync', xa[b * NL:(b + 1) * NL, :], x_in[b], [], xkeys[b * 16:(b + 1) * 16])
            B.dma('sync', xa[TL + b * NCX:TL + (b + 1) * NCX, :], ctx_in[b], [], xkeys[32 + 2 * b:34 + 2 * b])

        cT, cTk = T("cT", 24)
        cT3 = cT.rearrange("p (k r) -> p k r", r=3)
        for r in range(3):
            src = (c_in[r] if r < 2 else cc_in[0]).rearrange("(k p) -> p k", p=128)
            B.dma('sync', cT3[:, :, r], src, [], [cTk], nc_ok=True)
        B.act(cT, cT, AF.Silu, [cTk], [cTk])
        bt, btk = T("bt", 6 * D); modt, modtk = T("modt", 6 * D)
        wts = [T("wada%d" % i, 8 * 512) for i in range(2)]
        for l in range(2):
            B.dma('sync', bt[0:3, :], b_ada[l:l + 1, :].partition_broadcast(3), [modtk], [btk])
            for n in range(12):
                wt, wtk = wts[n % 2]
                wt3 = wt.rearrange("p (k n) -> p k n", n=512)
                B.dma('sync' if n % 2 == 0 else 'scalar', wt3, w_ada[l][:, n * 512:(n + 1) * 512].rearrange("(k p) n -> p k n", p=128), [], [wtk])
                ps, psk = B.bank()
                for k in range(8):
                    B.mm(ps[0:3, :], cT3[:, k, :], wt3[:, k, :], k == 0, k == 7, [cTk, wtk], [psk])
                B.tt('vector', modt[0:3, n * 512:(n + 1) * 512], ps[0:3, :], bt[0:3, n * 512:(n + 1) * 512], ALU.add, [psk, btk], [modtk])
            B.dma('sync', mod[l], modt[0:3, :], [modtk], [('mod', l)])
        if done('ada'):
            P.emit(); return nc

        def load_mod_tiles(l, gvec, off_sh, off_sc):
            gb, gbk = T("gvecb", D)
            B.dma('sync', gb, gvec[l:l + 1, :].partition_broadcast(128), [], [gbk])
            res = []
            for r in range(3):
                G, Gk = T("G%d" % r, D); S, Sk = T("S%d" % r, D)
                B.dma('sync', G, mod[l, r:r + 1, off_sc:off_sc + D].partition_broadcast(128), [('mod', l)], [Gk])
                B.dma('scalar', S, mod[l, r:r + 1, off_sh:off_sh + D].partition_broadcast(128), [('mod', l)], [Sk])
                B.stt('vector', G, G, 1.0, gb, ALU.add, ALU.mult, [Gk, gbk], [Gk])
                res.append((G, Gk, S, Sk))
            return res

        def load_gate_tiles(l, off):
            res = []
            for r in range(3):
                G, Gk = T("GT%d" % r, D)
                B.dma('sync', G, mod[l, r:r + 1, off:off + D].partition_broadcast(128), [('mod', l)], [Gk])
                res.append((G, Gk))
            return res

        def norm_tile(xt, xk, h, hk, mt, ss, ssk):
            G, Gk, S, Sk = mt
            B.ms('gpsimd', ss, 0.0, [ssk])
            B.act(h, xt, AF.Square, [xk, ssk], [hk, ssk], accum_out=ss[:, 0:1])
            B.ts('vector', ss[:, 1:2], ss[:, 0:1], 1.0 / D, 1e-6, ALU.mult, ALU.add, [ssk], [ssk])
            B.act(ss[:, 1:2], ss[:, 1:2], AF.Sqrt, [ssk], [ssk])
            B.P.op('vector', lambda e: e.reciprocal(out=ss[:, 1:2], in_=ss[:, 1:2]), [ssk], [ssk])
            B.stt('vector', h, xt, ss[:, 1:2], G, ALU.mult, ALU.mult, [xk, ssk, Gk, hk], [hk])
            B.tt('gpsimd', h, h, S, ALU.add, [hk, Sk], [hk])

        def norm_tile_g(xt, xk, h, hk, mt, ss, ssk):
            G, Gk, S, Sk = mt
            B.ms('gpsimd', ss, 0.0, [ssk])
            B.act(h, xt, AF.Square, [xk, ssk], [hk, ssk], accum_out=ss[:, 0:1])
            yield
            B.ts('vector', ss[:, 1:2], ss[:, 0:1], 1.0 / D, 1e-6, ALU.mult, ALU.add, [ssk], [ssk])
            B.act(ss[:, 1:2], ss[:, 1:2], AF.Sqrt, [ssk], [ssk])
            yield
            B.P.op('vector', lambda e: e.reciprocal(out=ss[:, 1:2], in_=ss[:, 1:2]), [ssk], [ssk])
            B.stt('vector', h, xt, ss[:, 1:2], G, ALU.mult, ALU.mult, [xk, ssk, Gk, hk], [hk])
            B.tt('gpsimd', h, h, S, ALU.add, [hk, Sk], [hk])
            yield

        def transpose_tile(h, hk, dst3, dstk):
            for half in range(2):
                ps, psk = B.bank()
                for kk in range(4):
                    k = half * 4 + kk
                    B.tr(ps[:, kk * 128:(kk + 1) * 128], h[:, k * 128:(k + 1) * 128], [hk], [psk])
                B.cp('scalar' if half == 0 else 'vector', dst3[:, half * 4:(half + 1) * 4, :], ps.rearrange("p (k t) -> p k t", t=128), [psk], [dstk])

        hT3 = hT.rearrange("(k p) t -> p k t", p=128)
        sT3 = sT.rearrange("(k p) t -> p k t", p=128)

        def phase_norm_T(l):
            B.phase()
            mts = load_mod_tiles(l, g_mix, 0, D)
            bufs = [(T("xt%d" % i, D), T("h%d" % i, D), T("hTt%d" % i, D), T("ss%d" % i, 2)) for i in range(5)]

            def tile_gen(i):
                (xt, xk), (h, hk), (ht, htk), (ss, ssk) = bufs[i % 5]
                B.dma('sync', xt, xa[i * 128:(i + 1) * 128, :], [xkeys[i]], [xk])
                yield
                for _ in norm_tile_g(xt, xk, h, hk, mts[tile_row(i)], ss, ssk):
                    yield
                ht3 = ht.rearrange("p (k t) -> p k t", t=128)
                transpose_tile(h, hk, ht3, htk)
                B.dma('scalar', hT3[:, :, i * 128:(i + 1) * 128], ht3, [htk], [('hT', i)])
            run_pipeline([tile_gen(i) for i in range(NT)])

        def bc3(ap2):
            return ap2.unsqueeze(2).to_broadcast([128, 32, 16])

        def phase_s5_setup():
            B.phase()
            dcol, dcolk = T("dcol", 8)
            B.dma('sync', dcol, s5_d[0].rearrange("(j p) -> p j", p=128), [], [dcolk], nc_ok=True)
            Wsb, Wsbk = T("Wsb", 8 * 8 * 128, BF16); Wsb4 = Wsb.rearrange("p (j e m) -> p j e m", j=8, e=8)
            Bsb, Bsbk = T("Bsb", 8 * 8 * 2 * 128, BF16); Bsb5 = Bsb.rearrange("p (j s r m) -> p j s r m", j=8, s=8, r=2)
            Csb, Csbk = T("Csb", 8 * 2 * 32 * 32, BF16); Csb5 = Csb.rearrange("p (t r g m) -> p t r g m", t=8, r=2, g=32)
            Are, Arek = T("Are", 32); Aim, Aimk = T("Aim", 32); DT, DTk = T("DT", 32)
            lr, lrk = T("lr", 32); li, lik = T("li", 32)
            LP, LPk = T("LP", 9 * 2 * 32); LP4 = LP.rearrange("p (e r g) -> p e r g", e=9, r=2)
            sm = [T("sm%d" % i, 32) for i in range(6)]
            ki, kik = T("ki", 32, I32)
            Bre, Brek = T("Bre", 512); Bim, Bimk = T("Bim", 512); Cre, Crek = T("Cre", 512); Cim, Cimk = T("Cim", 512)
            bbr, bbrk = T("bbr", 512); bbi, bbik = T("bbi", 512)
            t1, t1k = T("t1", 512); t2, t2k = T("t2", 512); Ere, Erek = T("Ere", 512); Eim, Eimk = T("Eim", 512)
            Cmr, Cmrk = T("Cmr", 1024); Cmi, Cmik = T("Cmi", 1024); Emr, Emrk = T("Emr", 1024); Emi, Emik = T("Emi", 1024)
            v3 = lambda a: a.rearrange("p (g c) -> p g c", c=16)
            v4 = lambda a: a.rearrange("p (g h c) -> p g h c", h=2, c=16)
            Cns = [T("Cn%d" % z, 8 * 64) for z in range(2)]
            Cxs = [T("Cx%d" % z, 128) for z in range(2)]; Cts = [T("Ct%d" % z, 128) for z in range(2)]
            pm, pmk = T("pm", 2); B.dma('sync', pm, k_pm, [], [pmk])
            for d in range(2):
                B.dma('sync', Are, s5_a_re[0, d].rearrange("(gp g2) p -> (g2 p) gp", g2=2), [], [Arek], nc_ok=True)
                B.dma('sync', Aim, s5_a_im[0, d].rearrange("(gp g2) p -> (g2 p) gp", g2=2), [], [Aimk], nc_ok=True)
                for g2 in range(2):
                    B.dma('sync', DT[g2 * 64:(g2 + 1) * 64, :], s5_log_dt[0, d:d + 1, :].rearrange("o (gp g2) -> o gp g2", g2=2)[:, :, g2].partition_broadcast(64), [], [DTk], nc_ok=True)
                B.dma('sync', v3(Bre), s5_b_re[0, d].rearrange("(gp g2) p c -> (g2 p) gp c", g2=2), [], [Brek], nc_ok=True)
                B.dma('scalar', v3(Bim), s5_b_im[0, d].rearrange("(gp g2) p c -> (g2 p) gp c", g2=2), [], [Bimk], nc_ok=True)
                for ci, (src, dst, dk) in enumerate(((s5_c_re, Cre, Crek), (s5_c_im, Cim, Cimk))):
                    Cn, Cnk = Cns[ci]
                    Cn3 = Cn.rearrange("p (j q) -> p j q", q=64)
                    B.dma('sync' if ci == 0 else 'scalar', Cn3, src[0, d].rearrange("(j g) c p -> (g c) j p", g=8), [], [Cnk])
                    for j in range(8):
                        Cx, Cxk = Cxs[j % 2]; Ct, Ctk = Cts[j % 2]
                        Cx3 = Cx.rearrange("p (h q) -> p h q", q=64)
                        for h in range(2):
                            B.ts('vector' if h == 0 else 'gpsimd', Cx3[:, h, :], Cn3[:, j, :], pm[:, h:h + 1], None, ALU.mult, None, [Cnk, pmk], [Cxk])
                        ps, psk = B.bank()
                        B.tr(ps[:, 0:128], Cx, [Cxk], [psk])
                        B.cp('scalar', Ct, ps[:, 0:128], [psk], [Ctk])
                        Ct4 = Ct.rearrange("p (q h c) -> p q h c", q=4, h=2)
                        B.tt('vector', v3(dst)[:, 4 * j:4 * j + 4, :], Ct4[:, :, 0, :], Ct4[:, :, 1, :], ALU.add, [Ctk], [dk])
                B.act(DT, DT, AF.Exp, [DTk], [DTk])
                B.tt('vector', lr, Are, DT, ALU.mult, [Arek, DTk], [lrk])
                B.tt('vector', li, Aim, DT, ALU.mult, [Aimk, DTk], [lik])
                B.ms('vector', LP4[:, 0, 0, :], 1.0, [LPk]); B.ms('vector', LP4[:, 0, 1, :], 0.0, [LPk])
                (mag, magk), (tq, tqk), (kf, kfk), (yy, yyk), (sn, snk), (den, denk) = sm
                for e in range(1, 9):
                    B.act(mag, lr, AF.Exp, [lrk], [magk], scale=float(e))
                    for which, off in ((1, 0.0), (0, 0.25)):
                        B.ts('vector', tq, li, e / TWO_PI, off, ALU.mult, ALU.add, [lik], [tqk])
                        B.cp('vector', ki, tq, [tqk], [kik])
                        B.cp('vector', kf, ki, [kik], [kfk])
                        B.ts('vector', kf, kf, -TWO_PI, off * TWO_PI, ALU.mult, ALU.add, [kfk], [kfk])
                        B.stt('vector', yy, li, float(e), kf, ALU.mult, ALU.add, [lik, kfk], [yyk])
                        B.act(sn, yy, AF.Sin, [yyk], [snk])
                        B.tt('vector', LP4[:, e, which, :], mag, sn, ALU.mult, [magk, snk], [LPk])
                for i8 in range(8):
                    e8 = 8 * (i8 + 1)
                    pr = []
                    B.act(mag, lr, AF.Exp, [lrk], [magk], scale=float(e8))
                    for which, off in ((1, 0.0), (0, 0.25)):
                        B.ts('vector', tq, li, e8 / TWO_PI, off, ALU.mult, ALU.add, [lik], [tqk])
                        B.cp('vector', ki, tq, [tqk], [kik])
                        B.cp('vector', kf, ki, [kik], [kfk])
                        B.ts('vector', kf, kf, -TWO_PI, off * TWO_PI, ALU.mult, ALU.add, [kfk], [kfk])
                        B.stt('vector', yy, li, float(e8), kf, ALU.mult, ALU.add, [lik, kfk], [yyk])
                        B.act(sn, yy, AF.Sin, [yyk], [snk])
                        if which == 1:
                            B.tt('vector', den, mag, sn, ALU.mult, [magk, snk], [denk])
                        else:
                            B.tt('vector', tq, mag, sn, ALU.mult, [magk, snk], [tqk])
                    g2v = lambda a: a.rearrange("p (g b) -> p g b", b=2)
                    reb = tq.unsqueeze(2).to_broadcast([128, 32, 2]); imb = den.unsqueeze(2).to_broadcast([128, 32, 2])
                    for r_ in range(2):
                        B.cp('vector', g2v(CA5[:, d, i8, r_, :]), reb, [tqk], [CAk])
                    B.cp('vector', g2v(CBp4[:, d, i8, :]), imb, [denk], [CBpk])
                    B.ts('vector', g2v(CBn4[:, d, i8, :]), imb, -1.0, None, ALU.mult, None, [denk], [CBnk])
                nr, nrk = sm[0]; qr, qrk = sm[1]; qi, qik = sm[2]; ta, tak = sm[3]; tb, tbk = sm[4]
                B.ts('vector', nr, LP4[:, 1, 0, :], -1.0, None, ALU.add, None, [LPk], [nrk])
                ni = LP4[:, 1, 1, :]
                B.tt('vector', den, Are, Are, ALU.mult, [Arek], [denk])
                B.tt('vector', ta, Aim, Aim, ALU.mult, [Aimk], [tak])
                B.tt('vector', den, den, ta, ALU.add, [denk, tak], [denk])
                B.P.op('vector', lambda e: e.reciprocal(out=den, in_=den), [denk], [denk])
                B.tt('vector', ta, nr, Are, ALU.mult, [nrk, Arek], [tak])
                B.tt('vector', tb, ni, Aim, ALU.mult, [LPk, Aimk], [tbk])
                B.tt('vector', ta, ta, tb, ALU.add, [tak, tbk], [tak])
                B.tt('vector', qr, ta, den, ALU.mult, [tak, denk], [qrk])
                B.tt('vector', ta, ni, Are, ALU.mult, [LPk, Arek], [tak])
                B.tt('vector', tb, nr, Aim, ALU.mult, [nrk, Aimk], [tbk])
                B.tt('vector', ta, ta, tb, ALU.subtract, [tak, tbk], [tak])
                B.tt('vector', qi, ta, den, ALU.mult, [tak, denk], [qik])

                def cmul(outr, outrk, outi, outik, ar_, ark, ai_, aik, br_, brk, bi_, bik):
                    B.tt('vector', v3(t1), v3(ar_), br_, ALU.mult, [ark, brk], [t1k])
                    B.tt('gpsimd', v3(t2), v3(ai_), bi_, ALU.mult, [aik, bik], [t2k])
                    B.tt('vector', outr, t1, t2, ALU.subtract, [t1k, t2k], [outrk])
                    B.tt('vector', v3(t1), v3(ar_), bi_, ALU.mult, [ark, bik, outrk], [t1k])
                    B.tt('gpsimd', v3(t2), v3(ai_), br_, ALU.mult, [aik, brk, outrk], [t2k])
                    B.tt('vector', outi, t1, t2, ALU.add, [t1k, t2k], [outik])
                cmul(bbr, bbrk, bbi, bbik, Bre, Brek, Bim, Bimk, bc3(qr), qrk, bc3(qi), qik)
                for h in range(2):
                    B.ts('vector', v4(Cmr)[:, :, h, :], v3(Cre), g2m[:, h:h + 1], None, ALU.mult, None, [Crek, g2mk], [Cmrk])
                    B.ts('vector', v4(Cmi)[:, :, h, :], v3(Cim), g2m[:, h:h + 1], -1.0, ALU.mult, ALU.mult, [Cimk, g2mk], [Cmik])
                for e in range(9):
                    Lr_b = bc3(LP4[:, e, 0, :]); Li_b = bc3(LP4[:, e, 1, :])
                    if e <= 7:
                        cmul(Ere, Erek, Eim, Eimk, bbr, bbrk, bbi, bbik, Lr_b, LPk, Li_b, LPk)
                        for h in range(2):
                            B.ts('vector', v4(Emr)[:, :, h, :], v3(Ere), g2m[:, h:h + 1], None, ALU.mult, None, [Erek, g2mk], [Emrk])
                            B.ts('gpsimd', v4(Emi)[:, :, h, :], v3(Eim), g2m[:, h:h + 1], None, ALU.mult, None, [Eimk, g2mk], [Emik])
                        s_idx = (7 - e) if d == 0 else e
                        for j in range(8):
                            sl = slice(j * 128, (j + 1) * 128)
                            for ri, (Em, Emk) in enumerate(((Emr, Emrk), (Emi, Emik))):
                                ps, psk = B.bank()
                                B.tr(ps[:, 0:128], Em[:, sl], [Emk], [psk])
                                B.cp('scalar', Bsb5[:, j, s_idx, ri, :], ps[:, 0:128], [psk], [Bsbk])
                            ps, psk = B.bank()
                            B.mm(ps[:, 0:128], Emr[:, sl], Cmr[:, sl], True, False, [Emrk, Cmrk], [psk])
                            B.mm(ps[:, 0:128], Emi[:, sl], Cmi[:, sl], False, True, [Emik, Cmik], [psk])
                            B.tt('vector', Wsb4[:, j, e, :], ps[:, 0:128], blk16, ALU.mult, [psk, blk16k], [Wsbk])
                            if d == 0 and e == 0:
                                B.stt('vector', Wsb4[:, j, 0, :], B.ident, dcol[:, j:j + 1], Wsb4[:, j, 0, :], ALU.mult, ALU.add, [B.identk, dcolk, Wsbk], [Wsbk])
                    if e >= 1:
                        cmul(Ere, Erek, Eim, Eimk, Cre, Crek, Cim, Cimk, Lr_b, LPk, Li_b, LPk)
                        t_idx = (e - 1) if d == 0 else (8 - e)
                        for h in range(2):
                            B.ts('vector', Csb5[:, t_idx, 0, :, h * 16:(h + 1) * 16], v3(Ere), g2m[:, h:h + 1], None, ALU.mult, None, [Erek, g2mk], [Csbk])
                            B.ts('vector', Csb5[:, t_idx, 1, :, h * 16:(h + 1) * 16], v3(Eim), g2m[:, h:h + 1], -1.0, ALU.mult, ALU.mult, [Eimk, g2mk], [Csbk])
                B.dma('sync', Wblk_d[d], Wsb4, [Wsbk], [('Wblk', d)])
                B.dma('sync', Bst_d[d], Bsb5, [Bsbk], [('Bst', d)])
                B.dma('sync', Cst_d[d], Csb5, [Csbk], [('Cst', d)])

        def phase_s5_main():
            B.phase()
            NBUF = 2
            hjs = [T("hj%d" % z, NB * 2304, BF16) for z in range(NBUF)]
            Wjs = [T("Wj%d" % z, 2 * 8 * 128, BF16) for z in range(NBUF)]
            Bjs = [T("Bj%d" % z, 2 * 8 * 2 * 128, BF16) for z in range(NBUF)]
            Cjs = [T("Cj%d" % z, 2 * 8 * 2 * 4 * 32, BF16) for z in range(NBUF)]
            Bjzs = [T("Bjz%d" % z, 2 * 8 * 2 * 128, BF16) for z in range(NBUF)]
            Cjzs = [T("Cjz%d" % z, 2 * 8 * 2 * 64, BF16) for z in range(NBUF)]
            Hbs = [[T("Hb%d_%d" % (z, d), 16 * 288, BF16) for d in range(2)] for z in range(NBUF)]
            Hl = [T("Hl%d" % d, 16 * 288) for d in range(2)]
            v5 = lambda a_, k: a_.rearrange("p (r q b k) -> p r q b k", r=2, q=4, b=2, k=k)
            Sa = [T("Sa%d" % d, 576) for d in range(2)]; Si = [T("Si%d" % d, 576) for d in range(2)]
            tA = [T("tA%d" % d, 576) for d in range(2)]; tB = [T("tB%d" % d, 576) for d in range(2)]
            vA = lambda a_: a_.rearrange("p (r g k i) -> p r g k i", r=2, g=8, i=8)
            vS = lambda a_: a_.rearrange("p (r g k) -> p r g k", r=2, g=8)
            ysb, ysbk = T("ysb", NB * 2304, BF16)
            ysb3 = ysb.rearrange("p (b t) -> p b t", b=NB); ysb4 = ysb.rearrange("p (b k s) -> p b k s", b=NB, s=8)
            yas = [(T("ya%d" % z, 288), T("yb%d" % z, 288)) for z in range(2)]
            engs = ['vector', 'gpsimd']
            hkeys = [('hT', i) for i in range(NT)]

            def views(j):
                z = j % NBUF
                hj, hjk = hjs[z]; Wj, Wjk = Wjs[z]; Bj, Bjk = Bjs[z]; Cj, Cjk = Cjs[z]; Bjz, Bjzk = Bjzs[z]; Cjz, Cjzk = Cjzs[z]
                return dict(hj=hj, hjk=hjk, hj3=hj.rearrange("p (b t) -> p b t", b=NB), hj4=hj.rearrange("p (b k s) -> p b k s", b=NB, s=8),
                            Wj4=Wj.rearrange("p (d e m) -> p d e m", d=2, e=8), Wjk=Wjk,
                            Bj=Bj, Bj5=Bj.rearrange("p (d s r m) -> p d s r m", d=2, s=8, r=2), Bjk=Bjk,
                            Cj6=Cj.rearrange("p (d t r q m) -> p d t r q m", d=2, t=8, r=2, q=4), Cjk=Cjk,
                            Bjz=Bjz, Bjz5=Bjz.rearrange("p (d s r m) -> p d s r m", d=2, s=8, r=2), Bjzk=Bjzk,
                            Cjz=Cjz, Cjz5=Cjz.rearrange("p (d t r m) -> p d t r m", d=2, t=8, r=2), Cjzk=Cjzk, Hb=Hbs[z])

            def load(j):
                v = views(j); rows = slice(j * 128, (j + 1) * 128)
                for b in range(NB):
                    B.dma('gpsimd', v['hj3'][:, b, 0:NCX], hT[rows, TL + b * NCX:TL + (b + 1) * NCX], hkeys, [v['hjk']])
                    B.dma('gpsimd', v['hj3'][:, b, NCX:2304], hT[rows, b * NL:(b + 1) * NL], hkeys, [v['hjk']])
                for d in range(2):
                    B.dma('sync', v['Wj4'][:, d], Wblk_d[d, :, j], [('Wblk', d)], [v['Wjk']])
                    B.dma('sync', v['Bj5'][:, d], Bst_d[d, :, j], [('Bst', d)], [v['Bjk']])
                    B.dma('sync', v['Cj6'][:, d], Cst_d[d, :, :, :, 4 * j:4 * j + 4, :], [('Cst', d)], [v['Cjk']])
                B.cp('vector', v['Bjz'][64:128, :], v['Bj'][64:128, :], [v['Bjk']], [v['Bjzk']])
                B.ms('vector', v['Bjz'][64:96, :], 0.0, [v['Bjzk']])
                B.ms('gpsimd', v['Cjz'], 0.0, [v['Cjzk']])
                for d in range(2):
                    B.cp('gpsimd', v['Cjz5'][:, d, :, :, 32:64], v['Cj6'][:, d, :, :, 3, :], [v['Cjk'], v['Cjzk']], [v['Cjzk']])

            def state(j):
                v = views(j)
                for d in range(2):
                    Hl5 = v5(Hl[d][0], 288)
                    for ri in range(2):
                        for q in range(4):
                            for b in range(NB):
                                ps, psk = B.bank()
                                for s_ in range(8):
                                    if q < 3:
                                        B.mm(ps[:, 0:288], v['Bj5'][32 * q:32 * q + 32, d, s_, ri, :], v['hj4'][32 * q:32 * q + 32, b, :, s_], s_ == 0, s_ == 7, [v['Bjk'], v['hjk']], [psk])
                                    else:
                                        B.mm(ps[:, 0:288], v['Bjz5'][64:128, d, s_, ri, :], v['hj4'][64:128, b, :, s_], s_ == 0, s_ == 7, [v['Bjzk'], v['hjk']], [psk])
                                B.cp('scalar', Hl5[:, ri, q, b, :], ps[:, 0:288], [psk], [Hl[d][1]])

            def chain(j):
                v = views(j)

                def cm(d, src, i8, n):
                    eng = engs[d]
                    gsl = slice(8 * j, 8 * j + 8)
                    LAb = CA5[:, d, i8, :, gsl].unsqueeze(3).to_broadcast([128, 2, 8, n])
                    LNb = CBn4[:, d, i8, gsl].unsqueeze(2).to_broadcast([128, 8, n])
                    LPb = CBp4[:, d, i8, gsl].unsqueeze(2).to_broadcast([128, 8, n])
                    t1 = tA[d][0][:, 0:16 * n].rearrange("p (r g k) -> p r g k", r=2, g=8); t1k = tA[d][1]
                    t2 = tB[d][0][:, 0:16 * n].rearrange("p (r g k) -> p r g k", r=2, g=8); t2k = tB[d][1]
                    rk = [Hl[d][1], Sa[d][1], Si[d][1], CAk, CBnk, CBpk]
                    B.tt(eng, t1, src, LAb, ALU.mult, rk, [t1k])
                    B.tt(eng, t2[:, 0], src[:, 1], LNb, ALU.mult, rk, [t2k])
                    B.tt(eng, t2[:, 1], src[:, 0], LPb, ALU.mult, rk, [t2k])
                    B.tt(eng, t1, t1, t2, ALU.add, [t1k, t2k], [t1k])
                    return t1, t1k
                A5 = [vA(Hl[d][0]) for d in range(2)]; Ak = [Hl[d][1] for d in range(2)]
                S4 = [vS(Sa[d][0]) for d in range(2)]; Sk = [Sa[d][1] for d in range(2)]
                I4 = [vS(Si[d][0]) for d in range(2)]; Ik = [Si[d][1] for d in range(2)]
                for step in range(7):
                    for d in range(2):
                        i = step + 1 if d == 0 else 6 - step
                        prev = i - 1 if d == 0 else i + 1
                        t1, t1k = cm(d, A5[d][:, :, :, :, prev], 0, 36)
                        B.tt(engs[d], A5[d][:, :, :, :, i], A5[d][:, :, :, :, i], t1, ALU.add, [Ak[d], t1k], [Ak[d]])
                    yield
                seqs = [[(0, None)] + [(b_, b_ - 1) for b_ in range(1, 36)],
                        [(3, None), (2, 3), (1, 2), (0, 1), (35, 0)] + [(b_, b_ + 1) for b_ in range(34, 3, -1)]]
                endi = [7, 0]
                for step in range(36):
                    for d in range(2):
                        bd_, bs_ = seqs[d][step]
                        if bs_ is None:
                            B.cp(engs[d], S4[d][:, :, :, bd_:bd_ + 1], A5[d][:, :, :, bd_:bd_ + 1, endi[d]], [Ak[d]], [Sk[d]])
                        else:
                            t1, t1k = cm(d, S4[d][:, :, :, bs_:bs_ + 1], 7, 1)
                            B.tt(engs[d], S4[d][:, :, :, bd_:bd_ + 1], A5[d][:, :, :, bd_:bd_ + 1, endi[d]], t1, ALU.add, [Ak[d], t1k], [Sk[d]])
                    if step % 2 == 1:
                        yield
                B.ms(engs[0], I4[0][:, :, :, 0:1], 0.0, [Ik[0]])
                B.cp(engs[0], I4[0][:, :, :, 1:36], S4[0][:, :, :, 0:35], [Sk[0]], [Ik[0]])
                B.ms(engs[1], I4[1][:, :, :, 3:4], 0.0, [Ik[1]])
                B.cp(engs[1], I4[1][:, :, :, 0:3], S4[1][:, :, :, 1:4], [Sk[1]], [Ik[1]])
                B.cp(engs[1], I4[1][:, :, :, 4:35], S4[1][:, :, :, 5:36], [Sk[1]], [Ik[1]])
                B.cp(engs[1], I4[1][:, :, :, 35:36], S4[1][:, :, :, 0:1], [Sk[1]], [Ik[1]])
                yield
                for i in range(8):
                    for d in range(2):
                        pw = i if d == 0 else 7 - i
                        t1, t1k = cm(d, I4[d], pw, 36)
                        B.tt(engs[d], A5[d][:, :, :, :, i], A5[d][:, :, :, :, i], t1, ALU.add, [Ak[d], t1k], [Ak[d]])
                    yield
                vK = lambda a_: a_.rearrange("p (r g k) -> p r g k", r=2, g=8)
                Af = vK(Hl[0][0]); Ab = vK(Hl[1][0]); Hbf = vK(v['Hb'][0][0]); Hbb = vK(v['Hb'][1][0])
                hk0 = v['Hb'][0][1]; hk1 = v['Hb'][1][1]
                B.ms('vector', Hbf[:, :, :, 0:1], 0.0, [hk0])
                B.cp('scalar', Hbf[:, :, :, 1:288], Af[:, :, :, 0:287], [Hl[0][1]], [hk0])
                B.cp('scalar', Hbb[:, :, :, 0:287], Ab[:, :, :, 1:288], [Hl[1][1]], [hk1])
                B.ms('vector', Hbb[:, :, :, 31:32], 0.0, [hk1])
                B.cp('vector', Hbb[:, :, :, 287:288], Ab[:, :, :, 0:1], [Hl[1][1]], [hk1])
                yield

            def outp(j, gen):
                v = views(j); rows = slice(j * 128, (j + 1) * 128)
                gi = 0
                for t in range(8):
                    for b in range(NB):
                        ps, psk = B.bank()
                        first = True
                        for s_ in range(0, t + 1):
                            B.mm(ps[:, 0:288], v['Wj4'][:, 0, t - s_, :], v['hj4'][:, b, :, s_], first, False, [v['Wjk'], v['hjk']], [psk]); first = False
                        for s_ in range(t, 8):
                            B.mm(ps[:, 0:288], v['Wj4'][:, 1, s_ - t, :], v['hj4'][:, b, :, s_], False, False, [v['Wjk'], v['hjk']], [psk])
                        cnt_ = 0
                        for d in range(2):
                            Hb5 = v5(v['Hb'][d][0], 288)
                            for ri in range(2):
                                for q in range(4):
                                    cnt_ += 1
                                    last_round = (d == 1 and ri == 1)
                                    if q < 3:
                                        B.mm(ps[32 * q:32 * q + 32, 0:288], v['Cj6'][:, d, t, ri, q, :], Hb5[:, ri, q, b, 0:288], False, last_round and q < 2, [v['Cjk'], v['Hb'][d][1]], [psk])
                                    else:
                                        B.mm(ps[64:128, 0:288], v['Cjz5'][:, d, t, ri, :], Hb5[:, ri, q, b, 0:288], False, last_round, [v['Cjzk'], v['Hb'][d][1]], [psk])
                        (ya, yak), (yb, ybk) = yas[gi % 2]; gi += 1
                        B.cp('scalar', ya, ps[:, 0:288], [psk], [yak])
                        B.act(yb, ya, AF.Square, [yak], [ybk])
                        B.ts('vector', yb, yb, 0.044715, 1.0, ALU.mult, ALU.add, [ybk], [ybk])
                        B.tt('vector', yb, yb, ya, ALU.mult, [ybk, yak], [ybk])
                        B.act(yb, yb, AF.Sigmoid, [ybk], [ybk], scale=1.5957691216057308)
                        B.tt('vector', ysb4[:, b, :, t], ya, yb, ALU.mult, [yak, ybk], [ysbk])
                        if gen is not None:
                            for _ in range(5):
                                next(gen, None)
                if gen is not None:
                    for _ in gen:
                        pass
                for b in range(NB):
                    B.dma('sync', sT[rows, TL + b * NCX:TL + (b + 1) * NCX], ysb3[:, b, 0:NCX], [ysbk], [('sT', j)])
                    B.dma('sync', sT[rows, b * NL:(b + 1) * NL], ysb3[:, b, NCX:2304], [ysbk], [('sT', j)])

            load(0); state(0)
            for _ in chain(0):
                pass
            load(1)
            for j in range(8):
                gen = None
                if j + 1 < 8:
                    state(j + 1)
                    gen = chain(j + 1)
                outp(j, gen)
                if j + 2 < 8:
                    load(j + 2)

        def phase_glu():
            B.phase()
            wg, wgk = T("wglu", 8 * 2048, BF16); wg3 = wg.rearrange("p (k n) -> p k n", n=2048)
            for k in range(8):
                B.dma('gpsimd', wg3[:, k, :], s5_w_glu[0, k * 128:(k + 1) * 128, :], [], [wgk])
            bg, bgk = T("bglu", 2048)
            B.dma('sync', bg, s5_b_glu[0:1, :].partition_broadcast(128), [], [bgk])
            gts = load_gate_tiles(0, 2 * D)
            bufs = [(T("yt%d" % i, 1024, BF16), T("xt%d" % i, D), T("xn%d" % i, D), T("at%d" % i, 512), T("gt%d" % i, 512)) for i in range(3)]
            skeys = [('sT', j) for j in range(8)]
            def glu_gen(i):
                (yt, ytk), (xt, xk), (xn, xnk), (a_t, atk), (g_t, gtk) = bufs[i % 3]
                yt3 = yt.rearrange("p (k t) -> p k t", t=128)
                B.dma('scalar', yt3, sT3[:, :, i * 128:(i + 1) * 128], skeys, [ytk])
                B.dma('sync', xt, xa[i * 128:(i + 1) * 128, :], [xkeys[i]], [xk])
                yield
                pss = [B.bank() for n in range(4)]
                for n in range(4):
                    for k in range(8):
                        B.mm(pss[n][0], yt3[:, k, :], wg3[:, k, n * 512:(n + 1) * 512], k == 0, k == 7, [ytk, wgk], [pss[n][1]])
                yield
                GT, GTk = gts[tile_row(i)]
                for hh in range(2):
                    cs = slice(hh * 512, (hh + 1) * 512)
                    B.tt('vector', a_t, pss[hh][0], bg[:, cs], ALU.add, [pss[hh][1], bgk], [atk])
                    B.tt('vector', g_t, pss[2 + hh][0], bg[:, 1024 + hh * 512:1024 + (hh + 1) * 512], ALU.add, [pss[2 + hh][1], bgk], [gtk])
                    B.act(g_t, g_t, AF.Sigmoid, [gtk], [gtk])
                    B.tt('gpsimd', a_t, a_t, g_t, ALU.mult, [atk, gtk], [atk])
                    B.tt('gpsimd', a_t, a_t, GT[:, cs], ALU.mult, [atk, GTk], [atk])
                    B.tt('gpsimd', xn[:, cs], a_t, xt[:, cs], ALU.add, [atk, xk], [xnk])
                B.dma('sync', xa[i * 128:(i + 1) * 128, :], xn, [xnk], [xkeys[i]])
            run_pipeline([glu_gen(i) for i in range(NT)])


        def phase_moe(l, ntiles, final):
            B.phase()
            mts = load_mod_tiles(l, g_ffn, 3 * D, 4 * D)
            wr, wrk = T("wr", 8 * 32); wr3 = wr.rearrange("p (k e) -> p k e", e=32)
            B.dma('sync', wr3, moe_w_router[l].rearrange("(k p) e -> p k e", p=128), [], [wrk])
            brb, brbk = T("brb", 32); B.dma('sync', brb, moe_b_router[l:l + 1, :].partition_broadcast(128), [], [brbk])
            slot0, slot0k = T("slot0", 32); B.dma('sync', slot0, k_slot, [], [slot0k])
            base, basek = T("base", 32); B.ms('vector', base, 0.0, [basek])
            NRB = 4
            bufs = [(T("xt%d" % i, D), T("h%d" % i, D), T("hTt%d" % i, D), T("ss%d" % i, 2)) for i in range(NRB)]
            smalls = [(T("lg%d" % z, 32), T("top8%d" % z, 8), T("mask%d" % z, 32), T("sl%d" % z, 32),
                       T("oh%d" % z, 32 * 4), T("nb%d" % z, 2), T("ex%d" % z, 4), T("destf%d" % z, 4)) for z in range(NRB)]

            def router_gen(i):
                (lg, lgk), (top8, top8k), (mask, maskk), (sl, slk), (oh4, ohk), (nb, nbk), (ex, exk), (destf, destfk) = smalls[i % NRB]
                (xt, xk), (h, hk), (ht, htk), (ss, ssk) = bufs[i % NRB]
                B.dma('sync', xt, xa[i * 128:(i + 1) * 128, :], [xkeys[i]], [xk])
                g_ = norm_tile_g(xt, xk, h, hk, mts[tile_row(i)], ss, ssk)
                next(g_)
                yield
                next(g_); next(g_)
                ht3 = ht.rearrange("p (k t) -> p k t", t=128)
                transpose_tile(h, hk, ht3, htk)
                yield
                ps, psk = B.bank()
                for k in range(8):
                    B.mm(ps[:, 0:32], ht3[:, k, :], wr3[:, k, :], k == 0, k == 7, [htk, wrk], [psk])
                B.tt('vector', lg, ps[:, 0:32], brb, ALU.add, [psk, brbk], [lgk])
                B.P.op('vector', lambda e: e.max(out=top8, in_=lg), [lgk], [top8k])
                B.ts('vector', mask, lg, top8[:, 3:4], None, ALU.is_ge, None, [lgk, top8k], [maskk])
                B.ts('vector', nb[:, 0:1], top8[:, 0:1], -1.0, None, ALU.mult, None, [top8k], [nbk])
                B.ms('vector', nb[:, 1:2], 0.0, [nbk])
                B.act(ex, top8[:, 0:4], AF.Exp, [top8k, nbk], [exk, nbk], bias=nb[:, 0:1], scale=1.0, accum_out=nb[:, 1:2])
                ps2, ps2k = B.bank()
                B.mm(ps2[:, 0:32], ltri, mask, True, True, [ltrik, maskk], [ps2k])
                ps3, ps3k = B.bank()
                B.mm(ps3[:, 0:32], ones, mask, True, True, [onesk, maskk], [ps3k])
                yield
                B.P.op('vector', lambda e: e.reciprocal(out=nb[:, 1:2], in_=nb[:, 1:2]), [nbk], [nbk])
                B.ts('vector', GATE3[:, i, :], ex, nb[:, 1:2], None, ALU.mult, None, [exk, nbk], [GATEk])
                B.tt('vector', sl, ps2[:, 0:32], base, ALU.add, [ps2k, basek], [slk])
                B.ts('vector', sl, sl, float(CAP - 1), None, ALU.min, None, [slk], [slk])
                B.tt('vector', sl, sl, slot0, ALU.add, [slk, slot0k], [slk])
                B.tt('vector', base, base, ps3[:, 0:32], ALU.add, [basek, ps3k, slk], [basek])
                for k in range(4):
                    oh = oh4[:, 32 * k:32 * (k + 1)]
                    B.ts('vector', oh, lg, top8[:, k:k + 1], None, ALU.is_equal, None, [lgk, top8k], [ohk])
                    B.tt('vector', oh, oh, sl, ALU.mult, [ohk, slk], [ohk])
                for k in range(4):
                    B.P.op('vector', lambda e, k=k: e.reduce_sum(out=destf[:, k:k + 1], in_=oh4[:, 32 * k:32 * (k + 1)], axis=AX.X), [ohk], [destfk])
                B.ts('vector', destf, destf, float(NSLOT - 1), None, ALU.min, None, [destfk], [destfk])
                B.cp('vector', DEST3[:, i, :], destf, [destfk], [DESTk])
                for k in range(4):
                    B.P.dma('gpsimd', lambda e, k=k: e.indirect_dma_start(out=Xs, out_offset=bass.IndirectOffsetOnAxis(ap=DEST3[:, i, k:k + 1], axis=0), in_=h, in_offset=None), [hk, DESTk], ['Xs'])
            run_pipeline([router_gen(i) for i in range(ntiles)])
            B.dma('sync', cnt_out[:, 32 * l:32 * (l + 1)], base, [basek], [('cnt', l)])
            B.phase()
            wgu = [T("wgu%d" % i, 8 * 2048, BF16) for i in range(2)]
            wd = [T("wd%d" % i, 8 * 1024, BF16) for i in range(2)]
            bgu = [T("bgu%d" % i, 16) for i in range(2)]; bdb = [T("bdb%d" % i, D) for i in range(2)]
            xTs = [T("xT%d" % i, 8 * CAP, BF16) for i in range(2)]
            aT, aTk = T("aT", 8 * CAP, BF16); aT3 = aT.rearrange("p (k t) -> p k t", t=CAP)
            xs = [T("xs%d" % i, D, BF16) for i in range(CAPT)]; yo = [T("yo%d" % i, D, BF16) for i in range(2)]
            ep = [(T("g_t%d" % i, HALF), T("u_t%d" % i, HALF), T("s_t%d" % i, HALF)) for i in range(2)]
            cnt = 0

            def load_w(e):
                (wg, wgk) = wgu[e % 2]; (wdd, wdk) = wd[e % 2]; (bg, bgk) = bgu[e % 2]; (bd, bdk) = bdb[e % 2]
                wg3 = wg.rearrange("p (k n) -> p k n", n=2048); wd3 = wdd.rearrange("p (k n) -> p k n", n=1024)
                for k4 in range(2):
                    B.dma('gpsimd', wg3[:, 4 * k4:4 * k4 + 4, :], moe_w_gu[l, e, 512 * k4:512 * (k4 + 1), :].rearrange("(k p) n -> p k n", p=128), [], [wgk])
                B.dma('gpsimd', wd3, moe_w_down[l, e].rearrange("(k p) n -> p k n", p=128), [], [wdk])
                B.dma('scalar', bg, moe_b_gu[l, e].rearrange("(c p) -> p c", p=128), [], [bgk], nc_ok=True)
                B.dma('scalar', bd, moe_b_down[l, e:e + 1, :].partition_broadcast(128), [], [bdk])

            def load_x(e):
                for stl in range(CAPT):
                    r0 = e * CAP + stl * 128
                    B.dma('sync', xs[stl][0], Xs[r0:r0 + 128, :], ['Xs'], [xs[stl][1]])

            def transposes(e):
                xT, xTk = xTs[e % 2]; xT3 = xT.rearrange("p (k t) -> p k t", t=CAP)
                for stl in range(CAPT):
                    (x_, x_k) = xs[stl]
                    ps, psk = B.bank()
                    psb = ps.bitcast(BF16)
                    for k in range(8):
                        B.P.op('tensor', lambda e, o=psb[:, k * 128:(k + 1) * 128], i_=x_[:, k * 128:(k + 1) * 128]: e.transpose(o, i_, identb), [x_k, identbk], [psk])
                    B.cp('scalar' if stl % 2 == 0 else 'vector', xT3[:, :, stl * 128:(stl + 1) * 128], psb.rearrange("p (k t) -> p k t", t=128), [psk], [xTk])
            load_w(0); load_x(0); transposes(0); load_x(1)
            for e in range(32):
                (wg, wgk) = wgu[e % 2]; (wdd, wdk) = wd[e % 2]; (bg, bgk) = bgu[e % 2]; (bd, bdk) = bdb[e % 2]
                wg3 = wg.rearrange("p (k n) -> p k n", n=2048); wd3 = wdd.rearrange("p (k n) -> p k n", n=1024)
                xT, xTk = xTs[e % 2]; xT3 = xT.rearrange("p (k t) -> p k t", t=CAP)
                if e + 1 < 32:
                    load_w(e + 1)
                for fc in range(8):
                    for hf in range(2):
                        cols = slice(hf * HALF, (hf + 1) * HALF)
                        (g_t, gk_), (u_t, uk_), (s_t, sk_) = ep[cnt % 2]; cnt += 1
                        psg, psgk = B.bank(); psu, psuk = B.bank()
                        for k in range(8):
                            B.mm(psg[:, 0:HALF], wg3[:, k, fc * 128:(fc + 1) * 128], xT3[:, k, cols], k == 0, k == 7, [wgk, xTk], [psgk])
                        for k in range(8):
                            B.mm(psu[:, 0:HALF], wg3[:, k, 1024 + fc * 128:1024 + (fc + 1) * 128], xT3[:, k, cols], k == 0, k == 7, [wgk, xTk], [psuk])
                        B.ts('vector', g_t, psg[:, 0:HALF], bg[:, fc:fc + 1], 7.0, ALU.add, ALU.min, [psgk, bgk], [gk_])
                        B.ts('vector', u_t, psu[:, 0:HALF], bg[:, 8 + fc:9 + fc], 7.0, ALU.add, ALU.min, [psuk, bgk], [uk_])
                        B.ts('vector', u_t, u_t, -7.0, 1.0, ALU.max, ALU.add, [uk_], [uk_])
                        B.act(s_t, g_t, AF.Sigmoid, [gk_], [sk_], scale=1.702)
                        B.tt('gpsimd', g_t, g_t, s_t, ALU.mult, [gk_, sk_], [gk_])
                        B.tt('vector', aT3[:, fc, cols], g_t, u_t, ALU.mult, [gk_, uk_], [aTk])
                if e + 1 < 32:
                    transposes(e + 1)
                    if e + 2 < 32:
                        load_x(e + 2)
                for stl in range(CAPT):
                    (y_, y_k) = yo[stl % 2]
                    for nh in range(2):
                        ps, psk = B.bank()
                        for k in range(8):
                            B.mm(ps, aT3[:, k, stl * 128:(stl + 1) * 128], wd3[:, k, nh * 512:(nh + 1) * 512], k == 0, k == 7, [aTk, wdk], [psk])
                        B.tt('vector', y_[:, nh * 512:(nh + 1) * 512], ps, bd[:, nh * 512:(nh + 1) * 512], ALU.add, [psk, bdk], [y_k])
                    r0 = e * CAP + stl * 128
                    B.dma('sync', Ys[r0:r0 + 128, :], y_, [y_k], ['Ys'])
            B.phase()
            gts = load_gate_tiles(l, 5 * D)
            bufs = [([T("cg%d_%d" % (i, k), D, BF16) for k in range(4)], T("acc%d" % i, D), T("cx%d" % i, D)) for i in range(3)]
            def comb_gen(i):
                gl, (acc, acck), (xt, xk) = bufs[i % 3]
                B.dma('sync', xt, xa[i * 128:(i + 1) * 128, :], [xkeys[i]], [xk])
                for k in range(4):
                    B.P.dma('gpsimd', lambda e, i=i, k=k, g=gl[k][0]: e.indirect_dma_start(out=g, out_offset=None, in_=Ys, in_offset=bass.IndirectOffsetOnAxis(ap=DEST3[:, i, k:k + 1], axis=0)), ['Ys', DESTk], [gl[k][1]])
                yield
                yield
                B.ts('vector', acc, gl[0][0], GATE3[:, i, 0:1], None, ALU.mult, None, [gl[0][1], GATEk], [acck])
                for k in range(1, 4):
                    B.stt('vector', acc, gl[k][0], GATE3[:, i, k:k + 1], acc, ALU.mult, ALU.add, [gl[k][1], GATEk, acck], [acck])
                GT, GTk = gts[tile_row(i)]
                B.tt('gpsimd', acc, acc, GT, ALU.mult, [acck, GTk], [acck])
                B.tt('gpsimd', acc, acc, xt, ALU.add, [acck, xk], [acck])
                if final:
                    B.dma('sync', outf[i * 128:(i + 1) * 128, :], acc, [acck], [('out', i)])
                else:
                    B.dma('sync', xa[i * 128:(i + 1) * 128, :], acc, [acck], [xkeys[i]])
            run_pipeline([comb_gen(i) for i in range(ntiles)])

        def phase_attn():
            B.phase()
            B.brange = (0, 3)
            blk64, blk64k = T("blk64", 128); B.dma('sync', blk64, k_blk64, [], [blk64k])
            rot, rotk = T("rot", 128); B.dma('sync', rot, k_rot, [], [rotk])
            cs, csk = T("cos", NL); B.dma('sync', cs, k_cos, [], [csk])
            sn, snk = T("sin", NL); B.dma('scalar', sn, k_sin, [], [snk])
            gc, gck = T("gcols", 8)
            for c in range(2):
                B.dma('sync', gc[c * 64:(c + 1) * 64, 0:1], da_q_gain[0:1, :].rearrange("o d -> d o"), [], [gck], nc_ok=True)
                B.dma('sync', gc[c * 64:(c + 1) * 64, 1:2], da_k_gain[0:1, :].rearrange("o d -> d o"), [], [gck], nc_ok=True)
            B.dma('sync', gc[:, 2:3], da_sub_gain[0:1, :].rearrange("o d -> d o"), [], [gck], nc_ok=True)
            B.ts('vector', gc[:, 2:3], gc[:, 2:3], 1.0 - LAMBDA_INIT, None, ALU.mult, None, [gck], [gck])
            lt = [T("lamt%d" % i, 64) for i in range(4)]
            for i in range(4):
                B.dma('sync', lt[i][0], da_lam[i][0:1, :].partition_broadcast(128), [], [lt[i][1]])
            for pr in range(2):
                a_, ak_ = lt[2 * pr]; b_, bk_ = lt[2 * pr + 1]
                B.tt('vector', a_, a_, b_, ALU.mult, [ak_, bk_], [ak_])
                B.P.op('vector', lambda e, a_=a_, pr=pr: e.reduce_sum(out=gc[:, 5 + pr:6 + pr], in_=a_, axis=AX.X), [ak_], [gck])
            B.act(gc[:, 5:7], gc[:, 5:7], AF.Exp, [gck], [gck])
            B.tt('vector', gc[:, 3:4], gc[:, 5:6], gc[:, 6:7], ALU.subtract, [gck], [gck])
            B.ts('vector', gc[:, 4:5], gc[:, 3:4], LAMBDA_INIT, -1.0, ALU.add, ALU.mult, [gck], [gck])
            wo, wok = T("wo", 8 * 1024, BF16); wo3 = wo.rearrange("p (k n) -> p k n", n=1024)
            B.dma('gpsimd', wo3, da_w_o[0].rearrange("(k p) n -> p k n", p=128), [], [wok])
            gts = load_gate_tiles(1, 2 * D)
            hb, hbk = T("hb", 8 * 2304, BF16); hb3 = hb.rearrange("p (k t) -> p k t", t=2304)
            onT, onTk = T("onT", 8 * NL, BF16); onT3 = onT.rearrange("p (h t) -> p h t", t=NL)
            wq, wqk = T("wq", 1024, BF16); wk_, wkk = T("wk", 1024, BF16); wv, wvk = T("wv", 1024, BF16)
            wq3 = wq.rearrange("p (k n) -> p k n", n=128); wk3 = wk_.rearrange("p (k n) -> p k n", n=128); wv3 = wv.rearrange("p (k n) -> p k n", n=128)
            qn, qnk = T("qn", NL, BF16)
            kz = [T("kz%d" % c, 2304, BF16) for c in range(2)]
            for c in range(2):
                B.ms('vector', kz[c][0], 0.0, [kz[c][1]])
            Es2 = [[T("Es%d_%d" % (p_, c), 512) for c in range(2)] for p_ in range(2)]
            vv, vvk = T("vv", 18 * 128, BF16); vv3 = vv.rearrange("p (t e) -> p t e", e=128)
            Eb = [T("E%d" % i, 512, BF16) for i in range(3)]
            pn_bufs = [(T("qf%d" % z, 512), T("sq%d" % z, 512), T("rs%d" % z, 512), T("trp%d" % z, 512)) for z in range(2)]
            pn_cnt = [0]
            (sq, sqk), (rs, rsk) = pn_bufs[0][1], pn_bufs[0][2]
            c0, c0k = T("c0", 512); c1, c1k = T("c1", 512)
            xb = [(T("axt%d" % i, D), T("axn%d" % i, D)) for i in range(2)]
            hkeys = [('hT', i) for i in range(NT)]
            wqkv3 = da_w_qkv[0].rearrange("(k p) n -> p k n", p=128)

            def proj_norm(w3, wkey, col0, ncols, gcol, rope0, dst, dstk):
                (qf, qfk), (sq, sqk), (rs, rsk), (tr_, trk) = pn_bufs[pn_cnt[0] % 2]; pn_cnt[0] += 1
                ps, psk = B.bank()
                for k in range(8):
                    B.mm(ps[:, 0:ncols], w3[:, k, :], hb3[:, k, col0:col0 + ncols], k == 0, k == 7, [wkey, hbk], [psk])
                B.cp('scalar', qf[:, 0:ncols], ps[:, 0:ncols], [psk], [qfk])
                B.act(sq[:, 0:ncols], qf[:, 0:ncols], AF.Square, [qfk], [sqk])
                ps2, ps2k = B.bank()
                B.mm(ps2[:, 0:ncols], blk64, sq[:, 0:ncols], True, True, [blk64k, sqk], [ps2k])
                B.ts('vector', rs[:, 0:ncols], ps2[:, 0:ncols], 1e-6, None, ALU.add, None, [ps2k], [rsk])
                B.act(rs[:, 0:ncols], rs[:, 0:ncols], AF.Ln, [rsk], [rsk])
                B.act(rs[:, 0:ncols], rs[:, 0:ncols], AF.Exp, [rsk], [rsk], scale=-0.5)
                B.stt('vector', qf[:, 0:ncols], qf[:, 0:ncols], gcol, rs[:, 0:ncols], ALU.mult, ALU.mult, [qfk, gck, rsk], [qfk])
                if rope0 is None:
                    if isinstance(dst, list):
                        for (d_ap, d_k, rs_) in dst:
                            B.cp('vector', d_ap[rs_], qf[rs_, 0:ncols], [qfk], [d_k])
                    else:
                        B.cp('vector', dst, qf[:, 0:ncols], [qfk], [dstk])
                else:
                    ps3, ps3k = B.bank()
                    B.mm(ps3[:, 0:ncols], rot, qf[:, 0:ncols], True, True, [rotk, qfk], [ps3k])
                    B.tt('vector', tr_[:, 0:ncols], ps3[:, 0:ncols], sn[:, rope0:rope0 + ncols], ALU.mult, [ps3k, snk], [trk])
                    B.tt('gpsimd', sq[:, 0:ncols], qf[:, 0:ncols], cs[:, rope0:rope0 + ncols], ALU.mult, [qfk, csk, sqk], [sqk])
                    if isinstance(dst, list):
                        for (d_ap, d_k, rs_) in dst:
                            B.tt('vector', d_ap[rs_], sq[rs_, 0:ncols], tr_[rs_, 0:ncols], ALU.add, [sqk, trk], [d_k])
                    else:
                        B.tt('vector', dst, sq[:, 0:ncols], tr_[:, 0:ncols], ALU.add, [sqk, trk], [dstk])

            ecnt = 0
            for b in range(NB):
                B.dma('gpsimd', hb3[:, :, 0:NCX], hT3[:, :, TL + b * NCX:TL + (b + 1) * NCX], hkeys, [hbk])
                B.dma('gpsimd', hb3[:, :, NCX:2304], hT3[:, :, b * NL:(b + 1) * NL], hkeys, [hbk])
                for h in range(8):
                    B.dma('gpsimd', wq3, wqkv3[:, :, h * 128:(h + 1) * 128], [], [wqk])
                    B.dma('gpsimd', wk3, wqkv3[:, :, D + h * 128:D + (h + 1) * 128], [], [wkk])
                    B.dma('gpsimd', wv3, wqkv3[:, :, 2 * D + h * 128:2 * D + (h + 1) * 128], [], [wvk])
                    for qc in range(4):
                        proj_norm(wq3, wqk, NCX + qc * 512, 512, gc[:, 0:1], qc * 512, qn[:, qc * 512:(qc + 1) * 512], qnk)
                    def kdst(c0_, c1_):
                        return [(kz[c][0][:, c0_:c1_], kz[c][1], slice(c * 64, (c + 1) * 64)) for c in range(2)]
                    proj_norm(wk3, wkk, 0, NCX, gc[:, 1:2], None, kdst(0, NCX), None)
                    for qc in range(4):
                        proj_norm(wk3, wkk, NCX + qc * 512, 512, gc[:, 1:2], qc * 512, kdst(NCX + qc * 512, NCX + (qc + 1) * 512), None)
                    for kt in range(18):
                        ps, psk = B.bank()
                        for k in range(8):
                            B.mm(ps[:, 0:128], hb3[:, k, kt * 128:(kt + 1) * 128], wv3[:, k, :], k == 0, k == 7, [hbk, wvk], [psk])
                        B.cp('scalar' if kt % 2 == 0 else 'vector', vv3[:, kt, :], ps[:, 0:128], [psk], [vvk])
                    pending = None
                    for qc in range(4):
                        qs = slice(qc * 512, (qc + 1) * 512)
                        par = qc % 2
                        items = [(c, kt) for kt in range(18) for c in range(2)]
                        sc = {}

                        def score(ii, qs=qs):
                            c, kt = items[ii]
                            ps, psk = B.bank()
                            B.mm(ps, kz[c][0][:, kt * 128:(kt + 1) * 128], qn[:, qs], True, True, [kz[c][1], qnk], [psk])
                            sc[ii] = (ps, psk)

                        def make_combine(par=par, qs=qs, h=h):
                            def combine():
                                pd, pdk = pst[7][:], 'ps7'
                                for c, (cc, cck) in enumerate(((c0, c0k), (c1, c1k))):
                                    po, pok = pst[3 + 2 * par + c][:], 'ps%d' % (3 + 2 * par + c)
                                    Es_, Esk_ = Es2[par][c]
                                    B.mm(pd, ones, Es_, True, True, [onesk, Esk_], [pdk])
                                    B.act(cc, pd, AF.Ln, [pdk], [cck])
                                    B.act(cc, cc, AF.Exp, [cck], [cck], scale=-1.0)
                                    B.tt('vector', cc, cc, po, ALU.mult, [cck, pok], [cck])
                                B.stt('vector', c0, c1, gc[:, 4:5], c0, ALU.mult, ALU.add, [c1k, gck, c0k], [c0k])
                                B.act(sq, c0, AF.Square, [c0k], [sqk])
                                B.mm(pd, ones, sq, True, True, [onesk, sqk], [pdk])
                                B.ts('vector', rs, pd, 1.0 / 128, 1e-6, ALU.mult, ALU.add, [pdk], [rsk])
                                B.act(rs, rs, AF.Ln, [rsk], [rsk])
                                B.act(rs, rs, AF.Exp, [rsk], [rsk], scale=-0.5)
                                B.stt('vector', onT3[:, h, qs], c0, gc[:, 2:3], rs, ALU.mult, ALU.mult, [c0k, gck, rsk], [onTk])
                            return combine
                        score(0); score(1)
                        for ii, (c, kt) in enumerate(items):
                            if ii + 2 < len(items):
                                score(ii + 2)
                            if ii == 8 and pending is not None:
                                pending(); pending = None
                            po, pok = pst[3 + 2 * par + c][:], 'ps%d' % (3 + 2 * par + c)
                            Es_, Esk_ = Es2[par][c]
                            ps, psk = sc.pop(ii)
                            E_, Ek_ = Eb[ecnt % 3]; ecnt += 1
                            B.act(E_, ps, AF.Exp, [psk], [Ek_], scale=0.125)
                            B.mm(po, vv3[:, kt, :], E_, kt == 0, kt == 17, [vvk, Ek_], [pok])
                            eng_ = 'vector' if c == 0 else 'gpsimd'
                            if kt == 0:
                                B.cp(eng_, Es_, E_, [Ek_], [Esk_])
                            else:
                                B.tt(eng_, Es_, Es_, E_, ALU.add, [Ek_, Esk_], [Esk_])
                        pending = make_combine()
                    pending(); pending = None
                GT, GTk = gts[b]
                for tt_ in range(16):
                    i = b * 16 + tt_
                    (xt, xk), (xn, xnk) = xb[tt_ % 2]
                    B.dma('sync', xt, xa[i * 128:(i + 1) * 128, :], [xkeys[i]], [xk])
                    for nh in range(2):
                        cs_ = slice(nh * 512, (nh + 1) * 512)
                        ps, psk = B.bank()
                        for h in range(8):
                            B.mm(ps, onT3[:, h, tt_ * 128:(tt_ + 1) * 128], wo3[:, h, cs_], h == 0, h == 7, [onTk, wok], [psk])
                        B.tt('vector', xn[:, cs_], ps, GT[:, cs_], ALU.mult, [psk, GTk], [xnk])
                        B.tt('gpsimd', xn[:, cs_], xn[:, cs_], xt[:, cs_], ALU.add, [xnk, xk], [xnk])
                    B.dma('sync', xa[i * 128:(i + 1) * 128, :], xn, [xnk], [xkeys[i]])
            B.brange = (0, 8)

        phase_norm_T(0)
        if done('norm0'):
            P.emit(); return nc
        phase_s5_setup()
        if done('s5setup'):
            P.emit(); return nc
        phase_s5_main()
        if done('s5main'):
            P.emit(); return nc
        phase_glu()
        if done('glu'):
            P.emit(); return nc
        B.persist = persist_small
        phase_moe(0, NT, False)
        if done('moe0'):
            P.emit(); return nc
        phase_norm_T(1)
        phase_attn()
        if done('attn'):
            P.emit(); return nc
        phase_moe(1, 32, True)
        P.emit()
        print("nops", P.nops, {e: len(P.ops[e]) for e in ENGS})
    return nc


def make_consts():
    i = np.arange(128)
    k = {}
    k["k_ident"] = np.eye(128, dtype=np.float32)
    k["k_ltri"] = (i[:, None] < i[None, :]).astype(np.float32)
    k["k_ones"] = np.ones((128, 128), np.float32)
    k["k_blk16"] = (i[:, None] // 16 == i[None, :] // 16).astype(np.float32)
    k["k_g2m"] = (i[:, None] // 64 == np.arange(2)[None, :]).astype(np.float32)
    k["k_blk64"] = (i[:, None] // 64 == i[None, :] // 64).astype(np.float32) / 64.0
    rot = np.zeros((128, 128), np.float32)
    for c in range(2):
        for d in range(64):
            if d < 32:
                rot[c * 64 + d + 32, c * 64 + d] = -1.0
            else:
                rot[c * 64 + d - 32, c * 64 + d] = 1.0
    k["k_rot"] = rot
    tok = np.arange(NL)
    row = (tok // 64).astype(np.float32); col = (tok % 64).astype(np.float32)
    inv = np.exp(-math.log(10000.0) * np.arange(16, dtype=np.float32) / 16).astype(np.float32)
    ang = np.concatenate([row[:, None] * inv, col[:, None] * inv], axis=-1).astype(np.float32)
    f = (i % 64) % 32
    k["k_cos"] = np.ascontiguousarray(np.cos(ang).astype(np.float32)[:, f].T)
    k["k_sin"] = np.ascontiguousarray(np.sin(ang).astype(np.float32)[:, f].T)
    k["k_pm"] = (((i[:, None] // 16) % 2) == np.arange(2)[None, :]).astype(np.float32)
    k["k_slot"] = np.tile((np.arange(32, dtype=np.float32) * CAP)[None, :], (128, 1))
    return k


_NC_CACHE = {}


def kernel(**inputs):
    inp = {k: np.ascontiguousarray(np.asarray(v, dtype=np.float32)) for k, v in inputs.items()}
    if "nc" not in _NC_CACHE:
        _NC_CACHE["nc"] = build_nc()
    nc = _NC_CACHE["nc"]
    consts = make_consts()
    shared = {k: v for k, v in inp.items() if k not in ("x", "c", "ctx", "c_ctx")}
    for n in ("q1", "k1", "q2", "k2"):
        pass
    in_maps = []
    for core in range(8):
        m = dict(shared)
        m.update(consts)
        m["x"] = inp["x"][2 * core:2 * core + 2]
        m["ctx"] = inp["ctx"][2 * core:2 * core + 2]
        m["c"] = inp["c"][2 * core:2 * core + 2]
        m["c_ctx"] = inp["c_ctx"].reshape(1, D)
        in_maps.append(m)
    res = run_bass_kernel_spmd(nc, in_maps, core_ids=list(range(8)))
    _NC_CACHE["cnt"] = [r["cnt_out"][0] for r in res.results]
    return np.concatenate([r["out"] for r in res.results], axis=0).astype(np.float32)
```

```python
import numpy as np
from contextlib import ExitStack
import concourse.bass as bass
import concourse.mybir as mybir

F32 = mybir.dt.float32
F32R = mybir.dt.float32r
I32 = mybir.dt.int32
U32 = mybir.dt.uint32
ALU = mybir.AluOpType
AF = mybir.ActivationFunctionType
AX = mybir.AxisListType

ENGS = ['tensor', 'vector', 'scalar', 'gpsimd', 'sync']
DMA_SLOTS = {'sync': 12, 'gpsimd': 8, 'scalar': 6}


class Prog:
    def __init__(self, nc):
        self.nc = nc
        self.ops = {e: [] for e in ENGS}
        self.cnt = {e: 0 for e in ENGS}
        self.seen = {e: {} for e in ENGS}
        self.lastw = {}
        self.readers = {}
        self.dma_next = {q: 0 for q in DMA_SLOTS}
        self.dma_uses = {}
        self.dma_ep = {}
        self.epoch = {e: 0 for e in ENGS}
        self.used = set()
        self.nops = 0

    def _deps(self, eng, reads, writes):
        deps = {}

        def add(t):
            if t is None:
                return
            s, v = t
            if deps.get(s, 0) < v:
                deps[s] = v
        for r in reads:
            add(self.lastw.get(r))
        for w in writes:
            add(self.lastw.get(w))
            for s, v in self.readers.get(w, {}).items():
                add((s, v))
        waits = []
        for s, v in deps.items():
            if eng == 'tensor' and s[0] == 'tensor':
                continue
            if self.seen[eng].get(s, 0) >= v:
                continue
            self.seen[eng][s] = v
            waits.append((s, v))
        return waits

    def _update(self, tk, reads, writes):
        s, v = tk
        for w in writes:
            self.lastw[w] = tk
            self.readers[w] = {}
        for r in reads:
            d = self.readers.setdefault(r, {})
            if d.get(s, 0) < v:
                d[s] = v

    def op(self, eng, fn, reads=(), writes=()):
        waits = self._deps(eng, reads, writes)
        self.cnt[eng] += 1
        if self.cnt[eng] > 12000:
            self.epoch[eng] += 1
            self.cnt[eng] = 1
        ek = (eng, self.epoch[eng])
        self.used.add(ek)
        tk = (ek, self.cnt[eng])
        self.ops[eng].append((waits, fn, (ek, 1)))
        self._update(tk, reads, writes)
        self.nops += 1

    def dma(self, q, fn, reads=(), writes=()):
        waits = self._deps(q, reads, writes)
        n = DMA_SLOTS[q]
        slot = self.dma_next[q]
        self.dma_next[q] = (slot + 1) % n
        ep = self.dma_ep.get((q, slot), 0)
        if self.dma_uses.get(('d', q, slot, ep), 0) >= 700:
            ep += 1
            self.dma_ep[(q, slot)] = ep
            old = ('d', q, slot, ep - 1)
            if self.seen[q].get(old, 0) < 16 * 700:
                self.seen[q][old] = 16 * 700
                waits.append((old, 16 * 700))
        key = ('d', q, slot, ep)
        uses = self.dma_uses.get(key, 0)
        if uses > 0 and self.seen[q].get(key, 0) < 16 * uses:
            self.seen[q][key] = 16 * uses
            waits.append((key, 16 * uses))
        self.dma_uses[key] = uses + 1
        tk = (key, 16 * (uses + 1))
        self.ops[q].append((waits, fn, (key, 16)))
        self._update(tk, reads, writes)
        self.nops += 1

    def barrier(self):
        latest = {}
        for e in ENGS:
            if self.cnt[e] > 0:
                latest[(e, self.epoch[e])] = self.cnt[e]
        for key, uses in self.dma_uses.items():
            latest[key] = 16 * uses
        for e in ENGS:
            waits = []
            for s, v in latest.items():
                if self.seen[e].get(s, 0) < v:
                    self.seen[e][s] = v
                    waits.append((s, v))
            if waits:
                self.ops[e].append((waits, None, None))
        self.lastw.clear()
        self.readers.clear()

    def emit(self):
        nc = self.nc
        self.barrier()
        keys = sorted(self.used) + list(self.dma_uses.keys())
        with ExitStack() as st:
            semh = {}
            for i, k in enumerate(keys):
                semh[k] = st.enter_context(nc.semaphore("s%d" % i))
            block = st.enter_context(nc.Block())
            for e in ENGS:
                def body(engobj, e=e):
                    for waits, fn, inc in self.ops[e]:
                        for s, v in waits:
                            engobj.wait_ge(semh[s], v)
                        if fn is not None:
                            ins = fn(engobj)
                            ins.then_inc(semh[inc[0]], inc[1])
                getattr(block, e)(body)


class Arena:
    def __init__(self, big, ncols):
        self.big = big
        self.ncols = ncols
        self.off = 0
        self.gen = 0

    def reset(self):
        self.off = 0
        self.gen += 1

    def alloc(self, name, cols):
        cols = (cols + 1) // 2 * 2
        assert self.off + cols <= self.ncols, (name, self.off, cols, self.ncols)
        ap = self.big[:, self.off:self.off + cols]
        self.off += cols
        return ap, (name, self.gen)

BF16 = mybir.dt.bfloat16
from concourse.bass_utils import run_bass_kernel_spmd
import math

NB = 2; NL = 2048; NCX = 256; D = 1024
TL = NB * NL; TT = TL + NB * NCX
NT = TT // 128
CAPT = 8; CAP = CAPT * 128; NSLOT = 32 * CAP
HALF = CAP // 2
LAMBDA_INIT = 0.8 - 0.6 * math.exp(-0.3 * 1)
TWO_PI = 2.0 * math.pi


class Bld:
    def __init__(self, nc, P, big, pst, ncols):
        self.nc = nc; self.P = P; self.ar = Arena(big, ncols); self.pst = pst; self.pb = 0; self.brange = (0, 8)

    def T(self, name, cols, dt=F32):
        if dt == BF16:
            ap, k = self.ar.alloc(name, (cols + 1) // 2)
            return ap.bitcast(BF16)[:, 0:cols], k
        ap, k = self.ar.alloc(name, cols)
        if dt != F32:
            ap = ap.bitcast(dt)
        return ap, k

    def bank(self):
        lo, hi = self.brange
        i = lo + self.pb % (hi - lo); self.pb += 1
        return self.pst[i][:], 'ps%d' % i

    def mm(self, out, lhsT, rhs, start, stop, r, w):
        self.P.op('tensor', lambda e: e.matmul(out, lhsT=lhsT, rhs=rhs, start=start, stop=stop), r, w)

    def tr(self, out, in_, r, w):
        ident = self.ident
        self.P.op('tensor', lambda e: e.transpose(out, in_, ident), list(r) + [self.identk], w)

    def act(self, out, in_, func, r, w, **kw):
        self.P.op('scalar', lambda e: e.activation(out=out, in_=in_, func=func, **kw), r, w)

    def ts(self, eng, out, in0, s1, s2, op0, op1, r, w):
        if op1 is None:
            self.P.op(eng, lambda e: e.tensor_scalar(out=out, in0=in0, scalar1=s1, scalar2=None, op0=op0), r, w)
        else:
            self.P.op(eng, lambda e: e.tensor_scalar(out=out, in0=in0, scalar1=s1, scalar2=s2, op0=op0, op1=op1), r, w)

    def tt(self, eng, out, in0, in1, op, r, w):
        self.P.op(eng, lambda e: e.tensor_tensor(out=out, in0=in0, in1=in1, op=op), r, w)

    def stt(self, eng, out, in0, scalar, in1, op0, op1, r, w):
        self.P.op(eng, lambda e: e.scalar_tensor_tensor(out=out, in0=in0, scalar=scalar, in1=in1, op0=op0, op1=op1), r, w)

    def cp(self, eng, out, in_, r, w):
        if eng == 'scalar':
            self.P.op(eng, lambda e: e.copy(out=out, in_=in_), r, w)
        else:
            self.P.op(eng, lambda e: e.tensor_copy(out=out, in_=in_), r, w)

    def ms(self, eng, out, val, w):
        self.P.op(eng, lambda e: e.memset(out, val), (), w)

    def dma(self, q, out, in_, r, w, nc_ok=False):
        if nc_ok:
            self.P.dma(q, lambda e: e.dma_start(out=out, in_=in_, allow_slow_non_contiguous=True), r, w)
        else:
            self.P.dma(q, lambda e: e.dma_start(out=out, in_=in_), r, w)

    def phase(self):
        self.P.barrier()
        self.ar.reset()
        self.ar.off = self.persist


def tile_row(i):
    return i // 16 if i < 32 else 2


def run_pipeline(gens):
    active = []
    for g in gens:
        for a in list(active):
            if next(a, 'done') == 'done':
                active.remove(a)
        active.append(g)
        if next(g, 'done') == 'done':
            active.remove(g)
    while active:
        for a in list(active):
            if next(a, 'done') == 'done':
                active.remove(a)


def build_nc(stop_after=None, debug=False):
    nc = bass.Bass("TRN2", target_bir_lowering=False)

    def din(name, shape, dt=F32):
        return nc.dram_tensor(name, shape, dt, kind="ExternalInput").ap()

    def dscr(name, shape, dt=F32):
        return nc.dram_tensor(name, shape, dt, kind=("ExternalOutput" if debug else "Internal")).ap()

    x_in = din("x", [NB, NL, D]); ctx_in = din("ctx", [NB, NCX, D]); c_in = din("c", [NB, D]); cc_in = din("c_ctx", [1, D])
    w_ada = din("w_ada", [2, D, 6 * D]); b_ada = din("b_ada", [2, 6 * D]); g_mix = din("g_mix", [2, D]); g_ffn = din("g_ffn", [2, D])
    s5_a_re = din("s5_a_re", [1, 2, 64, 64]); s5_a_im = din("s5_a_im", [1, 2, 64, 64]); s5_log_dt = din("s5_log_dt", [1, 2, 64])
    s5_b_re = din("s5_b_re", [1, 2, 64, 64, 16]); s5_b_im = din("s5_b_im", [1, 2, 64, 64, 16])
    s5_c_re = din("s5_c_re", [1, 2, 64, 16, 64]); s5_c_im = din("s5_c_im", [1, 2, 64, 16, 64])
    s5_d = din("s5_d", [1, D]); s5_w_glu = din("s5_w_glu", [1, D, 2 * D]); s5_b_glu = din("s5_b_glu", [1, 2 * D])
    da_w_qkv = din("da_w_qkv", [1, D, 3 * D]); da_w_o = din("da_w_o", [1, D, D])
    da_q_gain = din("da_q_gain", [1, 64]); da_k_gain = din("da_k_gain", [1, 64])
    da_lam = [din("da_lam_" + n, [1, 64]) for n in ("q1", "k1", "q2", "k2")]
    da_sub_gain = din("da_sub_gain", [1, 128])
    moe_w_router = din("moe_w_router", [2, D, 32]); moe_b_router = din("moe_b_router", [2, 32])
    moe_w_gu = din("moe_w_gu", [2, 32, D, 2 * D]); moe_b_gu = din("moe_b_gu", [2, 32, 2 * D])
    moe_w_down = din("moe_w_down", [2, 32, D, D]); moe_b_down = din("moe_b_down", [2, 32, D])
    k_ident = din("k_ident", [128, 128]); k_ltri = din("k_ltri", [128, 128]); k_ones = din("k_ones", [128, 128])
    k_blk16 = din("k_blk16", [128, 128]); k_g2m = din("k_g2m", [128, 2]); k_blk64 = din("k_blk64", [128, 128])
    k_rot = din("k_rot", [128, 128]); k_cos = din("k_cos", [128, NL]); k_sin = din("k_sin", [128, NL])
    k_slot = din("k_slot", [128, 32]); k_pm = din("k_pm", [128, 2]); k_trash = din("k_trash", [128, 1])
    out = nc.dram_tensor("out", [NB, NL, D], F32, kind="ExternalOutput").ap()
    cnt_out = nc.dram_tensor("cnt_out", [128, 64], F32, kind="ExternalOutput").ap()
    xa = dscr("xa", [TT, D]); mod = dscr("mod", [2, 3, 6 * D]); hT = dscr("hT", [D, TT]); sT = dscr("sT", [D, TT], BF16)
    Xs = dscr("Xs", [NSLOT + 128, D], BF16); Ys = dscr("Ys", [NSLOT + 128, D], BF16)
    Wblk_d = dscr("Wblk_d", [2, 128, 8, 8, 128], BF16); Bst_d = dscr("Bst_d", [2, 128, 8, 8, 2, 128], BF16)
    Cst_d = dscr("Cst_d", [2, 128, 8, 2, 32, 32], BF16)
    outf = out.rearrange("b n d -> (b n) d")

    with ExitStack() as st:
        NCOLS = 53000
        big_t = st.enter_context(nc.sbuf_tensor("big", [128, NCOLS], F32))
        pst = [st.enter_context(nc.psum_tensor("ps%d" % i, [128, 512], F32)) for i in range(8)]
        P = Prog(nc)
        B = Bld(nc, P, big_t[:], pst, NCOLS)
        T = B.T
        B.ident, B.identk = T("ident", 128)
        B.dma('sync', B.ident, k_ident, [], [B.identk])
        ltri, ltrik = T("ltri", 128); B.dma('sync', ltri, k_ltri, [], [ltrik])
        ones, onesk = T("ones", 128); B.dma('sync', ones, k_ones, [], [onesk])
        onesb, onesbk = T("onesb", 128, BF16); B.cp('vector', onesb, ones, [onesk], [onesbk])
        identb, identbk = T("identb", 128, BF16); B.cp('vector', identb, B.ident, [B.identk], [identbk])
        blk16, blk16k = T("blk16", 128); B.dma('sync', blk16, k_blk16, [], [blk16k])
        g2m, g2mk = T("g2m", 2); B.dma('sync', g2m, k_g2m, [], [g2mk])
        DEST, DESTk = T("DEST", NT * 4, I32); GATE, GATEk = T("GATE", NT * 4)
        DEST3 = DEST.rearrange("p (i k) -> p i k", k=4); GATE3 = GATE.rearrange("p (i k) -> p i k", k=4)
        persist_small = B.ar.off
        CA, CAk = T("CA", 2 * 8 * 2 * 64)
        CA5 = CA.rearrange("p (d i r g) -> p d i r g", d=2, i=8, r=2)
        CBn, CBnk = T("CBn", 2 * 8 * 64); CBn4 = CBn.rearrange("p (d i g) -> p d i g", d=2, i=8)
        CBp, CBpk = T("CBp", 2 * 8 * 64); CBp4 = CBp.rearrange("p (d i g) -> p d i g", d=2, i=8)
        B.persist = B.ar.off
        xkeys = [('xa', i) for i in range(NT)]

        def done(name):
            return stop_after == name

        for b in range(NB):
            B.dma('sync', xa[b * NL:(b + 1) * NL, :], x_in[b], [], xkeys[b * 16:(b + 1) * 16])
            B.dma('sync', xa[TL + b * NCX:TL + (b + 1) * NCX, :], ctx_in[b], [], xkeys[32 + 2 * b:34 + 2 * b])

        cT, cTk = T("cT", 24)
        cT3 = cT.rearrange("p (k r) -> p k r", r=3)
        for r in range(3):
            src = (c_in[r] if r < 2 else cc_in[0]).rearrange("(k p) -> p k", p=128)
            B.dma('sync', cT3[:, :, r], src, [], [cTk], nc_ok=True)
        B.act(cT, cT, AF.Silu, [cTk], [cTk])
        bt, btk = T("bt", 6 * D); modt, modtk = T("modt", 6 * D)
        wts = [T("wada%d" % i, 8 * 512) for i in range(2)]
        for l in range(2):
            B.dma('sync', bt[0:3, :], b_ada[l:l + 1, :].partition_broadcast(3), [modtk], [btk])
            for n in range(12):
                wt, wtk = wts[n % 2]
                wt3 = wt.rearrange("p (k n) -> p k n", n=512)
                B.dma('sync' if n % 2 == 0 else 'scalar', wt3, w_ada[l][:, n * 512:(n + 1) * 512].rearrange("(k p) n -> p k n", p=128), [], [wtk])
                ps, psk = B.bank()
                for k in range(8):
                    B.mm(ps[0:3, :], cT3[:, k, :], wt3[:, k, :], k == 0, k == 7, [cTk, wtk], [psk])
                B.tt('vector', modt[0:3, n * 512:(n + 1) * 512], ps[0:3, :], bt[0:3, n * 512:(n + 1) * 512], ALU.add, [psk, btk], [modtk])
            B.dma('sync', mod[l], modt[0:3, :], [modtk], [('mod', l)])
        if done('ada'):
            P.emit(); return nc

        def load_mod_tiles(l, gvec, off_sh, off_sc):
            gb, gbk = T("gvecb", D)
            B.dma('sync', gb, gvec[l:l + 1, :].partition_broadcast(128), [], [gbk])
            res = []
            for r in range(3):
                G, Gk = T("G%d" % r, D); S, Sk = T("S%d" % r, D)
                B.dma('sync', G, mod[l, r:r + 1, off_sc:off_sc + D].partition_broadcast(128), [('mod', l)], [Gk])
                B.dma('scalar', S, mod[l, r:r + 1, off_sh:off_sh + D].partition_broadcast(128), [('mod', l)], [Sk])
                B.stt('vector', G, G, 1.0, gb, ALU.add, ALU.mult, [Gk, gbk], [Gk])
                res.append((G, Gk, S, Sk))
            return res

        def load_gate_tiles(l, off):
            res = []
            for r in range(3):
                G, Gk = T("GT%d" % r, D)
                B.dma('sync', G, mod[l, r:r + 1, off:off + D].partition_broadcast(128), [('mod', l)], [Gk])
                res.append((G, Gk))
            return res

        def norm_tile(xt, xk, h, hk, mt, ss, ssk):
            G, Gk, S, Sk = mt
            B.ms('gpsimd', ss, 0.0, [ssk])
            B.act(h, xt, AF.Square, [xk, ssk], [hk, ssk], accum_out=ss[:, 0:1])
            B.ts('vector', ss[:, 1:2], ss[:, 0:1], 1.0 / D, 1e-6, ALU.mult, ALU.add, [ssk], [ssk])
            B.act(ss[:, 1:2], ss[:, 1:2], AF.Sqrt, [ssk], [ssk])
            B.P.op('vector', lambda e: e.reciprocal(out=ss[:, 1:2], in_=ss[:, 1:2]), [ssk], [ssk])
            B.stt('vector', h, xt, ss[:, 1:2], G, ALU.mult, ALU.mult, [xk, ssk, Gk, hk], [hk])
            B.tt('gpsimd', h, h, S, ALU.add, [hk, Sk], [hk])

        def norm_tile_g(xt, xk, h, hk, mt, ss, ssk):
            G, Gk, S, Sk = mt
            B.ms('gpsimd', ss, 0.0, [ssk])
            B.act(h, xt, AF.Square, [xk, ssk], [hk, ssk], accum_out=ss[:, 0:1])
            yield
            B.ts('vector', ss[:, 1:2], ss[:, 0:1], 1.0 / D, 1e-6, ALU.mult, ALU.add, [ssk], [ssk])
            B.act(ss[:, 1:2], ss[:, 1:2], AF.Sqrt, [ssk], [ssk])
            yield
            B.P.op('vector', lambda e: e.reciprocal(out=ss[:, 1:2], in_=ss[:, 1:2]), [ssk], [ssk])
            B.stt('vector', h, xt, ss[:, 1:2], G, ALU.mult, ALU.mult, [xk, ssk, Gk, hk], [hk])
            B.tt('gpsimd', h, h, S, ALU.add, [hk, Sk], [hk])
            yield

        def transpose_tile(h, hk, dst3, dstk):
            for half in range(2):
                ps, psk = B.bank()
                for kk in range(4):
                    k = half * 4 + kk
                    B.tr(ps[:, kk * 128:(kk + 1) * 128], h[:, k * 128:(k + 1) * 128], [hk], [psk])
                B.cp('scalar' if half == 0 else 'vector', dst3[:, half * 4:(half + 1) * 4, :], ps.rearrange("p (k t) -> p k t", t=128), [psk], [dstk])

        hT3 = hT.rearrange("(k p) t -> p k t", p=128)
        sT3 = sT.rearrange("(k p) t -> p k t", p=128)

        def phase_norm_T(l):
            B.phase()
            mts = load_mod_tiles(l, g_mix, 0, D)
            bufs = [(T("xt%d" % i, D), T("h%d" % i, D), T("hTt%d" % i, D), T("ss%d" % i, 2)) for i in range(5)]

            def tile_gen(i):
                (xt, xk), (h, hk), (ht, htk), (ss, ssk) = bufs[i % 5]
                B.dma('sync', xt, xa[i * 128:(i + 1) * 128, :], [xkeys[i]], [xk])
                yield
                for _ in norm_tile_g(xt, xk, h, hk, mts[tile_row(i)], ss, ssk):
                    yield
                ht3 = ht.rearrange("p (k t) -> p k t", t=128)
                transpose_tile(h, hk, ht3, htk)
                B.dma('scalar', hT3[:, :, i * 128:(i + 1) * 128], ht3, [htk], [('hT', i)])
            run_pipeline([tile_gen(i) for i in range(NT)])

        def bc3(ap2):
            return ap2.unsqueeze(2).to_broadcast([128, 32, 16])

        def phase_s5_setup():
            B.phase()
            dcol, dcolk = T("dcol", 8)
            B.dma('sync', dcol, s5_d[0].rearrange("(j p) -> p j", p=128), [], [dcolk], nc_ok=True)
            Wsb, Wsbk = T("Wsb", 8 * 8 * 128, BF16); Wsb4 = Wsb.rearrange("p (j e m) -> p j e m", j=8, e=8)
            Bsb, Bsbk = T("Bsb", 8 * 8 * 2 * 128, BF16); Bsb5 = Bsb.rearrange("p (j s r m) -> p j s r m", j=8, s=8, r=2)
            Csb, Csbk = T("Csb", 8 * 2 * 32 * 32, BF16); Csb5 = Csb.rearrange("p (t r g m) -> p t r g m", t=8, r=2, g=32)
            Are, Arek = T("Are", 32); Aim, Aimk = T("Aim", 32); DT, DTk = T("DT", 32)
            lr, lrk = T("lr", 32); li, lik = T("li", 32)
            LP, LPk = T("LP", 9 * 2 * 32); LP4 = LP.rearrange("p (e r g) -> p e r g", e=9, r=2)
            sm = [T("sm%d" % i, 32) for i in range(6)]
            ki, kik = T("ki", 32, I32)
            Bre, Brek = T("Bre", 512); Bim, Bimk = T("Bim", 512); Cre, Crek = T("Cre", 512); Cim, Cimk = T("Cim", 512)
            bbr, bbrk = T("bbr", 512); bbi, bbik = T("bbi", 512)
            t1, t1k = T("t1", 512); t2, t2k = T("t2", 512); Ere, Erek = T("Ere", 512); Eim, Eimk = T("Eim", 512)
            Cmr, Cmrk = T("Cmr", 1024); Cmi, Cmik = T("Cmi", 1024); Emr, Emrk = T("Emr", 1024); Emi, Emik = T("Emi", 1024)
            v3 = lambda a: a.rearrange("p (g c) -> p g c", c=16)
            v4 = lambda a: a.rearrange("p (g h c) -> p g h c", h=2, c=16)
            Cns = [T("Cn%d" % z, 8 * 64) for z in range(2)]
            Cxs = [T("Cx%d" % z, 128) for z in range(2)]; Cts = [T("Ct%d" % z, 128) for z in range(2)]
            pm, pmk = T("pm", 2); B.dma('sync', pm, k_pm, [], [pmk])
            for d in range(2):
                B.dma('sync', Are, s5_a_re[0, d].rearrange("(gp g2) p -> (g2 p) gp", g2=2), [], [Arek], nc_ok=True)
                B.dma('sync', Aim, s5_a_im[0, d].rearrange("(gp g2) p -> (g2 p) gp", g2=2), [], [Aimk], nc_ok=True)
                for g2 in range(2):
                    B.dma('sync', DT[g2 * 64:(g2 + 1) * 64, :], s5_log_dt[0, d:d + 1, :].rearrange("o (gp g2) -> o gp g2", g2=2)[:, :, g2].partition_broadcast(64), [], [DTk], nc_ok=True)
                B.dma('sync', v3(Bre), s5_b_re[0, d].rearrange("(gp g2) p c -> (g2 p) gp c", g2=2), [], [Brek], nc_ok=True)
                B.dma('scalar', v3(Bim), s5_b_im[0, d].rearrange("(gp g2) p c -> (g2 p) gp c", g2=2), [], [Bimk], nc_ok=True)
                for ci, (src, dst, dk) in enumerate(((s5_c_re, Cre, Crek), (s5_c_im, Cim, Cimk))):
                    Cn, Cnk = Cns[ci]
                    Cn3 = Cn.rearrange("p (j q) -> p j q", q=64)
                    B.dma('sync' if ci == 0 else 'scalar', Cn3, src[0, d].rearrange("(j g) c p -> (g c) j p", g=8), [], [Cnk])
                    for j in range(8):
                        Cx, Cxk = Cxs[j % 2]; Ct, Ctk = Cts[j % 2]
                        Cx3 = Cx.rearrange("p (h q) -> p h q", q=64)
                        for h in range(2):
                            B.ts('vector' if h == 0 else 'gpsimd', Cx3[:, h, :], Cn3[:, j, :], pm[:, h:h + 1], None, ALU.mult, None, [Cnk, pmk], [Cxk])
                        ps, psk = B.bank()
                        B.tr(ps[:, 0:128], Cx, [Cxk], [psk])
                        B.cp('scalar', Ct, ps[:, 0:128], [psk], [Ctk])
                        Ct4 = Ct.rearrange("p (q h c) -> p q h c", q=4, h=2)
                        B.tt('vector', v3(dst)[:, 4 * j:4 * j + 4, :], Ct4[:, :, 0, :], Ct4[:, :, 1, :], ALU.add, [Ctk], [dk])
                B.act(DT, DT, AF.Exp, [DTk], [DTk])
                B.tt('vector', lr, Are, DT, ALU.mult, [Arek, DTk], [lrk])
                B.tt('vector', li, Aim, DT, ALU.mult, [Aimk, DTk], [lik])
                B.ms('vector', LP4[:, 0, 0, :], 1.0, [LPk]); B.ms('vector', LP4[:, 0, 1, :], 0.0, [LPk])
                (mag, magk), (tq, tqk), (kf, kfk), (yy, yyk), (sn, snk), (den, denk) = sm
                for e in range(1, 9):
                    B.act(mag, lr, AF.Exp, [lrk], [magk], scale=float(e))
                    for which, off in ((1, 0.0), (0, 0.25)):
                        B.ts('vector', tq, li, e / TWO_PI, off, ALU.mult, ALU.add, [lik], [tqk])
                        B.cp('vector', ki, tq, [tqk], [kik])
                        B.cp('vector', kf, ki, [kik], [kfk])
                        B.ts('vector', kf, kf, -TWO_PI, off * TWO_PI, ALU.mult, ALU.add, [kfk], [kfk])
                        B.stt('vector', yy, li, float(e), kf, ALU.mult, ALU.add, [lik, kfk], [yyk])
                        B.act(sn, yy, AF.Sin, [yyk], [snk])
                        B.tt('vector', LP4[:, e, which, :], mag, sn, ALU.mult, [magk, snk], [LPk])
                for i8 in range(8):
                    e8 = 8 * (i8 + 1)
                    pr = []
                    B.act(mag, lr, AF.Exp, [lrk], [magk], scale=float(e8))
                    for which, off in ((1, 0.0), (0, 0.25)):
                        B.ts('vector', tq, li, e8 / TWO_PI, off, ALU.mult, ALU.add, [lik], [tqk])
                        B.cp('vector', ki, tq, [tqk], [kik])
                        B.cp('vector', kf, ki, [kik], [kfk])
                        B.ts('vector', kf, kf, -TWO_PI, off * TWO_PI, ALU.mult, ALU.add, [kfk], [kfk])
                        B.stt('vector', yy, li, float(e8), kf, ALU.mult, ALU.add, [lik, kfk], [yyk])
                        B.act(sn, yy, AF.Sin, [yyk], [snk])
                        if which == 1:
                            B.tt('vector', den, mag, sn, ALU.mult, [magk, snk], [denk])
                        else:
                            B.tt('vector', tq, mag, sn, ALU.mult, [magk, snk], [tqk])
                    g2v = lambda a: a.rearrange("p (g b) -> p g b", b=2)
                    reb = tq.unsqueeze(2).to_broadcast([128, 32, 2]); imb = den.unsqueeze(2).to_broadcast([128, 32, 2])
                    for r_ in range(2):
                        B.cp('vector', g2v(CA5[:, d, i8, r_, :]), reb, [tqk], [CAk])
                    B.cp('vector', g2v(CBp4[:, d, i8, :]), imb, [denk], [CBpk])
                    B.ts('vector', g2v(CBn4[:, d, i8, :]), imb, -1.0, None, ALU.mult, None, [denk], [CBnk])
                nr, nrk = sm[0]; qr, qrk = sm[1]; qi, qik = sm[2]; ta, tak = sm[3]; tb, tbk = sm[4]
                B.ts('vector', nr, LP4[:, 1, 0, :], -1.0, None, ALU.add, None, [LPk], [nrk])
                ni = LP4[:, 1, 1, :]
                B.tt('vector', den, Are, Are, ALU.mult, [Arek], [denk])
                B.tt('vector', ta, Aim, Aim, ALU.mult, [Aimk], [tak])
                B.tt('vector', den, den, ta, ALU.add, [denk, tak], [denk])
                B.P.op('vector', lambda e: e.reciprocal(out=den, in_=den), [denk], [denk])
                B.tt('vector', ta, nr, Are, ALU.mult, [nrk, Arek], [tak])
                B.tt('vector', tb, ni, Aim, ALU.mult, [LPk, Aimk], [tbk])
                B.tt('vector', ta, ta, tb, ALU.add, [tak, tbk], [tak])
                B.tt('vector', qr, ta, den, ALU.mult, [tak, denk], [qrk])
                B.tt('vector', ta, ni, Are, ALU.mult, [LPk, Arek], [tak])
                B.tt('vector', tb, nr, Aim, ALU.mult, [nrk, Aimk], [tbk])
                B.tt('vector', ta, ta, tb, ALU.subtract, [tak, tbk], [tak])
                B.tt('vector', qi, ta, den, ALU.mult, [tak, denk], [qik])

                def cmul(outr, outrk, outi, outik, ar_, ark, ai_, aik, br_, brk, bi_, bik):
                    B.tt('vector', v3(t1), v3(ar_), br_, ALU.mult, [ark, brk], [t1k])
                    B.tt('gpsimd', v3(t2), v3(ai_), bi_, ALU.mult, [aik, bik], [t2k])
                    B.tt('vector', outr, t1, t2, ALU.subtract, [t1k, t2k], [outrk])
                    B.tt('vector', v3(t1), v3(ar_), bi_, ALU.mult, [ark, bik, outrk], [t1k])
                    B.tt('gpsimd', v3(t2), v3(ai_), br_, ALU.mult, [aik, brk, outrk], [t2k])
                    B.tt('vector', outi, t1, t2, ALU.add, [t1k, t2k], [outik])
                cmul(bbr, bbrk, bbi, bbik, Bre, Brek, Bim, Bimk, bc3(qr), qrk, bc3(qi), qik)
                for h in range(2):
                    B.ts('vector', v4(Cmr)[:, :, h, :], v3(Cre), g2m[:, h:h + 1], None, ALU.mult, None, [Crek, g2mk], [Cmrk])
                    B.ts('vector', v4(Cmi)[:, :, h, :], v3(Cim), g2m[:, h:h + 1], -1.0, ALU.mult, ALU.mult, [Cimk, g2mk], [Cmik])
                for e in range(9):
                    Lr_b = bc3(LP4[:, e, 0, :]); Li_b = bc3(LP4[:, e, 1, :])
                    if e <= 7:
                        cmul(Ere, Erek, Eim, Eimk, bbr, bbrk, bbi, bbik, Lr_b, LPk, Li_b, LPk)
                        for h in range(2):
                            B.ts('vector', v4(Emr)[:, :, h, :], v3(Ere), g2m[:, h:h + 1], None, ALU.mult, None, [Erek, g2mk], [Emrk])
                            B.ts('gpsimd', v4(Emi)[:, :, h, :], v3(Eim), g2m[:, h:h + 1], None, ALU.mult, None, [Eimk, g2mk], [Emik])
                        s_idx = (7 - e) if d == 0 else e
                        for j in range(8):
                            sl = slice(j * 128, (j + 1) * 128)
                            for ri, (Em, Emk) in enumerate(((Emr, Emrk), (Emi, Emik))):
                                ps, psk = B.bank()
                                B.tr(ps[:, 0:128], Em[:, sl], [Emk], [psk])
                                B.cp('scalar', Bsb5[:, j, s_idx, ri, :], ps[:, 0:128], [psk], [Bsbk])
                            ps, psk = B.bank()
                            B.mm(ps[:, 0:128], Emr[:, sl], Cmr[:, sl], True, False, [Emrk, Cmrk], [psk])
                            B.mm(ps[:, 0:128], Emi[:, sl], Cmi[:, sl], False, True, [Emik, Cmik], [psk])
                            B.tt('vector', Wsb4[:, j, e, :], ps[:, 0:128], blk16, ALU.mult, [psk, blk16k], [Wsbk])
                            if d == 0 and e == 0:
                                B.stt('vector', Wsb4[:, j, 0, :], B.ident, dcol[:, j:j + 1], Wsb4[:, j, 0, :], ALU.mult, ALU.add, [B.identk, dcolk, Wsbk], [Wsbk])
                    if e >= 1:
                        cmul(Ere, Erek, Eim, Eimk, Cre, Crek, Cim, Cimk, Lr_b, LPk, Li_b, LPk)
                        t_idx = (e - 1) if d == 0 else (8 - e)
                        for h in range(2):
                            B.ts('vector', Csb5[:, t_idx, 0, :, h * 16:(h + 1) * 16], v3(Ere), g2m[:, h:h + 1], None, ALU.mult, None, [Erek, g2mk], [Csbk])
                            B.ts('vector', Csb5[:, t_idx, 1, :, h * 16:(h + 1) * 16], v3(Eim), g2m[:, h:h + 1], -1.0, ALU.mult, ALU.mult, [Eimk, g2mk], [Csbk])
                B.dma('sync', Wblk_d[d], Wsb4, [Wsbk], [('Wblk', d)])
                B.dma('sync', Bst_d[d], Bsb5, [Bsbk], [('Bst', d)])
                B.dma('sync', Cst_d[d], Csb5, [Csbk], [('Cst', d)])

        def phase_s5_main():
            B.phase()
            NBUF = 2
            hjs = [T("hj%d" % z, NB * 2304, BF16) for z in range(NBUF)]
            Wjs = [T("Wj%d" % z, 2 * 8 * 128, BF16) for z in range(NBUF)]
            Bjs = [T("Bj%d" % z, 2 * 8 * 2 * 128, BF16) for z in range(NBUF)]
            Cjs = [T("Cj%d" % z, 2 * 8 * 2 * 4 * 32, BF16) for z in range(NBUF)]
            Bjzs = [T("Bjz%d" % z, 2 * 8 * 2 * 128, BF16) for z in range(NBUF)]
            Cjzs = [T("Cjz%d" % z, 2 * 8 * 2 * 64, BF16) for z in range(NBUF)]
            Hbs = [[T("Hb%d_%d" % (z, d), 16 * 288, BF16) for d in range(2)] for z in range(NBUF)]
            Hl = [T("Hl%d" % d, 16 * 288) for d in range(2)]
            v5 = lambda a_, k: a_.rearrange("p (r q b k) -> p r q b k", r=2, q=4, b=2, k=k)
            Sa = [T("Sa%d" % d, 576) for d in range(2)]; Si = [T("Si%d" % d, 576) for d in range(2)]
            tA = [T("tA%d" % d, 576) for d in range(2)]; tB = [T("tB%d" % d, 576) for d in range(2)]
            vA = lambda a_: a_.rearrange("p (r g k i) -> p r g k i", r=2, g=8, i=8)
            vS = lambda a_: a_.rearrange("p (r g k) -> p r g k", r=2, g=8)
            ysb, ysbk = T("ysb", NB * 2304, BF16)
            ysb3 = ysb.rearrange("p (b t) -> p b t", b=NB); ysb4 = ysb.rearrange("p (b k s) -> p b k s", b=NB, s=8)
            yas = [(T("ya%d" % z, 288), T("yb%d" % z, 288)) for z in range(2)]
            engs = ['vector', 'gpsimd']
            hkeys = [('hT', i) for i in range(NT)]

            def views(j):
                z = j % NBUF
                hj, hjk = hjs[z]; Wj, Wjk = Wjs[z]; Bj, Bjk = Bjs[z]; Cj, Cjk = Cjs[z]; Bjz, Bjzk = Bjzs[z]; Cjz, Cjzk = Cjzs[z]
                return dict(hj=hj, hjk=hjk, hj3=hj.rearrange("p (b t) -> p b t", b=NB), hj4=hj.rearrange("p (b k s) -> p b k s", b=NB, s=8),
                            Wj4=Wj.rearrange("p (d e m) -> p d e m", d=2, e=8), Wjk=Wjk,
                            Bj=Bj, Bj5=Bj.rearrange("p (d s r m) -> p d s r m", d=2, s=8, r=2), Bjk=Bjk,
                            Cj6=Cj.rearrange("p (d t r q m) -> p d t r q m", d=2, t=8, r=2, q=4), Cjk=Cjk,
                            Bjz=Bjz, Bjz5=Bjz.rearrange("p (d s r m) -> p d s r m", d=2, s=8, r=2), Bjzk=Bjzk,
                            Cjz=Cjz, Cjz5=Cjz.rearrange("p (d t r m) -> p d t r m", d=2, t=8, r=2), Cjzk=Cjzk, Hb=Hbs[z])

            def load(j):
                v = views(j); rows = slice(j * 128, (j + 1) * 128)
                for b in range(NB):
                    B.dma('gpsimd', v['hj3'][:, b, 0:NCX], hT[rows, TL + b * NCX:TL + (b + 1) * NCX], hkeys, [v['hjk']])
                    B.dma('gpsimd', v['hj3'][:, b, NCX:2304], hT[rows, b * NL:(b + 1) * NL], hkeys, [v['hjk']])
                for d in range(2):
                    B.dma('sync', v['Wj4'][:, d], Wblk_d[d, :, j], [('Wblk', d)], [v['Wjk']])
                    B.dma('sync', v['Bj5'][:, d], Bst_d[d, :, j], [('Bst', d)], [v['Bjk']])
                    B.dma('sync', v['Cj6'][:, d], Cst_d[d, :, :, :, 4 * j:4 * j + 4, :], [('Cst', d)], [v['Cjk']])
                B.cp('vector', v['Bjz'][64:128, :], v['Bj'][64:128, :], [v['Bjk']], [v['Bjzk']])
                B.ms('vector', v['Bjz'][64:96, :], 0.0, [v['Bjzk']])
                B.ms('gpsimd', v['Cjz'], 0.0, [v['Cjzk']])
                for d in range(2):
                    B.cp('gpsimd', v['Cjz5'][:, d, :, :, 32:64], v['Cj6'][:, d, :, :, 3, :], [v['Cjk'], v['Cjzk']], [v['Cjzk']])

            def state(j):
                v = views(j)
                for d in range(2):
                    Hl5 = v5(Hl[d][0], 288)
                    for ri in range(2):
                        for q in range(4):
                            for b in range(NB):
                                ps, psk = B.bank()
                                for s_ in range(8):
                                    if q < 3:
                                        B.mm(ps[:, 0:288], v['Bj5'][32 * q:32 * q + 32, d, s_, ri, :], v['hj4'][32 * q:32 * q + 32, b, :, s_], s_ == 0, s_ == 7, [v['Bjk'], v['hjk']], [psk])
                                    else:
                                        B.mm(ps[:, 0:288], v['Bjz5'][64:128, d, s_, ri, :], v['hj4'][64:128, b, :, s_], s_ == 0, s_ == 7, [v['Bjzk'], v['hjk']], [psk])
                                B.cp('scalar', Hl5[:, ri, q, b, :], ps[:, 0:288], [psk], [Hl[d][1]])

            def chain(j):
                v = views(j)

                def cm(d, src, i8, n):
                    eng = engs[d]
                    gsl = slice(8 * j, 8 * j + 8)
                    LAb = CA5[:, d, i8, :, gsl].unsqueeze(3).to_broadcast([128, 2, 8, n])
                    LNb = CBn4[:, d, i8, gsl].unsqueeze(2).to_broadcast([128, 8, n])
                    LPb = CBp4[:, d, i8, gsl].unsqueeze(2).to_broadcast([128, 8, n])
                    t1 = tA[d][0][:, 0:16 * n].rearrange("p (r g k) -> p r g k", r=2, g=8); t1k = tA[d][1]
                    t2 = tB[d][0][:, 0:16 * n].rearrange("p (r g k) -> p r g k", r=2, g=8); t2k = tB[d][1]
                    rk = [Hl[d][1], Sa[d][1], Si[d][1], CAk, CBnk, CBpk]
                    B.tt(eng, t1, src, LAb, ALU.mult, rk, [t1k])
                    B.tt(eng, t2[:, 0], src[:, 1], LNb, ALU.mult, rk, [t2k])
                    B.tt(eng, t2[:, 1], src[:, 0], LPb, ALU.mult, rk, [t2k])
                    B.tt(eng, t1, t1, t2, ALU.add, [t1k, t2k], [t1k])
                    return t1, t1k
                A5 = [vA(Hl[d][0]) for d in range(2)]; Ak = [Hl[d][1] for d in range(2)]
                S4 = [vS(Sa[d][0]) for d in range(2)]; Sk = [Sa[d][1] for d in range(2)]
                I4 = [vS(Si[d][0]) for d in range(2)]; Ik = [Si[d][1] for d in range(2)]
                for step in range(7):
                    for d in range(2):
                        i = step + 1 if d == 0 else 6 - step
                        prev = i - 1 if d == 0 else i + 1
                        t1, t1k = cm(d, A5[d][:, :, :, :, prev], 0, 36)
                        B.tt(engs[d], A5[d][:, :, :, :, i], A5[d][:, :, :, :, i], t1, ALU.add, [Ak[d], t1k], [Ak[d]])
                    yield
                seqs = [[(0, None)] + [(b_, b_ - 1) for b_ in range(1, 36)],
                        [(3, None), (2, 3), (1, 2), (0, 1), (35, 0)] + [(b_, b_ + 1) for b_ in range(34, 3, -1)]]
                endi = [7, 0]
                for step in range(36):
                    for d in range(2):
                        bd_, bs_ = seqs[d][step]
                        if bs_ is None:
                            B.cp(engs[d], S4[d][:, :, :, bd_:bd_ + 1], A5[d][:, :, :, bd_:bd_ + 1, endi[d]], [Ak[d]], [Sk[d]])
                        else:
                            t1, t1k = cm(d, S4[d][:, :, :, bs_:bs_ + 1], 7, 1)
                            B.tt(engs[d], S4[d][:, :, :, bd_:bd_ + 1], A5[d][:, :, :, bd_:bd_ + 1, endi[d]], t1, ALU.add, [Ak[d], t1k], [Sk[d]])
                    if step % 2 == 1:
                        yield
                B.ms(engs[0], I4[0][:, :, :, 0:1], 0.0, [Ik[0]])
                B.cp(engs[0], I4[0][:, :, :, 1:36], S4[0][:, :, :, 0:35], [Sk[0]], [Ik[0]])
                B.ms(engs[1], I4[1][:, :, :, 3:4], 0.0, [Ik[1]])
                B.cp(engs[1], I4[1][:, :, :, 0:3], S4[1][:, :, :, 1:4], [Sk[1]], [Ik[1]])
                B.cp(engs[1], I4[1][:, :, :, 4:35], S4[1][:, :, :, 5:36], [Sk[1]], [Ik[1]])
                B.cp(engs[1], I4[1][:, :, :, 35:36], S4[1][:, :, :, 0:1], [Sk[1]], [Ik[1]])
                yield
                for i in range(8):
                    for d in range(2):
                        pw = i if d == 0 else 7 - i
                        t1, t1k = cm(d, I4[d], pw, 36)
                        B.tt(engs[d], A5[d][:, :, :, :, i], A5[d][:, :, :, :, i], t1, ALU.add, [Ak[d], t1k], [Ak[d]])
                    yield
                vK = lambda a_: a_.rearrange("p (r g k) -> p r g k", r=2, g=8)
                Af = vK(Hl[0][0]); Ab = vK(Hl[1][0]); Hbf = vK(v['Hb'][0][0]); Hbb = vK(v['Hb'][1][0])
                hk0 = v['Hb'][0][1]; hk1 = v['Hb'][1][1]
                B.ms('vector', Hbf[:, :, :, 0:1], 0.0, [hk0])
                B.cp('scalar', Hbf[:, :, :, 1:288], Af[:, :, :, 0:287], [Hl[0][1]], [hk0])
                B.cp('scalar', Hbb[:, :, :, 0:287], Ab[:, :, :, 1:288], [Hl[1][1]], [hk1])
                B.ms('vector', Hbb[:, :, :, 31:32], 0.0, [hk1])
                B.cp('vector', Hbb[:, :, :, 287:288], Ab[:, :, :, 0:1], [Hl[1][1]], [hk1])
                yield

            def outp(j, gen):
                v = views(j); rows = slice(j * 128, (j + 1) * 128)
                gi = 0
                for t in range(8):
                    for b in range(NB):
                        ps, psk = B.bank()
                        first = True
                        for s_ in range(0, t + 1):
                            B.mm(ps[:, 0:288], v['Wj4'][:, 0, t - s_, :], v['hj4'][:, b, :, s_], first, False, [v['Wjk'], v['hjk']], [psk]); first = False
                        for s_ in range(t, 8):
                            B.mm(ps[:, 0:288], v['Wj4'][:, 1, s_ - t, :], v['hj4'][:, b, :, s_], False, False, [v['Wjk'], v['hjk']], [psk])
                        cnt_ = 0
                        for d in range(2):
                            Hb5 = v5(v['Hb'][d][0], 288)
                            for ri in range(2):
                                for q in range(4):
                                    cnt_ += 1
                                    last_round = (d == 1 and ri == 1)
                                    if q < 3:
                                        B.mm(ps[32 * q:32 * q + 32, 0:288], v['Cj6'][:, d, t, ri, q, :], Hb5[:, ri, q, b, 0:288], False, last_round and q < 2, [v['Cjk'], v['Hb'][d][1]], [psk])
                                    else:
                                        B.mm(ps[64:128, 0:288], v['Cjz5'][:, d, t, ri, :], Hb5[:, ri, q, b, 0:288], False, last_round, [v['Cjzk'], v['Hb'][d][1]], [psk])
                        (ya, yak), (yb, ybk) = yas[gi % 2]; gi += 1
                        B.cp('scalar', ya, ps[:, 0:288], [psk], [yak])
                        B.act(yb, ya, AF.Square, [yak], [ybk])
                        B.ts('vector', yb, yb, 0.044715, 1.0, ALU.mult, ALU.add, [ybk], [ybk])
                        B.tt('vector', yb, yb, ya, ALU.mult, [ybk, yak], [ybk])
                        B.act(yb, yb, AF.Sigmoid, [ybk], [ybk], scale=1.5957691216057308)
                        B.tt('vector', ysb4[:, b, :, t], ya, yb, ALU.mult, [yak, ybk], [ysbk])
                        if gen is not None:
                            for _ in range(5):
                                next(gen, None)
                if gen is not None:
                    for _ in gen:
                        pass
                for b in range(NB):
                    B.dma('sync', sT[rows, TL + b * NCX:TL + (b + 1) * NCX], ysb3[:, b, 0:NCX], [ysbk], [('sT', j)])
                    B.dma('sync', sT[rows, b * NL:(b + 1) * NL], ysb3[:, b, NCX:2304], [ysbk], [('sT', j)])

            load(0); state(0)
            for _ in chain(0):
                pass
            load(1)
            for j in range(8):
                gen = None
                if j + 1 < 8:
                    state(j + 1)
                    gen = chain(j + 1)
                outp(j, gen)
                if j + 2 < 8:
                    load(j + 2)

        def phase_glu():
            B.phase()
            wg, wgk = T("wglu", 8 * 2048, BF16); wg3 = wg.rearrange("p (k n) -> p k n", n=2048)
            for k in range(8):
                B.dma('gpsimd', wg3[:, k, :], s5_w_glu[0, k * 128:(k + 1) * 128, :], [], [wgk])
            bg, bgk = T("bglu", 2048)
            B.dma('sync', bg, s5_b_glu[0:1, :].partition_broadcast(128), [], [bgk])
            gts = load_gate_tiles(0, 2 * D)
            bufs = [(T("yt%d" % i, 1024, BF16), T("xt%d" % i, D), T("xn%d" % i, D), T("at%d" % i, 512), T("gt%d" % i, 512)) for i in range(3)]
            skeys = [('sT', j) for j in range(8)]
            def glu_gen(i):
                (yt, ytk), (xt, xk), (xn, xnk), (a_t, atk), (g_t, gtk) = bufs[i % 3]
                yt3 = yt.rearrange("p (k t) -> p k t", t=128)
                B.dma('scalar', yt3, sT3[:, :, i * 128:(i + 1) * 128], skeys, [ytk])
                B.dma('sync', xt, xa[i * 128:(i + 1) * 128, :], [xkeys[i]], [xk])
                yield
                pss = [B.bank() for n in range(4)]
                for n in range(4):
                    for k in range(8):
                        B.mm(pss[n][0], yt3[:, k, :], wg3[:, k, n * 512:(n + 1) * 512], k == 0, k == 7, [ytk, wgk], [pss[n][1]])
                yield
                GT, GTk = gts[tile_row(i)]
                for hh in range(2):
                    cs = slice(hh * 512, (hh + 1) * 512)
                    B.tt('vector', a_t, pss[hh][0], bg[:, cs], ALU.add, [pss[hh][1], bgk], [atk])
                    B.tt('vector', g_t, pss[2 + hh][0], bg[:, 1024 + hh * 512:1024 + (hh + 1) * 512], ALU.add, [pss[2 + hh][1], bgk], [gtk])
                    B.act(g_t, g_t, AF.Sigmoid, [gtk], [gtk])
                    B.tt('gpsimd', a_t, a_t, g_t, ALU.mult, [atk, gtk], [atk])
                    B.tt('gpsimd', a_t, a_t, GT[:, cs], ALU.mult, [atk, GTk], [atk])
                    B.tt('gpsimd', xn[:, cs], a_t, xt[:, cs], ALU.add, [atk, xk], [xnk])
                B.dma('sync', xa[i * 128:(i + 1) * 128, :], xn, [xnk], [xkeys[i]])
            run_pipeline([glu_gen(i) for i in range(NT)])


        def phase_moe(l, ntiles, final):
            B.phase()
            mts = load_mod_tiles(l, g_ffn, 3 * D, 4 * D)
            wr, wrk = T("wr", 8 * 32); wr3 = wr.rearrange("p (k e) -> p k e", e=32)
            B.dma('sync', wr3, moe_w_router[l].rearrange("(k p) e -> p k e", p=128), [], [wrk])
            brb, brbk = T("brb", 32); B.dma('sync', brb, moe_b_router[l:l + 1, :].partition_broadcast(128), [], [brbk])
            slot0, slot0k = T("slot0", 32); B.dma('sync', slot0, k_slot, [], [slot0k])
            base, basek = T("base", 32); B.ms('vector', base, 0.0, [basek])
            trash, trashk = T("trash", 2); B.dma('sync', trash[:, 0:1], k_trash, [], [trashk])
            zt, ztk = T("zt", D, BF16); B.ms('gpsimd', zt, 0.0, [ztk])
            B.dma('sync', Ys[NSLOT:NSLOT + 128, :], zt, [ztk], ['Ys'])
            NRB = 4
            bufs = [(T("xt%d" % i, D), T("h%d" % i, D), T("hTt%d" % i, D), T("ss%d" % i, 2)) for i in range(NRB)]
            smalls = [(T("lg%d" % z, 32), T("top8%d" % z, 8), T("mask%d" % z, 32), T("sl%d" % z, 32),
                       T("oh%d" % z, 32 * 4), T("nb%d" % z, 2), T("ex%d" % z, 4), T("destf%d" % z, 4), T("ovf%d" % z, 32), T("tmo%d" % z, 32), T("vld%d" % z, 4)) for z in range(NRB)]

            def router_gen(i):
                (lg, lgk), (top8, top8k), (mask, maskk), (sl, slk), (oh4, ohk), (nb, nbk), (ex, exk), (destf, destfk), (ovf, ovfk), (tmo, tmok), (vld, vldk) = smalls[i % NRB]
                (xt, xk), (h, hk), (ht, htk), (ss, ssk) = bufs[i % NRB]
                B.dma('sync', xt, xa[i * 128:(i + 1) * 128, :], [xkeys[i]], [xk])
                g_ = norm_tile_g(xt, xk, h, hk, mts[tile_row(i)], ss, ssk)
                next(g_)
                yield
                next(g_); next(g_)
                ht3 = ht.rearrange("p (k t) -> p k t", t=128)
                transpose_tile(h, hk, ht3, htk)
                yield
                ps, psk = B.bank()
                for k in range(8):
                    B.mm(ps[:, 0:32], ht3[:, k, :], wr3[:, k, :], k == 0, k == 7, [htk, wrk], [psk])
                B.tt('vector', lg, ps[:, 0:32], brb, ALU.add, [psk, brbk], [lgk])
                B.P.op('vector', lambda e: e.max(out=top8, in_=lg), [lgk], [top8k])
                B.ts('vector', mask, lg, top8[:, 3:4], None, ALU.is_ge, None, [lgk, top8k], [maskk])
                B.ts('vector', nb[:, 0:1], top8[:, 0:1], -1.0, None, ALU.mult, None, [top8k], [nbk])
                B.ms('vector', nb[:, 1:2], 0.0, [nbk])
                B.act(ex, top8[:, 0:4], AF.Exp, [top8k, nbk], [exk, nbk], bias=nb[:, 0:1], scale=1.0, accum_out=nb[:, 1:2])
                ps2, ps2k = B.bank()
                B.mm(ps2[:, 0:32], ltri, mask, True, True, [ltrik, maskk], [ps2k])
                ps3, ps3k = B.bank()
                B.mm(ps3[:, 0:32], ones, mask, True, True, [onesk, maskk], [ps3k])
                yield
                B.P.op('vector', lambda e: e.reciprocal(out=nb[:, 1:2], in_=nb[:, 1:2]), [nbk], [nbk])
                B.ts('vector', ex, ex, nb[:, 1:2], None, ALU.mult, None, [exk, nbk], [exk])
                B.tt('vector', sl, ps2[:, 0:32], base, ALU.add, [ps2k, basek], [slk])
                B.ts('vector', ovf, sl, float(CAP), None, ALU.is_ge, None, [slk], [ovfk])
                B.tt('vector', sl, sl, slot0, ALU.add, [slk, slot0k], [slk])
                B.ts('vector', tmo, sl, -1.0, trash[:, 0:1], ALU.mult, ALU.add, [slk, trashk], [tmok])
                B.tt('vector', tmo, tmo, ovf, ALU.mult, [tmok, ovfk], [tmok])
                B.tt('vector', sl, sl, tmo, ALU.add, [slk, tmok], [slk])
                B.tt('vector', base, base, ps3[:, 0:32], ALU.add, [basek, ps3k, slk], [basek])
                for k in range(4):
                    oh = oh4[:, 32 * k:32 * (k + 1)]
                    B.ts('vector', oh, lg, top8[:, k:k + 1], None, ALU.is_equal, None, [lgk, top8k], [ohk])
                    B.tt('vector', oh, oh, sl, ALU.mult, [ohk, slk], [ohk])
                for k in range(4):
                    B.P.op('vector', lambda e, k=k: e.reduce_sum(out=destf[:, k:k + 1], in_=oh4[:, 32 * k:32 * (k + 1)], axis=AX.X), [ohk], [destfk])
                B.ts('vector', destf, destf, float(NSLOT + 127), None, ALU.min, None, [destfk], [destfk])
                B.ts('vector', vld, destf, float(NSLOT), None, ALU.is_lt, None, [destfk], [vldk])
                B.tt('vector', GATE3[:, i, :], ex, vld, ALU.mult, [exk, vldk], [GATEk])
                B.cp('vector', DEST3[:, i, :], destf, [destfk], [DESTk])
                for k in range(4):
                    B.P.dma('gpsimd', lambda e, k=k: e.indirect_dma_start(out=Xs, out_offset=bass.IndirectOffsetOnAxis(ap=DEST3[:, i, k:k + 1], axis=0), in_=h, in_offset=None), [hk, DESTk], ['Xs'])
            run_pipeline([router_gen(i) for i in range(ntiles)])
            B.dma('sync', cnt_out[:, 32 * l:32 * (l + 1)], base, [basek], [('cnt', l)])
            B.phase()
            wgu = [T("wgu%d" % i, 8 * 2048, BF16) for i in range(2)]
            wd = [T("wd%d" % i, 8 * 1024, BF16) for i in range(2)]
            bgu = [T("bgu%d" % i, 16) for i in range(2)]; bdb = [T("bdb%d" % i, D) for i in range(2)]
            xTs = [T("xT%d" % i, 8 * CAP, BF16) for i in range(2)]
            aT, aTk = T("aT", 8 * CAP, BF16); aT3 = aT.rearrange("p (k t) -> p k t", t=CAP)
            xs = [T("xs%d" % i, D, BF16) for i in range(CAPT)]; yo = [T("yo%d" % i, D, BF16) for i in range(2)]
            ep = [(T("g_t%d" % i, HALF), T("u_t%d" % i, HALF), T("s_t%d" % i, HALF)) for i in range(2)]
            cnt = 0

            def load_w(e):
                (wg, wgk) = wgu[e % 2]; (wdd, wdk) = wd[e % 2]; (bg, bgk) = bgu[e % 2]; (bd, bdk) = bdb[e % 2]
                wg3 = wg.rearrange("p (k n) -> p k n", n=2048); wd3 = wdd.rearrange("p (k n) -> p k n", n=1024)
                for k4 in range(2):
                    B.dma('gpsimd', wg3[:, 4 * k4:4 * k4 + 4, :], moe_w_gu[l, e, 512 * k4:512 * (k4 + 1), :].rearrange("(k p) n -> p k n", p=128), [], [wgk])
                B.dma('gpsimd', wd3, moe_w_down[l, e].rearrange("(k p) n -> p k n", p=128), [], [wdk])
                B.dma('scalar', bg, moe_b_gu[l, e].rearrange("(c p) -> p c", p=128), [], [bgk], nc_ok=True)
                B.dma('scalar', bd, moe_b_down[l, e:e + 1, :].partition_broadcast(128), [], [bdk])

            def load_x(e):
                for stl in range(CAPT):
                    r0 = e * CAP + stl * 128
                    B.dma('sync', xs[stl][0], Xs[r0:r0 + 128, :], ['Xs'], [xs[stl][1]])

            def transposes(e):
                xT, xTk = xTs[e % 2]; xT3 = xT.rearrange("p (k t) -> p k t", t=CAP)
                for stl in range(CAPT):
                    (x_, x_k) = xs[stl]
                    ps, psk = B.bank()
                    psb = ps.bitcast(BF16)
                    for k in range(8):
                        B.P.op('tensor', lambda e, o=psb[:, k * 128:(k + 1) * 128], i_=x_[:, k * 128:(k + 1) * 128]: e.transpose(o, i_, identb), [x_k, identbk], [psk])
                    B.cp('scalar' if stl % 2 == 0 else 'vector', xT3[:, :, stl * 128:(stl + 1) * 128], psb.rearrange("p (k t) -> p k t", t=128), [psk], [xTk])
            load_w(0); load_x(0); transposes(0); load_x(1)
            for e in range(32):
                (wg, wgk) = wgu[e % 2]; (wdd, wdk) = wd[e % 2]; (bg, bgk) = bgu[e % 2]; (bd, bdk) = bdb[e % 2]
                wg3 = wg.rearrange("p (k n) -> p k n", n=2048); wd3 = wdd.rearrange("p (k n) -> p k n", n=1024)
                xT, xTk = xTs[e % 2]; xT3 = xT.rearrange("p (k t) -> p k t", t=CAP)
                if e + 1 < 32:
                    load_w(e + 1)
                for fc in range(8):
                    for hf in range(2):
                        cols = slice(hf * HALF, (hf + 1) * HALF)
                        (g_t, gk_), (u_t, uk_), (s_t, sk_) = ep[cnt % 2]; cnt += 1
                        psg, psgk = B.bank(); psu, psuk = B.bank()
                        for k in range(8):
                            B.mm(psg[:, 0:HALF], wg3[:, k, fc * 128:(fc + 1) * 128], xT3[:, k, cols], k == 0, k == 7, [wgk, xTk], [psgk])
                        for k in range(8):
                            B.mm(psu[:, 0:HALF], wg3[:, k, 1024 + fc * 128:1024 + (fc + 1) * 128], xT3[:, k, cols], k == 0, k == 7, [wgk, xTk], [psuk])
                        B.ts('vector', g_t, psg[:, 0:HALF], bg[:, fc:fc + 1], 7.0, ALU.add, ALU.min, [psgk, bgk], [gk_])
                        B.ts('vector', u_t, psu[:, 0:HALF], bg[:, 8 + fc:9 + fc], 7.0, ALU.add, ALU.min, [psuk, bgk], [uk_])
                        B.ts('vector', u_t, u_t, -7.0, 1.0, ALU.max, ALU.add, [uk_], [uk_])
                        B.act(s_t, g_t, AF.Sigmoid, [gk_], [sk_], scale=1.702)
                        B.tt('gpsimd', g_t, g_t, s_t, ALU.mult, [gk_, sk_], [gk_])
                        B.tt('vector', aT3[:, fc, cols], g_t, u_t, ALU.mult, [gk_, uk_], [aTk])
                if e + 1 < 32:
                    transposes(e + 1)
                    if e + 2 < 32:
                        load_x(e + 2)
                for stl in range(CAPT):
                    (y_, y_k) = yo[stl % 2]
                    for nh in range(2):
                        ps, psk = B.bank()
                        for k in range(8):
                            B.mm(ps, aT3[:, k, stl * 128:(stl + 1) * 128], wd3[:, k, nh * 512:(nh + 1) * 512], k == 0, k == 7, [aTk, wdk], [psk])
                        B.tt('vector', y_[:, nh * 512:(nh + 1) * 512], ps, bd[:, nh * 512:(nh + 1) * 512], ALU.add, [psk, bdk], [y_k])
                    r0 = e * CAP + stl * 128
                    B.dma('sync', Ys[r0:r0 + 128, :], y_, [y_k], ['Ys'])
            B.phase()
            gts = load_gate_tiles(l, 5 * D)
            bufs = [([T("cg%d_%d" % (i, k), D, BF16) for k in range(4)], T("acc%d" % i, D), T("cx%d" % i, D)) for i in range(3)]
            def comb_gen(i):
                gl, (acc, acck), (xt, xk) = bufs[i % 3]
                B.dma('sync', xt, xa[i * 128:(i + 1) * 128, :], [xkeys[i]], [xk])
                for k in range(4):
                    B.P.dma('gpsimd', lambda e, i=i, k=k, g=gl[k][0]: e.indirect_dma_start(out=g, out_offset=None, in_=Ys, in_offset=bass.IndirectOffsetOnAxis(ap=DEST3[:, i, k:k + 1], axis=0)), ['Ys', DESTk], [gl[k][1]])
                yield
                yield
                B.ts('vector', acc, gl[0][0], GATE3[:, i, 0:1], None, ALU.mult, None, [gl[0][1], GATEk], [acck])
                for k in range(1, 4):
                    B.stt('vector', acc, gl[k][0], GATE3[:, i, k:k + 1], acc, ALU.mult, ALU.add, [gl[k][1], GATEk, acck], [acck])
                GT, GTk = gts[tile_row(i)]
                B.tt('gpsimd', acc, acc, GT, ALU.mult, [acck, GTk], [acck])
                B.tt('gpsimd', acc, acc, xt, ALU.add, [acck, xk], [acck])
                if final:
                    B.dma('sync', outf[i * 128:(i + 1) * 128, :], acc, [acck], [('out', i)])
                else:
                    B.dma('sync', xa[i * 128:(i + 1) * 128, :], acc, [acck], [xkeys[i]])
            run_pipeline([comb_gen(i) for i in range(ntiles)])

        def phase_attn():
            B.phase()
            B.brange = (0, 3)
            blk64, blk64k = T("blk64", 128); B.dma('sync', blk64, k_blk64, [], [blk64k])
            rot, rotk = T("rot", 128); B.dma('sync', rot, k_rot, [], [rotk])
            cs, csk = T("cos", NL); B.dma('sync', cs, k_cos, [], [csk])
            sn, snk = T("sin", NL); B.dma('scalar', sn, k_sin, [], [snk])
            gc, gck = T("gcols", 8)
            for c in range(2):
                B.dma('sync', gc[c * 64:(c + 1) * 64, 0:1], da_q_gain[0:1, :].rearrange("o d -> d o"), [], [gck], nc_ok=True)
                B.dma('sync', gc[c * 64:(c + 1) * 64, 1:2], da_k_gain[0:1, :].rearrange("o d -> d o"), [], [gck], nc_ok=True)
            B.dma('sync', gc[:, 2:3], da_sub_gain[0:1, :].rearrange("o d -> d o"), [], [gck], nc_ok=True)
            B.ts('vector', gc[:, 2:3], gc[:, 2:3], 1.0 - LAMBDA_INIT, None, ALU.mult, None, [gck], [gck])
            lt = [T("lamt%d" % i, 64) for i in range(4)]
            for i in range(4):
                B.dma('sync', lt[i][0], da_lam[i][0:1, :].partition_broadcast(128), [], [lt[i][1]])
            for pr in range(2):
                a_, ak_ = lt[2 * pr]; b_, bk_ = lt[2 * pr + 1]
                B.tt('vector', a_, a_, b_, ALU.mult, [ak_, bk_], [ak_])
                B.P.op('vector', lambda e, a_=a_, pr=pr: e.reduce_sum(out=gc[:, 5 + pr:6 + pr], in_=a_, axis=AX.X), [ak_], [gck])
            B.act(gc[:, 5:7], gc[:, 5:7], AF.Exp, [gck], [gck])
            B.tt('vector', gc[:, 3:4], gc[:, 5:6], gc[:, 6:7], ALU.subtract, [gck], [gck])
            B.ts('vector', gc[:, 4:5], gc[:, 3:4], LAMBDA_INIT, -1.0, ALU.add, ALU.mult, [gck], [gck])
            wo, wok = T("wo", 8 * 1024, BF16); wo3 = wo.rearrange("p (k n) -> p k n", n=1024)
            B.dma('gpsimd', wo3, da_w_o[0].rearrange("(k p) n -> p k n", p=128), [], [wok])
            gts = load_gate_tiles(1, 2 * D)
            hb, hbk = T("hb", 8 * 2304, BF16); hb3 = hb.rearrange("p (k t) -> p k t", t=2304)
            onT, onTk = T("onT", 8 * NL, BF16); onT3 = onT.rearrange("p (h t) -> p h t", t=NL)
            wq, wqk = T("wq", 1024, BF16); wk_, wkk = T("wk", 1024, BF16); wv, wvk = T("wv", 1024, BF16)
            wq3 = wq.rearrange("p (k n) -> p k n", n=128); wk3 = wk_.rearrange("p (k n) -> p k n", n=128); wv3 = wv.rearrange("p (k n) -> p k n", n=128)
            qn, qnk = T("qn", NL, BF16)
            kz = [T("kz%d" % c, 2304, BF16) for c in range(2)]
            for c in range(2):
                B.ms('vector', kz[c][0], 0.0, [kz[c][1]])
            Es2 = [[T("Es%d_%d" % (p_, c), 512) for c in range(2)] for p_ in range(2)]
            vv, vvk = T("vv", 18 * 128, BF16); vv3 = vv.rearrange("p (t e) -> p t e", e=128)
            Eb = [T("E%d" % i, 512, BF16) for i in range(3)]
            pn_bufs = [(T("qf%d" % z, 512), T("sq%d" % z, 512), T("rs%d" % z, 512), T("trp%d" % z, 512)) for z in range(2)]
            pn_cnt = [0]
            (sq, sqk), (rs, rsk) = pn_bufs[0][1], pn_bufs[0][2]
            c0, c0k = T("c0", 512); c1, c1k = T("c1", 512)
            xb = [(T("axt%d" % i, D), T("axn%d" % i, D)) for i in range(2)]
            hkeys = [('hT', i) for i in range(NT)]
            wqkv3 = da_w_qkv[0].rearrange("(k p) n -> p k n", p=128)

            def proj_norm(w3, wkey, col0, ncols, gcol, rope0, dst, dstk):
                (qf, qfk), (sq, sqk), (rs, rsk), (tr_, trk) = pn_bufs[pn_cnt[0] % 2]; pn_cnt[0] += 1
                ps, psk = B.bank()
                for k in range(8):
                    B.mm(ps[:, 0:ncols], w3[:, k, :], hb3[:, k, col0:col0 + ncols], k == 0, k == 7, [wkey, hbk], [psk])
                B.cp('scalar', qf[:, 0:ncols], ps[:, 0:ncols], [psk], [qfk])
                B.act(sq[:, 0:ncols], qf[:, 0:ncols], AF.Square, [qfk], [sqk])
                ps2, ps2k = B.bank()
                B.mm(ps2[:, 0:ncols], blk64, sq[:, 0:ncols], True, True, [blk64k, sqk], [ps2k])
                B.ts('vector', rs[:, 0:ncols], ps2[:, 0:ncols], 1e-6, None, ALU.add, None, [ps2k], [rsk])
                B.act(rs[:, 0:ncols], rs[:, 0:ncols], AF.Ln, [rsk], [rsk])
                B.act(rs[:, 0:ncols], rs[:, 0:ncols], AF.Exp, [rsk], [rsk], scale=-0.5)
                B.stt('vector', qf[:, 0:ncols], qf[:, 0:ncols], gcol, rs[:, 0:ncols], ALU.mult, ALU.mult, [qfk, gck, rsk], [qfk])
                if rope0 is None:
                    if isinstance(dst, list):
                        for (d_ap, d_k, rs_) in dst:
                            B.cp('vector', d_ap[rs_], qf[rs_, 0:ncols], [qfk], [d_k])
                    else:
                        B.cp('vector', dst, qf[:, 0:ncols], [qfk], [dstk])
                else:
                    ps3, ps3k = B.bank()
                    B.mm(ps3[:, 0:ncols], rot, qf[:, 0:ncols], True, True, [rotk, qfk], [ps3k])
                    B.tt('vector', tr_[:, 0:ncols], ps3[:, 0:ncols], sn[:, rope0:rope0 + ncols], ALU.mult, [ps3k, snk], [trk])
                    B.tt('gpsimd', sq[:, 0:ncols], qf[:, 0:ncols], cs[:, rope0:rope0 + ncols], ALU.mult, [qfk, csk, sqk], [sqk])
                    if isinstance(dst, list):
                        for (d_ap, d_k, rs_) in dst:
                            B.tt('vector', d_ap[rs_], sq[rs_, 0:ncols], tr_[rs_, 0:ncols], ALU.add, [sqk, trk], [d_k])
                    else:
                        B.tt('vector', dst, sq[:, 0:ncols], tr_[:, 0:ncols], ALU.add, [sqk, trk], [dstk])

            ecnt = 0
            for b in range(NB):
                B.dma('gpsimd', hb3[:, :, 0:NCX], hT3[:, :, TL + b * NCX:TL + (b + 1) * NCX], hkeys, [hbk])
                B.dma('gpsimd', hb3[:, :, NCX:2304], hT3[:, :, b * NL:(b + 1) * NL], hkeys, [hbk])
                for h in range(8):
                    B.dma('gpsimd', wq3, wqkv3[:, :, h * 128:(h + 1) * 128], [], [wqk])
                    B.dma('gpsimd', wk3, wqkv3[:, :, D + h * 128:D + (h + 1) * 128], [], [wkk])
                    B.dma('gpsimd', wv3, wqkv3[:, :, 2 * D + h * 128:2 * D + (h + 1) * 128], [], [wvk])
                    for qc in range(4):
                        proj_norm(wq3, wqk, NCX + qc * 512, 512, gc[:, 0:1], qc * 512, qn[:, qc * 512:(qc + 1) * 512], qnk)
                    def kdst(c0_, c1_):
                        return [(kz[c][0][:, c0_:c1_], kz[c][1], slice(c * 64, (c + 1) * 64)) for c in range(2)]
                    proj_norm(wk3, wkk, 0, NCX, gc[:, 1:2], None, kdst(0, NCX), None)
                    for qc in range(4):
                        proj_norm(wk3, wkk, NCX + qc * 512, 512, gc[:, 1:2], qc * 512, kdst(NCX + qc * 512, NCX + (qc + 1) * 512), None)
                    for kt in range(18):
                        ps, psk = B.bank()
                        for k in range(8):
                            B.mm(ps[:, 0:128], hb3[:, k, kt * 128:(kt + 1) * 128], wv3[:, k, :], k == 0, k == 7, [hbk, wvk], [psk])
                        B.cp('scalar' if kt % 2 == 0 else 'vector', vv3[:, kt, :], ps[:, 0:128], [psk], [vvk])
                    pending = None
                    for qc in range(4):
                        qs = slice(qc * 512, (qc + 1) * 512)
                        par = qc % 2
                        items = [(c, kt) for kt in range(18) for c in range(2)]
                        sc = {}

                        def score(ii, qs=qs):
                            c, kt = items[ii]
                            ps, psk = B.bank()
                            B.mm(ps, kz[c][0][:, kt * 128:(kt + 1) * 128], qn[:, qs], True, True, [kz[c][1], qnk], [psk])
                            sc[ii] = (ps, psk)

                        def make_combine(par=par, qs=qs, h=h):
                            def combine():
                                pd, pdk = pst[7][:], 'ps7'
                                for c, (cc, cck) in enumerate(((c0, c0k), (c1, c1k))):
                                    po, pok = pst[3 + 2 * par + c][:], 'ps%d' % (3 + 2 * par + c)
                                    Es_, Esk_ = Es2[par][c]
                                    B.mm(pd, ones, Es_, True, True, [onesk, Esk_], [pdk])
                                    B.act(cc, pd, AF.Ln, [pdk], [cck])
                                    B.act(cc, cc, AF.Exp, [cck], [cck], scale=-1.0)
                                    B.tt('vector', cc, cc, po, ALU.mult, [cck, pok], [cck])
                                B.stt('vector', c0, c1, gc[:, 4:5], c0, ALU.mult, ALU.add, [c1k, gck, c0k], [c0k])
                                B.act(sq, c0, AF.Square, [c0k], [sqk])
                                B.mm(pd, ones, sq, True, True, [onesk, sqk], [pdk])
                                B.ts('vector', rs, pd, 1.0 / 128, 1e-6, ALU.mult, ALU.add, [pdk], [rsk])
                                B.act(rs, rs, AF.Ln, [rsk], [rsk])
                                B.act(rs, rs, AF.Exp, [rsk], [rsk], scale=-0.5)
                                B.stt('vector', onT3[:, h, qs], c0, gc[:, 2:3], rs, ALU.mult, ALU.mult, [c0k, gck, rsk], [onTk])
                            return combine
                        score(0); score(1)
                        for ii, (c, kt) in enumerate(items):
                            if ii + 2 < len(items):
                                score(ii + 2)
                            if ii == 8 and pending is not None:
                                pending(); pending = None
                            po, pok = pst[3 + 2 * par + c][:], 'ps%d' % (3 + 2 * par + c)
                            Es_, Esk_ = Es2[par][c]
                            ps, psk = sc.pop(ii)
                            E_, Ek_ = Eb[ecnt % 3]; ecnt += 1
                            B.act(E_, ps, AF.Exp, [psk], [Ek_], scale=0.125)
                            B.mm(po, vv3[:, kt, :], E_, kt == 0, kt == 17, [vvk, Ek_], [pok])
                            eng_ = 'vector' if c == 0 else 'gpsimd'
                            if kt == 0:
                                B.cp(eng_, Es_, E_, [Ek_], [Esk_])
                            else:
                                B.tt(eng_, Es_, Es_, E_, ALU.add, [Ek_, Esk_], [Esk_])
                        pending = make_combine()
                    pending(); pending = None
                GT, GTk = gts[b]
                for tt_ in range(16):
                    i = b * 16 + tt_
                    (xt, xk), (xn, xnk) = xb[tt_ % 2]
                    B.dma('sync', xt, xa[i * 128:(i + 1) * 128, :], [xkeys[i]], [xk])
                    for nh in range(2):
                        cs_ = slice(nh * 512, (nh + 1) * 512)
                        ps, psk = B.bank()
                        for h in range(8):
                            B.mm(ps, onT3[:, h, tt_ * 128:(tt_ + 1) * 128], wo3[:, h, cs_], h == 0, h == 7, [onTk, wok], [psk])
                        B.tt('vector', xn[:, cs_], ps, GT[:, cs_], ALU.mult, [psk, GTk], [xnk])
                        B.tt('gpsimd', xn[:, cs_], xn[:, cs_], xt[:, cs_], ALU.add, [xnk, xk], [xnk])
                    B.dma('sync', xa[i * 128:(i + 1) * 128, :], xn, [xnk], [xkeys[i]])
            B.brange = (0, 8)

        phase_norm_T(0)
        if done('norm0'):
            P.emit(); return nc
        phase_s5_setup()
        if done('s5setup'):
            P.emit(); return nc
        phase_s5_main()
        if done('s5main'):
            P.emit(); return nc
        phase_glu()
        if done('glu'):
            P.emit(); return nc
        B.persist = persist_small
        phase_moe(0, NT, False)
        if done('moe0'):
            P.emit(); return nc
        phase_norm_T(1)
        phase_attn()
        if done('attn'):
            P.emit(); return nc
        phase_moe(1, 32, True)
        P.emit()
        print("nops", P.nops, {e: len(P.ops[e]) for e in ENGS})
    return nc


def make_consts():
    i = np.arange(128)
    k = {}
    k["k_ident"] = np.eye(128, dtype=np.float32)
    k["k_ltri"] = (i[:, None] < i[None, :]).astype(np.float32)
    k["k_ones"] = np.ones((128, 128), np.float32)
    k["k_blk16"] = (i[:, None] // 16 == i[None, :] // 16).astype(np.float32)
    k["k_g2m"] = (i[:, None] // 64 == np.arange(2)[None, :]).astype(np.float32)
    k["k_blk64"] = (i[:, None] // 64 == i[None, :] // 64).astype(np.float32) / 64.0
    rot = np.zeros((128, 128), np.float32)
    for c in range(2):
        for d in range(64):
            if d < 32:
                rot[c * 64 + d + 32, c * 64 + d] = -1.0
            else:
                rot[c * 64 + d - 32, c * 64 + d] = 1.0
    k["k_rot"] = rot
    tok = np.arange(NL)
    row = (tok // 64).astype(np.float32); col = (tok % 64).astype(np.float32)
    inv = np.exp(-math.log(10000.0) * np.arange(16, dtype=np.float32) / 16).astype(np.float32)
    ang = np.concatenate([row[:, None] * inv, col[:, None] * inv], axis=-1).astype(np.float32)
    f = (i % 64) % 32
    k["k_cos"] = np.ascontiguousarray(np.cos(ang).astype(np.float32)[:, f].T)
    k["k_sin"] = np.ascontiguousarray(np.sin(ang).astype(np.float32)[:, f].T)
    k["k_pm"] = (((i[:, None] // 16) % 2) == np.arange(2)[None, :]).astype(np.float32)
    k["k_trash"] = (NSLOT + i).astype(np.float32).reshape(128, 1)
    k["k_slot"] = np.tile((np.arange(32, dtype=np.float32) * CAP)[None, :], (128, 1))
    return k


_NC_CACHE = {}


def kernel(**inputs):
    inp = {k: np.ascontiguousarray(np.asarray(v, dtype=np.float32)) for k, v in inputs.items()}
    if "nc" not in _NC_CACHE:
        _NC_CACHE["nc"] = build_nc()
    nc = _NC_CACHE["nc"]
    consts = make_consts()
    shared = {k: v for k, v in inp.items() if k not in ("x", "c", "ctx", "c_ctx")}
    for n in ("q1", "k1", "q2", "k2"):
        pass
    in_maps = []
    for core in range(8):
        m = dict(shared)
        m.update(consts)
        m["x"] = inp["x"][2 * core:2 * core + 2]
        m["ctx"] = inp["ctx"][2 * core:2 * core + 2]
        m["c"] = inp["c"][2 * core:2 * core + 2]
        m["c_ctx"] = inp["c_ctx"].reshape(1, D)
        in_maps.append(m)
    res = run_bass_kernel_spmd(nc, in_maps, core_ids=list(range(8)))
    _NC_CACHE["cnt"] = [r["cnt_out"][0] for r in res.results]
    return np.concatenate([r["out"] for r in res.results], axis=0).astype(np.float32)
```
